# Optimizing a Trainium2 kernel written in Bass

```python
import math
import jax, jax.numpy as jnp
from jax import lax
import numpy as np

D_MODEL = 1024
BATCH = 2
SEQ = 8192
DEPTH = 1

CHUNK = 64
Q_BLOCK = 128
GDN_HEADS = 4
GDN_DK = 128
GDN_DV = 128
GDN_CONV = 4
DIFF_HEADS = 4
DIFF_DH = 64
N_EXPERTS = 256
TOP_K = 8
N_GROUPS = 8
TOPK_GROUPS = 4
EXPERT_FF = 256
SHARED_FF = 256
ROUTED_SCALE = 2.5
EXPERT_BLOCK = 128
NORM_EPS = 1e-6

GDN_QK = GDN_HEADS * GDN_DK
GDN_V = GDN_HEADS * GDN_DV
CONV_CH = 2 * GDN_QK + GDN_V
DIFF_QK = DIFF_HEADS * 2 * DIFF_DH
DIFF_V = DIFF_HEADS * 2 * DIFF_DH
IN_SPLITS = (GDN_QK, GDN_QK, GDN_V, GDN_V, GDN_HEADS, GDN_HEADS, DIFF_QK, DIFF_QK, DIFF_V, D_MODEL, D_MODEL)
IN_COLS = sum(IN_SPLITS)

kernel_name = "hybrid_gdn_diffattn_moe_block"


def rms_norm(x, gain):
    xf = x.astype(jnp.float32)
    y = xf * lax.rsqrt(jnp.mean(xf * xf, axis=-1, keepdims=True) + NORM_EPS)
    return (y * gain.astype(jnp.float32)).astype(x.dtype)


def l2_normalize(x):
    xf = x.astype(jnp.float32)
    return (xf * lax.rsqrt(jnp.sum(xf * xf, axis=-1, keepdims=True) + NORM_EPS)).astype(x.dtype)


def causal_depthwise_conv(x, w):
    taps = w.shape[0]
    return lax.conv_general_dilated(
        x, w[:, None, :].astype(x.dtype), window_strides=(1,), padding=[(taps - 1, 0)],
        dimension_numbers=("NWC", "WIO", "NWC"), feature_group_count=x.shape[-1])


def gated_delta_rule_chunked(q, k, v, g, beta):
    b, s, h, dk = q.shape
    dv = v.shape[-1]
    n = s // CHUNK
    f32 = jnp.float32

    def chunks(t):
        return t.astype(f32).reshape(b, n, CHUNK, h, -1).transpose(0, 3, 1, 2, 4)

    q, k, v = chunks(q), chunks(k), chunks(v)
    g = chunks(g[..., None])[..., 0]
    beta = chunks(beta[..., None])[..., 0]
    G = jnp.cumsum(g, axis=-1)
    ii = jnp.arange(CHUNK)
    causal = ii[:, None] >= ii[None, :]
    strict = ii[:, None] > ii[None, :]
    decay = jnp.exp(jnp.where(causal, G[..., :, None] - G[..., None, :], -jnp.inf))
    kk = jnp.einsum("bhnik,bhnjk->bhnij", k, k)
    lower = jnp.where(strict, beta[..., :, None] * kk * decay, 0.0)
    a_mat = jnp.eye(CHUNK, dtype=f32) + lower
    rhs = jnp.concatenate([v * beta[..., None], k * (beta * jnp.exp(G))[..., None]], axis=-1)
    sol = lax.linalg.triangular_solve(a_mat, rhs, left_side=True, lower=True, unit_diagonal=True)
    u, w = sol[..., :dv], sol[..., dv:]
    a_qk = jnp.einsum("bhnik,bhnjk->bhnij", q, k) * decay

    def step(state, inp):
        qc, kc, uc, wc, gc, ac = inp
        v_new = uc - jnp.einsum("bhck,bhkv->bhcv", wc, state)
        out = (jnp.einsum("bhck,bhkv->bhcv", qc * jnp.exp(gc)[..., None], state)
               + jnp.einsum("bhcj,bhjv->bhcv", ac, v_new))
        g_last = gc[..., -1:]
        state = (state * jnp.exp(g_last)[..., None]
                 + jnp.einsum("bhck,bhcv->bhkv", kc * jnp.exp(g_last - gc)[..., None], v_new))
        return state, out

    mv = lambda t: jnp.moveaxis(t, 2, 0)
    state0 = jnp.zeros((b, h, dk, dv), f32)
    _, o = lax.scan(step, state0, (mv(q), mv(k), mv(u), mv(w), mv(G), mv(a_qk)))
    return o.transpose(1, 0, 3, 2, 4).reshape(b, s, h, dv)


def differential_attention(q, k, v, lam):
    b, s, h, _, d = q.shape
    n_blocks = s // Q_BLOCK
    pos = jnp.arange(s)
    slopes = 2.0 ** (-8.0 * jnp.arange(1, h + 1, dtype=jnp.float32) / h)
    scale = d ** -0.5

    def one_block(bi):
        start = bi * Q_BLOCK
        qb = lax.dynamic_slice_in_dim(q, start, Q_BLOCK, axis=1)
        tq = start + jnp.arange(Q_BLOCK)
        sc = jnp.einsum("bqhcd,bkhcd->bhcqk", qb, k, preferred_element_type=jnp.float32) * scale
        dist = jnp.abs(tq[:, None] - pos[None, :]).astype(jnp.float32)
        allowed = (pos[None, :] // CHUNK) <= (tq[:, None] // CHUNK)
        sc = jnp.where(allowed, sc - slopes[:, None, None, None] * dist, -jnp.inf)
        p = jax.nn.softmax(sc, axis=-1)
        a = p[:, :, 0] - lam * p[:, :, 1]
        return jnp.einsum("bhqk,bkhe->bqhe", a.astype(v.dtype), v)

    o = lax.map(one_block, jnp.arange(n_blocks))
    return o.transpose(1, 0, 2, 3, 4).reshape(b, s, h, 2 * d)


def hybrid_mixer(h, w_in, conv_w, a_log, dt_bias, gdn_norm_g, w_o_gdn, q_norm_g, k_norm_g,
                 lambda_q1, lambda_k1, lambda_q2, lambda_k2, subln_g, w_o_diff, w_out, lam_init):
    b, s, _ = h.shape
    f32 = jnp.float32
    proj = jnp.einsum("bsd,de->bse", h, w_in)
    split_at = np.cumsum(IN_SPLITS)[:-1].tolist()
    (qa, ka, va, za, beta_lin, decay_lin, qb, kb, vb, gate_a, gate_b) = jnp.split(proj, split_at, axis=-1)

    qkv = jax.nn.silu(causal_depthwise_conv(jnp.concatenate([qa, ka, va], axis=-1), conv_w))
    qa, ka, va = jnp.split(qkv, [GDN_QK, 2 * GDN_QK], axis=-1)
    qa = l2_normalize(qa.reshape(b, s, GDN_HEADS, GDN_DK)) * (GDN_DK ** -0.5)
    ka = l2_normalize(ka.reshape(b, s, GDN_HEADS, GDN_DK))
    va = va.reshape(b, s, GDN_HEADS, GDN_DV)
    beta = jax.nn.sigmoid(beta_lin.astype(f32))
    log_decay = -jnp.exp(a_log.astype(f32)) * jax.nn.softplus(decay_lin.astype(f32) + dt_bias.astype(f32))
    oa = gated_delta_rule_chunked(qa, ka, va, log_decay, beta)
    oa = rms_norm(oa, gdn_norm_g) * jax.nn.silu(za.reshape(b, s, GDN_HEADS, GDN_DV).astype(f32))
    ya = jnp.einsum("bse,ed->bsd", oa.reshape(b, s, GDN_V).astype(h.dtype), w_o_gdn)

    qb = rms_norm(qb.reshape(b, s, DIFF_HEADS, 2, DIFF_DH), q_norm_g)
    kb = rms_norm(kb.reshape(b, s, DIFF_HEADS, 2, DIFF_DH), k_norm_g)
    vb = vb.reshape(b, s, DIFF_HEADS, 2 * DIFF_DH)
    lam = (jnp.exp(jnp.sum(lambda_q1.astype(f32) * lambda_k1.astype(f32)))
           - jnp.exp(jnp.sum(lambda_q2.astype(f32) * lambda_k2.astype(f32))) + lam_init)
    ob = differential_attention(qb, kb, vb, lam)
    ob = rms_norm(ob, subln_g) * (1.0 - lam_init)
    yb = jnp.einsum("bse,ed->bsd", ob.reshape(b, s, DIFF_V), w_o_diff)

    merged = jax.nn.sigmoid(gate_a) * ya + jax.nn.sigmoid(gate_b) * yb
    return jnp.einsum("bsd,de->bse", merged, w_out)


def moe_ffn(h, w_router, router_bias, w_gate_up, w_down, ws_gate_up, ws_down):
    b, s, d = h.shape
    t = b * s
    f32 = jnp.float32
    hf = h.reshape(t, d)
    scores = jax.nn.sigmoid(jnp.einsum("td,de->te", hf, w_router, preferred_element_type=f32))
    choice = scores + router_bias.astype(f32)
    per_group = N_EXPERTS // N_GROUPS
    group_score = lax.top_k(choice.reshape(t, N_GROUPS, per_group), 2)[0].sum(-1)
    _, top_groups = lax.top_k(group_score, TOPK_GROUPS)
    group_mask = jnp.any(top_groups[..., None] == jnp.arange(N_GROUPS), axis=-2)
    expert_mask = jnp.repeat(group_mask, per_group, axis=-1)
    _, top_idx = lax.top_k(jnp.where(expert_mask, choice, -jnp.inf), TOP_K)
    top_w = jnp.take_along_axis(scores, top_idx, axis=-1)
    top_w = top_w / jnp.sum(top_w, axis=-1, keepdims=True) * ROUTED_SCALE

    n_assign = t * TOP_K
    flat_e = top_idx.reshape(-1).astype(jnp.int32)
    flat_tok = jnp.repeat(jnp.arange(t, dtype=jnp.int32), TOP_K)
    flat_w = top_w.reshape(-1)
    order = jnp.argsort(flat_e)
    sorted_e = flat_e[order]
    counts = jax.ops.segment_sum(jnp.ones_like(flat_e), flat_e, num_segments=N_EXPERTS)
    padded = (counts + EXPERT_BLOCK - 1) // EXPERT_BLOCK * EXPERT_BLOCK
    starts = jnp.cumsum(counts) - counts
    padded_ends = jnp.cumsum(padded)
    padded_starts = padded_ends - padded
    dest = padded_starts[sorted_e] + jnp.arange(n_assign, dtype=jnp.int32) - starts[sorted_e]
    n_blocks = -(-n_assign // EXPERT_BLOCK) + N_EXPERTS
    rows = n_blocks * EXPERT_BLOCK
    row_tok = jnp.zeros((rows,), jnp.int32).at[dest].set(flat_tok[order])
    row_w = jnp.zeros((rows,), f32).at[dest].set(flat_w[order])
    block_e = jnp.minimum(
        jnp.searchsorted(padded_ends, jnp.arange(n_blocks, dtype=jnp.int32) * EXPERT_BLOCK, side="right"),
        N_EXPERTS - 1)

    def expert_block(acc, blk):
        tok, wt, e = blk
        xb = hf[tok]
        gate, up = jnp.split(xb @ w_gate_up[e], 2, axis=-1)
        yb = (jax.nn.silu(gate) * up) @ w_down[e]
        return acc.at[tok].add(yb.astype(f32) * wt[:, None]), None

    routed, _ = lax.scan(expert_block, jnp.zeros((t, d), f32),
                         (row_tok.reshape(n_blocks, EXPERT_BLOCK), row_w.reshape(n_blocks, EXPERT_BLOCK), block_e))
    sg, su = jnp.split(hf @ ws_gate_up, 2, axis=-1)
    shared = (jax.nn.silu(sg) * su) @ ws_down
    return (routed + shared.astype(f32)).astype(h.dtype).reshape(b, s, d)


def setup_inputs(seed: int = 0) -> dict:
    key = jax.random.key(seed)
    ks = jax.random.split(key, 32)
    f32 = jnp.float32
    L = DEPTH

    def normal(k, shape, scale):
        return jax.random.normal(k, shape, f32) * scale

    def gain(k, shape):
        return 1.0 + 0.01 * jax.random.normal(k, shape, f32)

    dt = jnp.exp(jax.random.uniform(ks[8], (L, GDN_HEADS), f32, minval=math.log(1e-3), maxval=math.log(1e-1)))
    return {
        "x": normal(ks[0], (BATCH, SEQ, D_MODEL), 1.0),
        "c": normal(ks[1], (BATCH, D_MODEL), 1.0),
        "w_ada": normal(ks[2], (L, D_MODEL, 6 * D_MODEL), 0.5 * D_MODEL ** -0.5),
        "b_ada": normal(ks[3], (L, 6 * D_MODEL), 0.01),
        "norm1_g": gain(ks[4], (L, D_MODEL)),
        "w_in": normal(ks[5], (L, D_MODEL, IN_COLS), D_MODEL ** -0.5),
        "conv_w": normal(ks[6], (L, GDN_CONV, CONV_CH), GDN_CONV ** -0.5),
        "a_log": jnp.log(jax.random.uniform(ks[7], (L, GDN_HEADS), f32, minval=1.0, maxval=16.0)),
        "dt_bias": dt + jnp.log(-jnp.expm1(-dt)),
        "gdn_norm_g": gain(ks[9], (L, GDN_DV)),
        "w_o_gdn": normal(ks[10], (L, GDN_V, D_MODEL), GDN_V ** -0.5),
        "q_norm_g": gain(ks[11], (L, DIFF_DH)),
        "k_norm_g": gain(ks[12], (L, DIFF_DH)),
        "lambda_q1": normal(ks[13], (L, DIFF_DH), 0.1),
        "lambda_k1": normal(ks[14], (L, DIFF_DH), 0.1),
        "lambda_q2": normal(ks[15], (L, DIFF_DH), 0.1),
        "lambda_k2": normal(ks[16], (L, DIFF_DH), 0.1),
        "subln_g": gain(ks[17], (L, 2 * DIFF_DH)),
        "w_o_diff": normal(ks[18], (L, DIFF_V, D_MODEL), DIFF_V ** -0.5),
        "w_out": normal(ks[19], (L, D_MODEL, D_MODEL), D_MODEL ** -0.5),
        "norm2_g": gain(ks[20], (L, D_MODEL)),
        "w_router": normal(ks[21], (L, D_MODEL, N_EXPERTS), D_MODEL ** -0.5),
        "router_bias": normal(ks[22], (L, N_EXPERTS), 0.01),
        "w_exp_gate_up": normal(ks[23], (L, N_EXPERTS, D_MODEL, 2 * EXPERT_FF), D_MODEL ** -0.5),
        "w_exp_down": normal(ks[24], (L, N_EXPERTS, EXPERT_FF, D_MODEL), EXPERT_FF ** -0.5),
        "w_shared_gate_up": normal(ks[25], (L, D_MODEL, 2 * SHARED_FF), D_MODEL ** -0.5),
        "w_shared_down": normal(ks[26], (L, SHARED_FF, D_MODEL), SHARED_FF ** -0.5),
    }


def reference(x, c, w_ada, b_ada, norm1_g, w_in, conv_w, a_log, dt_bias, gdn_norm_g, w_o_gdn,
              q_norm_g, k_norm_g, lambda_q1, lambda_k1, lambda_q2, lambda_k2, subln_g, w_o_diff,
              w_out, norm2_g, w_router, router_bias, w_exp_gate_up, w_exp_down,
              w_shared_gate_up, w_shared_down):
    for layer in range(DEPTH):
        mod = jnp.einsum("bd,de->be", jax.nn.silu(c), w_ada[layer]) + b_ada[layer]
        shift1, scale1, gate1, shift2, scale2, gate2 = jnp.split(mod[:, None, :], 6, axis=-1)
        lam_init = 0.8 - 0.6 * math.exp(-0.3 * layer)

        h = rms_norm(x, norm1_g[layer]) * (1.0 + scale1) + shift1
        y = hybrid_mixer(h, w_in[layer], conv_w[layer], a_log[layer], dt_bias[layer], gdn_norm_g[layer],
                         w_o_gdn[layer], q_norm_g[layer], k_norm_g[layer], lambda_q1[layer], lambda_k1[layer],
                         lambda_q2[layer], lambda_k2[layer], subln_g[layer], w_o_diff[layer], w_out[layer],
                         lam_init)
        x = x + gate1 * y

        h = rms_norm(x, norm2_g[layer]) * (1.0 + scale2) + shift2
        y = moe_ffn(h, w_router[layer], router_bias[layer], w_exp_gate_up[layer], w_exp_down[layer],
                    w_shared_gate_up[layer], w_shared_down[layer])
        x = x + gate2 * y
    return x
```

```python
import contextlib
import numpy as np
import concourse.bass as bass
import concourse.mybir as mybir
from concourse.bass_utils import run_bass_kernel_spmd

F32 = mybir.dt.float32
BF16 = mybir.dt.bfloat16
I32 = mybir.dt.int32
AF = mybir.ActivationFunctionType
ALU = mybir.AluOpType


class Buf:
    __slots__ = ("name", "w", "r")

    def __init__(self, name):
        self.name = name
        self.w = None
        self.r = []


class Tn:
    def __init__(self, t, name):
        self.t = t
        self.name = name
        self.b = Buf(name)
        self.subs = {}

    def sub(self, key):
        if key not in self.subs:
            self.subs[key] = Buf(f"{self.name}:{key}")
        return self.subs[key]


class Op:
    __slots__ = ("eng", "fn", "deps", "is_dma", "sem", "val", "signals", "final", "inc")

    def __init__(self, eng, fn, is_dma=False):
        self.eng = eng
        self.fn = fn
        self.deps = []
        self.is_dma = is_dma
        self.sem = None
        self.val = None
        self.signals = False
        self.final = False
        self.inc = 1


def _buf(x):
    return x.b if hasattr(x, 'b') else x


class Sched:
    ENGS = ("pe", "act", "dve", "pool", "sp")

    def __init__(self, nc, n_dma_sems=None):
        self.nc = nc
        self.stack = contextlib.ExitStack()
        self.stacks = [self.stack]
        self.ops = {e: [] for e in self.ENGS}
        self.esem = {e: nc.alloc_semaphore(name=f"sem_{e}") for e in self.ENGS}
        nd = n_dma_sems or {"sp": 24, "pool": 12, "act": 4}
        self.dsem = {q: [nc.alloc_semaphore(name=f"dsem_{q}{i}") for i in range(n)] for q, n in nd.items()}
        self.dlast = {q: [None] * n for q, n in nd.items()}
        self.dcnt = {q: [0] * n for q, n in nd.items()}
        self.dnext = {q: 0 for q in nd}
        self.n_ops = 0

    def sb(self, name, shape, dtype=F32):
        t = self.stacks[-1].enter_context(self.nc.sbuf_tensor(name, list(shape), dtype))
        return Tn(t, name)

    @contextlib.contextmanager
    def scope(self):
        st = contextlib.ExitStack()
        self.stacks.append(st)
        try:
            yield
        finally:
            self.barrier()
            self.stacks.pop()
            st.close()

    def barrier(self):
        lasts = []
        for e in self.ENGS:
            for o in reversed(self.ops[e]):
                if o.fn is not None and not o.is_dma:
                    lasts.append(o)
                    break
        for q in self.dlast:
            lasts += [o for o in self.dlast[q] if o is not None]
        for e in self.ENGS:
            o = Op(e, None)
            o.sem = self.esem[e]
            o.deps = list(lasts)
            self.ops[e].append(o)

    def ps(self, name, shape, dtype=F32):
        t = self.stack.enter_context(self.nc.psum_tensor(name, list(shape), dtype))
        return Tn(t, name)

    def dram(self, ap, name):
        return Tn(ap, name)

    def scratch(self, name, shape, dtype=F32):
        t = self.nc.dram_tensor(name, list(shape), dtype, kind="Internal")
        return Tn(t.ap(), name)

    def _track(self, op, R, W):
        eng = op.eng
        deps = []

        def add(d, kind):
            if d is None:
                return
            if not d.is_dma and not op.is_dma and d.eng == eng:
                if eng == "pe":
                    return
            deps.append(d)

        Rb = [_buf(x) for x in R]
        Wb = [_buf(x) for x in W]
        for b in Rb:
            add(b.w, "raw")
        for b in Wb:
            add(b.w, "waw")
            for r in b.r:
                add(r, "war")
        for b in Rb:
            b.r.append(op)
        for b in Wb:
            b.w = op
            b.r = []
        seen = set()
        for d in deps:
            if id(d) not in seen and d is not op:
                seen.add(id(d))
                op.deps.append(d)

    def op(self, eng, fn, R=(), W=()):
        o = Op(eng, fn)
        o.sem = self.esem[eng]
        self._track(o, R, W)
        self.ops[eng].append(o)
        self.n_ops += 1
        return o

    def dma(self, q, out, in_, R=(), W=(), final=False, fn=None, **kw):
        if fn is None:
            fn = lambda e: e.dma_start(out=out, in_=in_, **kw)
        o = Op(q, fn, is_dma=True)
        o.inc = 16
        k = self.dnext[q]
        self.dnext[q] = (k + 1) % len(self.dsem[q])
        o.sem = self.dsem[q][k]
        self.dcnt[q][k] += 16
        o.val = self.dcnt[q][k]
        o.signals = True
        o.final = final
        self._track(o, R, W)
        prev = self.dlast[q][k]
        if prev is not None and prev not in o.deps:
            o.deps.append(prev)
        self.dlast[q][k] = o
        self.ops[q].append(o)
        self.n_ops += 1
        return o

    def cc(self, fn, R=(), W=()):
        if "cc" not in self.dsem:
            self.dsem["cc"] = [self.nc.alloc_semaphore(name="dsem_cc")]
            self.dlast["cc"] = [None]
            self.dcnt["cc"] = [0]
        o = Op("pool", fn, is_dma=True)
        o.inc = 1
        o.sem = self.dsem["cc"][0]
        self.dcnt["cc"][0] += 1
        o.val = self.dcnt["cc"][0]
        o.signals = True
        self._track(o, R, W)
        prev = self.dlast["cc"][0]
        if prev is not None and prev not in o.deps:
            o.deps.append(prev)
        self.dlast["cc"][0] = o
        self.ops["pool"].append(o)
        return o

    def emit(self):
        for e in self.ENGS:
            for o in self.ops[e]:
                for d in o.deps:
                    d.signals = True
        for e in self.ENGS:
            c = 0
            for o in self.ops[e]:
                if o.fn is None:
                    o.signals = False
                    o.val = c
                    continue
                if not o.is_dma and o.signals:
                    c += 1
                    o.val = c
        nc = self.nc

        def replay(ename):
            def run(e):
                waited = {}
                finals = []
                for o in self.ops[ename]:
                    need = {}
                    for d in o.deps:
                        k = id(d.sem)
                        if k not in need or need[k][1] < d.val:
                            need[k] = (d.sem, d.val)
                    for k, (sem, val) in need.items():
                        if waited.get(k, 0) >= val:
                            continue
                        e.wait_ge(sem, val)
                        waited[k] = val
                    if o.fn is None:
                        continue
                    ins = o.fn(e)
                    if o.signals:
                        ins.then_inc(o.sem, o.inc)
                    if o.final:
                        finals.append(o)
                for o in finals:
                    e.wait_ge(o.sem, o.val)
            return run

        with nc.Block() as block:
            block.sync(replay("sp"))
            block.tensor(replay("pe"))
            block.scalar(replay("act"))
            block.vector(replay("dve"))
            block.gpsimd(replay("pool"))
        self.stack.close()


AX = mybir.AxisListType
D = 1024
KC = 8
EPS = 1e-6
LAM_INIT = 0.2
W_OFF = dict(qA=0, kA=512, vA=1024, zA=1536, beta=2048, decay=2052, qB=2056, kB=2568, vB=3080, ga=3592, gb=4616)


class Bld:
    def __init__(self, nc):
        self.nc = nc
        self.S = Sched(nc)
        self.banks = [self.S.ps(f"bank{i}", [128, 512]) for i in range(8)]
        self.bi = 0
        self.ins = {}

    def inp(self, name, shape, dtype=F32):
        ap = self.nc.dram_tensor(name, list(shape), dtype, kind="ExternalInput").ap()
        t = self.S.dram(ap, name)
        self.ins[name] = t
        return t

    def outp(self, name, shape, dtype=F32):
        ap = self.nc.dram_tensor(name, list(shape), dtype, kind="ExternalOutput").ap()
        return self.S.dram(ap, name)

    def bank(self):
        b = self.banks[self.bi % 8]
        self.bi += 1
        return b

    def mm(self, bank, out, lhsT, rhs, R, start=True, stop=True):
        self.S.op("pe", lambda e: e.matmul(out, lhsT, rhs, start=start, stop=stop), R=R, W=[bank])

    def tp(self, bank, out, in_, ident, R):
        self.S.op("pe", lambda e: e.transpose(out, in_, ident), R=R, W=[bank])

    def act(self, out, in_, func, R, W, **kw):
        self.S.op("act", lambda e: e.activation(out=out, in_=in_, func=func, **kw), R=R, W=W)

    def ts(self, out, in0, s1, s2, op0, op1, R, W, eng="dve"):
        if op1 is None:
            self.S.op(eng, lambda e: e.tensor_scalar(out=out, in0=in0, scalar1=s1, scalar2=None, op0=op0), R=R, W=W)
        else:
            self.S.op(eng, lambda e: e.tensor_scalar(out=out, in0=in0, scalar1=s1, scalar2=s2, op0=op0, op1=op1), R=R, W=W)

    def tt(self, out, in0, in1, op, R, W, eng="dve"):
        self.S.op(eng, lambda e: e.tensor_tensor(out=out, in0=in0, in1=in1, op=op), R=R, W=W)

    def stt(self, out, in0, scalar, in1, op0, op1, R, W, eng="dve"):
        self.S.op(eng, lambda e: e.scalar_tensor_tensor(out=out, in0=in0, scalar=scalar, in1=in1, op0=op0, op1=op1), R=R, W=W)

    def cp(self, out, in_, R, W, eng="dve"):
        self.S.op(eng, lambda e: e.tensor_copy(out=out, in_=in_), R=R, W=W)

    def memset(self, ap, val, W, eng="dve"):
        self.S.op(eng, lambda e: e.memset(ap, val), R=[], W=W)

    def dma(self, out, in_, R, W, q="sp", final=False):
        self.S.dma(q, out, in_, R=R, W=W, final=final)

    def rstd(self, out, in_, R, W, scale, eps=EPS, lnbias=0.0):
        self.act(out, in_, AF.Ln, R=R, W=W, scale=scale, bias=eps)
        self.act(out, out, AF.Exp, R=W, W=W, scale=-0.5, bias=lnbias)


def build_consts(b, slope=None):
    S = b.S
    c = {}
    cin = b.inp("consts", [128, 128 * 6])
    ct = S.sb("consts_sb", [128, 128 * 6])
    b.dma(ct.t[:], cin.t[:, :], R=[cin], W=[ct])
    c["t"] = ct
    c["ident"] = ct.t[:, 0:128]
    c["ones"] = ct.t[:, 128:256]
    c["bd"] = ct.t[:, 256:384]
    c["UT"] = ct.t[0:64, 384:448]
    c["LTs"] = ct.t[0:64, 448:512]
    c["Cm"] = ct.t[:, 512:640]
    c["brel"] = ct.t[:, 640:704]
    return c


def host_consts(slope):
    c = np.zeros((128, 768), np.float32)
    c[:, 0:128] = np.eye(128)
    c[:, 128:256] = 1.0
    c[0:64, 256:320] = 1.0
    c[64:128, 320:384] = 1.0
    p = np.arange(64)[:, None]
    f = np.arange(64)[None, :]
    c[0:64, 384:448] = (p <= f)
    c[0:64, 448:512] = (p > f)
    k = np.arange(128)[:, None]
    q = np.arange(128)[None, :]
    cm = np.where(k <= q, 1.0, np.where((k // 64) == (q // 64), np.exp(-2.0 * slope * (k - q)), 0.0))
    c[:, 512:640] = cm
    r = np.arange(64)[None, :]
    c[:, 640:704] = slope * (k + 128.0 * (r - 63) - 127.0)
    return c


def emit_adaln(b, c, want):
    S = b.S
    ccol = b.inp("c_col", [128, 8])
    wada = b.inp("w_ada", [1024, 6144])
    bada = b.inp("b_ada", [1, 6144])
    g1c = b.inp("g1c", [128, 8])
    g2c = b.inp("g2c", [128, 8])
    modc = S.sb("modc", [128, 48])
    ab = S.sb("ab", [128, 32])
    out = {"modc": modc, "ab": ab}
    if want == "gates":
        out["g1bc"] = S.sb("g1bc", [128, 1024])
        out["g2bc"] = S.sb("g2bc", [128, 1024])
    with S.scope():
        _emit_adaln_body(b, c, want, out, ccol, wada, bada, g1c, g2c)
    return out


def _emit_adaln_body(b, c, want, out, ccol, wada, bada, g1c, g2c):
    S = b.S
    modc = out["modc"]
    ab = out["ab"]
    sc = S.sb("sc", [128, 8])
    gt = S.sb("gcols", [128, 16])
    brow = S.sb("brow", [1, 6144])
    modrow = S.sb("modrow", [1, 6144])
    wb = [S.sb(f"wadab{i}", [128, 8, 512]) for i in range(2)]
    b.dma(sc.t[:], ccol.t[:, :], R=[ccol], W=[sc])
    b.dma(gt.t[:, 0:8], g1c.t[:, :], R=[g1c], W=[gt])
    b.dma(gt.t[:, 8:16], g2c.t[:, :], R=[g2c], W=[gt])
    b.dma(brow.t[:], bada.t[:, :], R=[bada], W=[brow])
    b.act(sc.t[:], sc.t[:], AF.Silu, R=[sc], W=[sc])
    for jb in range(12):
        w = wb[jb % 2]
        b.dma(w.t[:], wada.t[:, jb * 512:(jb + 1) * 512].rearrange("(k p) c -> p k c", p=128), R=[wada], W=[w])
        bk = b.bank()
        for k in range(8):
            b.mm(bk, bk.t[0:1, 0:512], sc.t[:, k:k + 1], w.t[:, k, :], R=[sc, w], start=(k == 0), stop=(k == 7))
        b.tt(modrow.t[0:1, jb * 512:(jb + 1) * 512], bk.t[0:1, 0:512], brow.t[0:1, jb * 512:(jb + 1) * 512], ALU.add,
             R=[bk, brow], W=[modrow])
    bk = b.bank()
    for j in range(48):
        b.mm(bk, bk.t[:, j:j + 1], modrow.t[0:1, j * 128:(j + 1) * 128], c["ones"][0:1, 0:1], R=[modrow, c["t"]])
    b.cp(modc.t[:], bk.t[:, 0:48], R=[bk], W=[modc])
    b.ts(ab.t[:, 0:8], modc.t[:, 8:16], 1.0, None, ALU.add, None, R=[modc], W=[ab])
    b.tt(ab.t[:, 0:8], ab.t[:, 0:8], gt.t[:, 0:8], ALU.mult, R=[ab, gt], W=[ab])
    b.cp(ab.t[:, 8:16], modc.t[:, 0:8], R=[modc], W=[ab])
    b.ts(ab.t[:, 16:24], modc.t[:, 32:40], 1.0, None, ALU.add, None, R=[modc], W=[ab])
    b.tt(ab.t[:, 16:24], ab.t[:, 16:24], gt.t[:, 8:16], ALU.mult, R=[ab, gt], W=[ab])
    b.cp(ab.t[:, 24:32], modc.t[:, 24:32], R=[modc], W=[ab])
    if want == "gates":
        g1bc = out["g1bc"]
        g2bc = out["g2bc"]
        for (dst, off) in ((g1bc, 2048), (g2bc, 5120)):
            for hf in range(2):
                bk = b.bank()
                b.mm(bk, bk.t[:, 0:512], c["ones"][0:1, 0:128], modrow.t[0:1, off + hf * 512: off + (hf + 1) * 512],
                     R=[modrow, c["t"]])
                b.cp(dst.t[:, hf * 512:(hf + 1) * 512], bk.t[:, 0:512], R=[bk], W=[dst])
    return out


def emit_norm_hT(b, c, xsrc, row0, ntiles, acol, bcol, hT, xt_bufs, ssq, hT_dtype_ap=None, x_keep=None):
    S = b.S
    for tt in range(ntiles):
        xt = xt_bufs[tt % len(xt_bufs)] if x_keep is None else x_keep[tt]
        b.dma(xt.t[:], xsrc.t[row0 + tt * 128: row0 + (tt + 1) * 128, :], R=[xsrc], W=[xt])
    return


def norm_tile(b, c, xt, xn, ss, acol, bcol, hT, col0, scratch):
    b.act(scratch.t[:], xt.t[:], AF.Square, R=[xt], W=[scratch, ss], accum_out=ss.t[:, 0:1])
    b.rstd(ss.t[:, 0:1], ss.t[:, 0:1], R=[ss], W=[ss], scale=1.0 / D)
    b.ts(xn.t[:], xt.t[:], ss.t[:, 0:1], None, ALU.mult, None, R=[xt, ss], W=[xn])
    for k2 in range(2):
        bk = b.bank()
        for kk in range(4):
            k = k2 * 4 + kk
            b.tp(bk, bk.t[:, kk * 128:(kk + 1) * 128], xn.t[:, k * 128:(k + 1) * 128], c["ident"], R=[xn, c["t"]])
        for kk in range(4):
            k = k2 * 4 + kk
            b.act(hT[:, k, col0:col0 + 128], bk.t[:, kk * 128:(kk + 1) * 128], AF.Identity, R=[bk, acol[1]], W=[acol[2]],
                  scale=acol[0][:, k:k + 1], bias=bcol[0][:, k:k + 1])


def norm_tile2(b, c, xt, xn, ss, scratch, ab, aoff, hT, hTt, col0, xap=None, cols=None):
    if xap is None:
        xap = xt.t[:]
    b.act(scratch.t[:], xap, AF.Square, R=[xt], W=[scratch, ss], accum_out=ss.t[:, 0:1])
    b.rstd(ss.t[:, 0:1], ss.t[:, 0:1], R=[ss], W=[ss], scale=1.0 / D)
    b.ts(xn.t[:], xap, ss.t[:, 0:1], None, ALU.mult, None, R=[xt, ss], W=[xn])
    for k2 in range(2):
        bk = b.bank()
        for kk in range(4):
            k = k2 * 4 + kk
            b.tp(bk, bk.t[:, kk * 128:(kk + 1) * 128], xn.t[:, k * 128:(k + 1) * 128], c["ident"], R=[xn, c["t"]])
        for kk in range(4):
            k = k2 * 4 + kk
            for di, (dst, dstT) in enumerate(zip(hT, hTt)):
                cc0 = col0 if cols is None else cols[di]
                b.act(dst[:, k, cc0:cc0 + 128], bk.t[:, kk * 128:(kk + 1) * 128], AF.Identity, R=[bk, ab], W=[dstT],
                      scale=ab.t[:, aoff + k:aoff + k + 1], bias=ab.t[:, aoff + 8 + k:aoff + 9 + k])


def build_ab(Sq):
    nc = bass.Bass("TRN2", target_bir_lowering=False)
    b = Bld(nc)
    c = build_consts(b)
    ad = emit_adaln(b, c, want="none")
    send = b.outp("send", [Sq, 256])
    emit_ab(b, c, ad["ab"], Sq, lambda r0, n, c0, key: (send.t[r0:r0 + n, c0:c0 + 128], send.sub(key)))
    b.S.emit()
    return nc


def emit_ab(b, c, ab, Sq, send_at, after_group=None):
    with b.S.scope():
        _emit_ab_body(b, c, ab, Sq, send_at, after_group)


def _emit_ab_body(b, c, ab, Sq, send_at, after_group):
    S = b.S
    NG = Sq // 512
    xb = b.inp("xb", [Sq, 1024])
    wcm_d = b.inp("w_cm", [1024, 640])
    wzbd_d = b.inp("w_zbd", [1024, 130])
    wvb_d = b.inp("w_vb", [1024, 128])
    small_d = b.inp("small", [128, 384])
    lam_d = b.inp("lamrow", [1, 256])

    wcm = S.sb("wcm", [128, 8, 640])
    wzbd = S.sb("wzbd", [128, 8, 130])
    wvb = S.sb("wvb", [128, 8, 128])
    small = S.sb("small_sb", [128, 384])
    lamr = S.sb("lamr", [1, 256])
    b.dma(wcm.t[:], wcm_d.t[:, :].rearrange("(k p) c -> p k c", p=128), R=[wcm_d], W=[wcm])
    b.dma(wzbd.t[:], wzbd_d.t[:, :].rearrange("(k p) c -> p k c", p=128), R=[wzbd_d], W=[wzbd])
    b.dma(wvb.t[:], wvb_d.t[:, :].rearrange("(k p) c -> p k c", p=128), R=[wvb_d], W=[wvb])
    b.dma(small.t[:], small_d.t[:, :], R=[small_d], W=[small])
    b.dma(lamr.t[:], lam_d.t[:, :], R=[lam_d], W=[lamr])
    convw = lambda j, tap: small.t[:, j * 4 + tap: j * 4 + tap + 1]
    gng = small.t[0:64, 128:256]
    sub_g = S.sb("sub_g", [128, 128])
    b.ts(sub_g.t[:], small.t[:, 256:384], 1.0 - LAM_INIT, None, ALU.mult, None, R=[small], W=[sub_g])
    negA = S.sb("negA", [128, 1])
    b.act(negA.t[:], small.t[:, 12:13], AF.Exp, R=[small], W=[negA])
    b.ts(negA.t[:], negA.t[:], -1.0, None, ALU.mult, None, R=[negA], W=[negA])
    lt = S.sb("lam_t", [1, 136])
    b.tt(lt.t[0:1, 0:64], lamr.t[0:1, 0:64], lamr.t[0:1, 64:128], ALU.mult, R=[lamr], W=[lt])
    b.tt(lt.t[0:1, 64:128], lamr.t[0:1, 128:192], lamr.t[0:1, 192:256], ALU.mult, R=[lamr], W=[lt])
    b.S.op("dve", lambda e: e.tensor_reduce(out=lt.t[0:1, 128:129], in_=lt.t[0:1, 0:64], axis=AX.X, op=ALU.add), R=[lt], W=[lt])
    b.S.op("dve", lambda e: e.tensor_reduce(out=lt.t[0:1, 129:130], in_=lt.t[0:1, 64:128], axis=AX.X, op=ALU.add), R=[lt], W=[lt])
    b.act(lt.t[0:1, 128:130], lt.t[0:1, 128:130], AF.Exp, R=[lt], W=[lt])
    b.tt(lt.t[0:1, 130:131], lt.t[0:1, 129:130], lt.t[0:1, 128:129], ALU.subtract, R=[lt], W=[lt])
    b.ts(lt.t[0:1, 130:131], lt.t[0:1, 130:131], -LAM_INIT, None, ALU.add, None, R=[lt], W=[lt])
    neglam = S.sb("neglam", [128, 1])
    bk = b.bank()
    b.mm(bk, bk.t[:, 0:1], c["ones"][0:1, 0:128], lt.t[0:1, 130:131], R=[lt, c["t"]])
    b.cp(neglam.t[:], bk.t[:, 0:1], R=[bk], W=[neglam])

    qT = S.sb("qT_all", [128, Sq])
    kT = S.sb("kT_all", [128, Sq])
    Vx = S.sb("Vext", [128, Sq // 128, 130])
    b.memset(Vx.t[:, :, 128:130], 1.0, W=[Vx])
    hT = [S.sb(f"hT{i}", [128, 8, 512]) for i in range(1)]
    xt = [S.sb(f"xt{i}", [128, 1024]) for i in range(1)]
    xn = S.sb("xn", [128, 1024])
    scr = xn
    ssx = S.sb("ssx", [128, 1])
    pre = [S.sb(f"pre{j}", [128, 515]) for j in range(3)]
    for j in range(3):
        b.memset(pre[j].t[:, 0:3], 0.0, W=[pre[j]])
    cv = [S.sb(f"cv{j}", [128, 512]) for j in range(3)]
    sq = S.sb("sq", [128, 512])
    rs = S.sb("rs", [128, 512])
    qk_tmp = S.sb("qk_tmp", [128, 512])
    St = S.sb("state", [128, 128])
    b.memset(St.t[:], 0.0, W=[St])
    def cb(name, shape):
        return [S.sb(f"{name}{i}", shape) for i in range(2)]
    zb = cb("zb", [64, 132]); gcol = cb("gcol", [64, 8]); e1 = cb("e1", [64, 64]); dts = cb("dts", [64, 64])
    dcs = cb("dcs", [64, 64]); Mb = [cb("Ma", [64, 64]), cb("Mb", [64, 64])]; Nb = [cb("Na", [64, 64]), cb("Nb", [64, 64])]
    Tb = [cb("Ta", [64, 64]), cb("Tb", [64, 64])]; ktm = cb("ktm", [64, 128]); kbt = cb("kbt", [64, 128])
    vbt = cb("vbt", [64, 128]); kdec = cb("kdec", [64, 128]); Ub = cb("Ub", [64, 128]); WTb = cb("WTb", [128, 64])
    AqT = cb("AqT", [64, 64]); gl = cb("gl", [128, 2]); vnew = cb("vnew", [64, 128]); o1 = cb("o1", [64, 128])
    ob_ = cb("ob_", [64, 128]); szb = cb("szb", [64, 128]); tmp64 = cb("tmp64", [64, 64]); oast = cb("oast", [64, 128])
    ssg = cb("ssg", [64, 2])
    PT = [S.sb(f"PT{i}", [128, 512]) for i in range(2)]
    obacc = [S.sb(f"obacc{i}", [128, 128]) for i in range(4)]
    rz = S.sb("rz", [128, 8])
    obst = [S.sb(f"obst{i}", [128, 128]) for i in range(2)]
    sso = S.sb("sso", [128, 2])
    ident = c["ident"]; ct = c["t"]
    SCALE = 64 ** -0.5

    for g in range(NG):
        h = hT[0]
        for tt in range(4):
            x_ = xt[0]
            b.dma(x_.t[:], xb.t[g * 512 + tt * 128: g * 512 + (tt + 1) * 128, :], R=[xb], W=[x_])
            norm_tile2(b, c, x_, xn, ssx, scr, ab, 0, [h.t], [h], tt * 128)
        for j in range(5):
            bk = b.bank()
            for k in range(8):
                b.mm(bk, bk.t[:, 0:512], wcm.t[:, k, j * 128:(j + 1) * 128], h.t[:, k, :], R=[wcm, h], start=(k == 0), stop=(k == 7))
            if j < 3:
                b.act(pre[j].t[:, 3:515], bk.t[:, 0:512], AF.Copy, R=[bk], W=[pre[j]])
                y = cv[j]
                b.ts(y.t[:], pre[j].t[:, 0:512], convw(j, 0), None, ALU.mult, None, R=[pre[j], small], W=[y])
                for tap in range(1, 4):
                    b.stt(y.t[:], pre[j].t[:, tap:tap + 512], convw(j, tap), y.t[:], ALU.mult, ALU.add, R=[pre[j], small, y], W=[y])
                b.cp(pre[j].t[:, 0:3], pre[j].t[:, 512:515], R=[pre[j]], W=[pre[j]])
                b.act(y.t[:], y.t[:], AF.Silu, R=[y], W=[y])
                if j < 2:
                    b.tt(sq.t[:], y.t[:], y.t[:], ALU.mult, R=[y], W=[sq])
                    bk2 = b.bank()
                    b.mm(bk2, bk2.t[:, 0:512], c["ones"], sq.t[:], R=[sq, ct])
                    b.act(rs.t[:], bk2.t[:, 0:512], AF.Ln, R=[bk2], W=[rs], bias=EPS)
                    b.act(rs.t[:], rs.t[:], AF.Exp, R=[rs], W=[rs], scale=-0.5, bias=(float(np.log(128 ** -0.5)) if j == 0 else 0.0))
                    b.tt(y.t[:], y.t[:], rs.t[:], ALU.mult, R=[y, rs], W=[y])
            else:
                dst = qT if j == 3 else kT
                gcolm = small.t[:, 14:15] if j == 3 else small.t[:, 15:16]
                b.act(qk_tmp.t[:], bk.t[:, 0:512], AF.Copy, R=[bk], W=[qk_tmp])
                b.tt(sq.t[:], qk_tmp.t[:], qk_tmp.t[:], ALU.mult, R=[qk_tmp], W=[sq])
                bk2 = b.bank()
                b.mm(bk2, bk2.t[:, 0:512], c["bd"], sq.t[:], R=[sq, ct])
                b.act(rs.t[:], bk2.t[:, 0:512], AF.Ln, R=[bk2], W=[rs], scale=1.0 / 64, bias=EPS)
                b.act(rs.t[:], rs.t[:], AF.Exp, R=[rs], W=[rs], scale=-0.5)
                b.stt(dst.t[:, g * 512:(g + 1) * 512], qk_tmp.t[:], gcolm, rs.t[:], ALU.mult, ALU.mult, R=[qk_tmp, small, rs], W=[dst.sub(g)])
        for tt in range(4):
            bk = b.bank()
            for k in range(8):
                b.mm(bk, bk.t[:, 0:128], h.t[:, k, tt * 128:(tt + 1) * 128], wvb.t[:, k, :], R=[wvb, h], start=(k == 0), stop=(k == 7))
            b.cp(Vx.t[:, g * 4 + tt, 0:128], bk.t[:, 0:128], R=[bk], W=[Vx.sub(g)])
        qn, kn, vs = cv[0], cv[1], cv[2]
        for ch in range(8):
            p = ch % 2
            t0 = ch * 64
            kTc = kn.t[:, t0:t0 + 64]; qTc = qn.t[:, t0:t0 + 64]
            bk = b.bank()
            for k in range(8):
                b.mm(bk, bk.t[0:64, 0:130], h.t[:, k, t0:t0 + 64], wzbd.t[:, k, :], R=[wzbd, h], start=(k == 0), stop=(k == 7))
            z = zb[p]
            b.cp(z.t[:, 0:130], bk.t[0:64, 0:130], R=[bk], W=[z])
            gc = gcol[p]
            b.act(gc.t[:, 0:1], z.t[:, 128:129], AF.Sigmoid, R=[z], W=[gc])
            b.act(gc.t[:, 1:2], z.t[:, 129:130], AF.Exp, R=[z, small], W=[gc], bias=small.t[0:64, 13:14])
            b.act(gc.t[:, 1:2], gc.t[:, 1:2], AF.Ln, R=[gc], W=[gc], bias=1.0)
            b.tt(gc.t[:, 1:2], gc.t[:, 1:2], negA.t[0:64, 0:1], ALU.mult, R=[gc, negA], W=[gc])
            bk = b.bank()
            b.mm(bk, bk.t[0:64, 0:1], c["UT"], gc.t[:, 1:2], R=[gc, ct])
            b.cp(gc.t[:, 2:3], bk.t[0:64, 0:1], R=[bk], W=[gc])
            t64 = tmp64[p]
            b.ts(t64.t[:], c["UT"], gc.t[:, 1:2], None, ALU.mult, None, R=[gc, ct], W=[t64])
            bk = b.bank()
            b.mm(bk, bk.t[0:64, 0:64], c["ones"][0:64, 0:64], t64.t[:], R=[t64, ct])
            E = e1[p]
            b.ts(E.t[:], bk.t[0:64, 0:64], gc.t[:, 2:3], None, ALU.subtract, None, R=[bk, gc], W=[E])
            Dt = dts[p]; Dc = dcs[p]
            b.ts(Dt.t[:], E.t[:], 0.0, None, ALU.min, None, R=[E], W=[Dt])
            b.act(Dt.t[:], Dt.t[:], AF.Exp, R=[Dt], W=[Dt])
            b.tt(Dt.t[:], Dt.t[:], c["UT"], ALU.mult, R=[Dt, ct], W=[Dt])
            b.ts(Dc.t[:], E.t[:], -1.0, 0.0, ALU.mult, ALU.min, R=[E], W=[Dc])
            b.act(Dc.t[:], Dc.t[:], AF.Exp, R=[Dc], W=[Dc])
            b.tt(Dc.t[:], Dc.t[:], c["LTs"], ALU.mult, R=[Dc, ct], W=[Dc])
            bk = b.bank()
            b.mm(bk, bk.t[0:64, 0:64], kTc, kTc, R=[kn])
            M = Mb[0][p]; N = Nb[0][p]; T = Tb[0][p]
            b.stt(M.t[:], bk.t[0:64, 0:64], gc.t[:, 0:1], Dc.t[:], ALU.mult, ALU.mult, R=[bk, gc, Dc], W=[M])
            bk = b.bank()
            b.tp(bk, bk.t[0:64, 0:64], M.t[:], ident[0:64, 0:64], R=[M, ct])
            b.cp(N.t[:], bk.t[0:64, 0:64], R=[bk], W=[N])
            b.tt(T.t[:], ident[0:64, 0:64], N.t[:], ALU.subtract, R=[N, ct], W=[T])
            for s in range(1, 6):
                M2 = Mb[s % 2][p]; N2 = Nb[s % 2][p]; T2 = Tb[s % 2][p]
                bk = b.bank()
                b.mm(bk, bk.t[0:64, 0:64], N.t[:], M.t[:], R=[N, M])
                b.act(M2.t[:], bk.t[0:64, 0:64], AF.Copy, R=[bk], W=[M2])
                if s < 5:
                    bk = b.bank()
                    b.mm(bk, bk.t[0:64, 0:64], M.t[:], N.t[:], R=[N, M])
                    b.cp(N2.t[:], bk.t[0:64, 0:64], R=[bk], W=[N2])
                bk = b.bank()
                b.mm(bk, bk.t[0:64, 0:64], M2.t[:], T.t[:], R=[M2, T])
                b.tt(T2.t[:], bk.t[0:64, 0:64], T.t[:], ALU.add, R=[bk, T], W=[T2])
                M, N, T = M2, N2, T2
            bk = b.bank()
            b.tp(bk, bk.t[0:64, 0:128], kTc, ident, R=[kn, ct])
            b.tp(bk, bk.t[0:64, 128:256], vs.t[:, t0:t0 + 64], ident, R=[vs, ct])
            b.act(gc.t[:, 3:4], gc.t[:, 2:3], AF.Exp, R=[gc], W=[gc])
            b.tt(gc.t[:, 4:5], gc.t[:, 3:4], gc.t[:, 0:1], ALU.mult, R=[gc], W=[gc])
            b.cp(ktm[p].t[:], bk.t[0:64, 0:128], R=[bk], W=[ktm[p]])
            b.ts(kbt[p].t[:], bk.t[0:64, 0:128], gc.t[:, 4:5], None, ALU.mult, None, R=[bk, gc], W=[kbt[p]])
            b.ts(vbt[p].t[:], bk.t[0:64, 128:256], gc.t[:, 0:1], None, ALU.mult, None, R=[bk, gc], W=[vbt[p]])
            bk = b.bank()
            b.mm(bk, bk.t[0:64, 0:128], T.t[:], vbt[p].t[:], R=[T, vbt[p]])
            b.cp(Ub[p].t[:], bk.t[0:64, 0:128], R=[bk], W=[Ub[p]])
            bk = b.bank()
            b.mm(bk, bk.t[:, 0:64], kbt[p].t[:], T.t[:], R=[T, kbt[p]])
            b.act(WTb[p].t[:], bk.t[:, 0:64], AF.Copy, R=[bk], W=[WTb[p]])
            bk = b.bank()
            b.mm(bk, bk.t[0:64, 0:64], kTc, qTc, R=[kn, qn])
            b.tt(AqT[p].t[:], bk.t[0:64, 0:64], Dt.t[:], ALU.mult, R=[bk, Dt], W=[AqT[p]])
            bk = b.bank()
            b.mm(bk, bk.t[:, 0:1], c["ones"][0:64, 0:128], gc.t[:, 1:2], R=[gc, ct])
            b.cp(gl[p].t[:, 0:1], bk.t[:, 0:1], R=[bk], W=[gl[p]])
            b.act(gl[p].t[:, 1:2], gl[p].t[:, 0:1], AF.Exp, R=[gl[p]], W=[gl[p]])
            b.act(gc.t[:, 5:6], gc.t[:, 2:3], AF.Exp, R=[gc, gl[p]], W=[gc], scale=-1.0, bias=gl[p].t[0:64, 0:1])
            b.ts(kdec[p].t[:], ktm[p].t[:], gc.t[:, 5:6], None, ALU.mult, None, R=[ktm[p], gc], W=[kdec[p]])
            bkv = b.bank()
            b.mm(bkv, bkv.t[0:64, 0:128], WTb[p].t[:], St.t[:], R=[WTb[p], St])
            b.tt(vnew[p].t[:], Ub[p].t[:], bkv.t[0:64, 0:128], ALU.subtract, R=[bkv, Ub[p]], W=[vnew[p]])
            bk1 = b.bank()
            b.mm(bk1, bk1.t[0:64, 0:128], qTc, St.t[:], R=[qn, St])
            b.act(o1[p].t[:], bk1.t[0:64, 0:128], AF.Copy, R=[bk1, gc], W=[o1[p]], scale=gc.t[:, 3:4])
            bk2 = b.bank()
            b.mm(bk2, bk2.t[0:64, 0:128], AqT[p].t[:], vnew[p].t[:], R=[AqT[p], vnew[p]])
            b.tt(ob_[p].t[:], bk2.t[0:64, 0:128], o1[p].t[:], ALU.add, R=[bk2, o1[p]], W=[ob_[p]])
            bks = b.bank()
            b.mm(bks, bks.t[:, 0:128], kdec[p].t[:], vnew[p].t[:], R=[kdec[p], vnew[p]])
            b.stt(St.t[:], St.t[:], gl[p].t[:, 1:2], bks.t[:, 0:128], ALU.mult, ALU.add, R=[St, gl[p], bks], W=[St])
            b.act(szb[p].t[:], ob_[p].t[:], AF.Square, R=[ob_[p]], W=[szb[p], ssg[p]], accum_out=ssg[p].t[:, 0:1])
            b.rstd(ssg[p].t[:, 0:1], ssg[p].t[:, 0:1], R=[ssg[p]], W=[ssg[p]], scale=1.0 / 128)
            b.stt(ob_[p].t[:], ob_[p].t[:], ssg[p].t[:, 0:1], gng, ALU.mult, ALU.mult, R=[ob_[p], ssg[p], small], W=[ob_[p]])
            b.act(szb[p].t[:], z.t[:, 0:128], AF.Silu, R=[z], W=[szb[p]])
            b.tt(oast[p].t[:], ob_[p].t[:], szb[p].t[:], ALU.mult, R=[ob_[p], szb[p]], W=[oast[p]])
            sap, sbuf_ = send_at(g * 512 + t0, 64, 0, ("a", g, ch))
            b.dma(sap, oast[p].t[:], R=[oast[p]], W=[sbuf_], final=True)
        for comp in range(2):
            lo, hi = comp * 64, (comp + 1) * 64
            nkb = 4 * g + 4
            for kb in range(nkb):
                rel = kb - 4 * g
                qlo = max(rel, 0) * 128
                ps = b.banks[kb % 2]
                b.mm(ps, ps.t[:, qlo:512], kT.t[lo:hi, kb * 128:(kb + 1) * 128], qT.t[lo:hi, g * 512 + qlo:(g + 1) * 512],
                     R=[kT.sub(kb // 4), qT.sub(g)])
                P = PT[kb % 2]
                for qs in range(max(rel, 0), 4):
                    r_ = kb - (4 * g + qs) + 63
                    b.act(P.t[:, qs * 128:(qs + 1) * 128], ps.t[:, qs * 128:(qs + 1) * 128], AF.Exp, R=[ps, ct], W=[P], scale=SCALE,
                          bias=c["brel"][:, r_:r_ + 1])
                if rel >= 0:
                    b.tt(P.t[:, rel * 128:(rel + 1) * 128], P.t[:, rel * 128:(rel + 1) * 128], c["Cm"], ALU.mult, R=[P, ct], W=[P])
                for qs in range(max(rel, 0), 4):
                    po = b.banks[4 + qs]
                    b.mm(po, po.t[:, 0:130], P.t[:, qs * 128:(qs + 1) * 128], Vx.t[:, kb, 0:130], R=[P, Vx.sub(kb // 4)],
                         start=(kb == 0), stop=(kb == 4 * g + qs))
            for qs in range(4):
                po = b.banks[4 + qs]
                rcol = rz.t[:, comp * 4 + qs: comp * 4 + qs + 1]
                b.S.op("dve", lambda e, rcol=rcol, po=po: e.reciprocal(out=rcol, in_=po.t[:, 128:129]), R=[po], W=[rz])
                if comp == 0:
                    b.ts(obacc[qs].t[:], po.t[:, 0:128], rcol, None, ALU.mult, None, R=[po, rz], W=[obacc[qs]])
                else:
                    b.tt(rcol, rcol, neglam.t[:, 0:1], ALU.mult, R=[rz, neglam], W=[rz])
                    b.stt(obacc[qs].t[:], po.t[:, 0:128], rcol, obacc[qs].t[:], ALU.mult, ALU.add, R=[po, rz, obacc[qs]], W=[obacc[qs]])
                    o_ = obst[qs % 2]
                    b.act(o_.t[:], obacc[qs].t[:], AF.Square, R=[obacc[qs]], W=[o_, sso], accum_out=sso.t[:, 0:1])
                    b.rstd(sso.t[:, 0:1], sso.t[:, 0:1], R=[sso], W=[sso], scale=1.0 / 128)
                    b.stt(o_.t[:], obacc[qs].t[:], sso.t[:, 0:1], sub_g.t[:], ALU.mult, ALU.mult, R=[obacc[qs], sso, sub_g], W=[o_])
                    sap, sbuf_ = send_at(g * 512 + qs * 128, 128, 128, ("b", g, qs))
                    b.dma(sap, o_.t[:], R=[o_], W=[sbuf_], final=True)
        if after_group is not None:
            after_group(g)


def _cols(v):
    return np.ascontiguousarray(np.asarray(v, np.float32).reshape(-1, 128).T)


def alibi_slope(h):
    return float(2.0 ** (-8.0 * (h + 1) / 4))


def prep_common(inp, bidx):
    return {
        "c_col": _cols(inp["c"][bidx]),
        "w_ada": np.ascontiguousarray(inp["w_ada"][0]),
        "b_ada": np.ascontiguousarray(inp["b_ada"][0].reshape(1, -1)),
        "g1c": _cols(inp["norm1_g"][0]),
        "g2c": _cols(inp["norm2_g"][0]),
    }


def prep_ab(inp, bidx, h):
    w_in = inp["w_in"][0]
    sl = lambda name, width=128: w_in[:, W_OFF[name] + h * width: W_OFF[name] + (h + 1) * width]
    m = prep_common(inp, bidx)
    m["consts"] = host_consts(alibi_slope(h))
    m["xb"] = np.ascontiguousarray(inp["x"][bidx])
    m["w_cm"] = np.ascontiguousarray(np.concatenate([sl("qA"), sl("kA"), sl("vA"), sl("qB"), sl("kB")], axis=1))
    m["w_zbd"] = np.ascontiguousarray(np.concatenate([sl("zA"), sl("beta", 1), sl("decay", 1)], axis=1))
    m["w_vb"] = np.ascontiguousarray(sl("vB"))
    small = np.zeros((128, 384), np.float32)
    cw = inp["conv_w"][0]
    for j in range(3):
        small[:, j * 4:(j + 1) * 4] = cw[:, j * 512 + h * 128: j * 512 + (h + 1) * 128].T
    small[:, 12] = inp["a_log"][0, h]
    small[:, 13] = inp["dt_bias"][0, h]
    small[:, 14] = np.tile(inp["q_norm_g"][0], 2)
    small[:, 15] = np.tile(inp["k_norm_g"][0], 2)
    small[:, 128:256] = np.tile(inp["gdn_norm_g"][0][None, :], (128, 1))
    small[:, 256:384] = np.tile(inp["subln_g"][0][None, :], (128, 1))
    m["small"] = small
    m["lamrow"] = np.ascontiguousarray(np.concatenate(
        [inp["lambda_q1"][0], inp["lambda_k1"][0], inp["lambda_q2"][0], inp["lambda_k2"][0]]).reshape(1, 256))
    return m


def build_d(Sq, n_exp=257):
    nc = bass.Bass("TRN2", target_bir_lowering=False)
    b = Bld(nc)
    c = build_consts(b)
    ad = emit_adaln(b, c, want="gates")
    emit_d(b, c, ad, Sq, None, n_exp)
    b.S.emit()
    return nc


def build_fused(Sq, n_exp=257):
    nc = bass.Bass("TRN2", target_bir_lowering=False)
    b = Bld(nc)
    S = b.S
    c = build_consts(b)
    ad = emit_adaln(b, c, want="gates")
    CH = min(1024, Sq)
    NCH = Sq // CH
    sends = [S.dram(nc.dram_tensor(f"send_bounce{i}", [CH, 256], F32).ap(), f"send{i}") for i in range(NCH)]
    gaths = [S.dram(nc.dram_tensor(f"gath_bounce{i}", [4 * CH, 256], F32).ap(), f"gath{i}") for i in range(NCH)]

    def send_at(r0, n, c0, key):
        ch, lr = r0 // CH, r0 % CH
        return sends[ch].t[lr:lr + n, c0:c0 + 128], sends[ch].sub(key)

    def after_group(g):
        if ((g + 1) * 512) % CH == 0:
            ch = ((g + 1) * 512) // CH - 1
            sd, gt = sends[ch], gaths[ch]
            S.cc(lambda e: e.collective_compute("AllGather", ALU.bypass, replica_groups=[[0, 1, 2, 3], [4, 5, 6, 7]],
                                                ins=[sd.t.opt()], outs=[gt.t.opt()]),
                 R=[sd] + list(sd.subs.values()), W=[gt])

    emit_ab(b, c, ad["ab"], Sq, send_at, after_group)
    emit_d(b, c, ad, Sq, (gaths, CH), n_exp)
    S.emit()
    return nc


def emit_d(b, c, ad, Sq, gath, n_exp):
    S = b.S
    nc = b.nc
    T = Sq // 4
    NT = T // 128
    MG = min(512, T)
    NTG = T // MG
    TPG = MG // 128
    ab, g1bc, g2bc = ad["ab"], ad["g1bc"], ad["g2bc"]
    ident = c["ident"]; ct = c["t"]
    xs = b.inp("xs", [T, 1024])
    if gath is None:
        oa_in = b.inp("oa_in", [T, 512])
        ob_in = b.inp("ob_in", [T, 512])
    else:
        sel_d = b.inp("sel", [128, 4])
    wg_d = b.inp("w_gates", [1024, 2048])
    wog_d = b.inp("w_o_gdn", [512, 1024])
    wod_d = b.inp("w_o_diff", [512, 1024])
    wout_d = b.inp("w_out", [1024, 1024])
    wr_d = b.inp("w_router", [1024, 256])
    rb_d = b.inp("rbias", [128, 256])
    wgu_d = b.inp("w_gu", [n_exp, 1024, 512])
    wdn_d = b.inp("w_dn", [n_exp, 256, 1024])
    outd = b.outp("out", [T, 1024])

    acc = S.sb("acc", [128, NT, 1024])
    h2b = S.sb("h2T_bf", [128, 8, T], BF16)
    wt = S.sb("wt_all", [128, NT, n_exp + 1])
    b.memset(wt.t[:, :, 256:n_exp + 1], 1.0, W=[wt])
    with S.scope():
        wr = S.sb("wr", [128, 8, 256]); rb = S.sb("rb", [128, 256])
        b.dma(wr.t[:], wr_d.t[:, :].rearrange("(k p) c -> p k c", p=128), R=[wr_d], W=[wr])
        b.dma(rb.t[:], rb_d.t[:, :], R=[rb_d], W=[rb])
        wgb = [S.sb(f"wgb{i}", [128, 8, 512]) for i in range(2)]
        xt = [S.sb(f"dxt{i}", [128, 1024]) for i in range(1)]
        xn = S.sb("dxn", [128, 1024]); ssx = S.sb("dssx", [128, 1])
        hT1 = S.sb("hT1", [128, 8, 128]); h2f = hT1
        sg = S.sb("sg", [128, 2048])
        if gath is None:
            oat = S.sb("oat", [128, 512]); obt = S.sb("obt", [128, 512])
        else:
            cand = [S.sb(f"cand{i}", [128, 4, 256]) for i in range(2)]
            cmb = S.sb("cmb", [128, 4, 256])
            sel = S.sb("sel_sb", [128, 4])
            b.dma(sel.t[:], sel_d.t[:, :], R=[sel_d], W=[sel])
            gaths, CH = gath
        oaT = S.sb("oaT", [128, 4, 128]); obT = S.sb("obT", [128, 4, 128])
        mg = S.sb("merged", [128, 1024]); tmp = S.sb("dtmp", [128, 512]); mT = hT1
        scr = mg
        class _V:
            def __init__(self, tn, lo):
                self.t = tn.t[:, lo:lo + 256]; self.b = tn.b
        sc_ = _V(sg, 0); ch = _V(sg, 256); mc = _V(sg, 512)
        m8 = S.sb("r_m8", [128, 8]); gs = S.sb("r_gs", [128, 8]); gm = S.sb("r_gm", [128, 8]); pen = S.sb("r_pen", [128, 8])
        rsum = S.sb("r_sum", [128, 2])
        gi = 0
        for tt in range(NT):
            x_ = xt[0]
            b.dma(x_.t[:], xs.t[tt * 128:(tt + 1) * 128, :], R=[xs], W=[x_])
            if gath is None:
                b.dma(oat.t[:], oa_in.t[tt * 128:(tt + 1) * 128, :], R=[oa_in], W=[oat])
                b.dma(obt.t[:], ob_in.t[tt * 128:(tt + 1) * 128, :], R=[ob_in], W=[obt])
                srcs = ((oat, lambda cc: oat.t[:, cc * 128:(cc + 1) * 128]), (obt, lambda cc: obt.t[:, cc * 128:(cc + 1) * 128]))
            else:
                cf = lambda t_: t_.t[:].rearrange("p a b -> p (a b)")
                for hp in range(4):
                    cd = cand[hp % 2]
                    gtok = hp * T + tt * 128
                    gsrc = gaths[gtok // CH]
                    lr = gtok % CH
                    b.dma(cd.t[:], gsrc.t.rearrange("(i s) c -> s i c", i=4)[lr:lr + 128, :, :], R=[gsrc], W=[cd])
                    if hp == 0:
                        b.ts(cf(cmb), cf(cd), sel.t[:, 0:1], None, ALU.mult, None, R=[cd, sel], W=[cmb])
                    else:
                        b.stt(cf(cmb), cf(cd), sel.t[:, hp:hp + 1], cf(cmb), ALU.mult, ALU.add, R=[cd, sel, cmb], W=[cmb])
                srcs = ((cmb, lambda cc: cmb.t[:, cc, 0:128]), (cmb, lambda cc: cmb.t[:, cc, 128:256]))
            norm_tile2(b, c, x_, xn, ssx, scr, ab, 0, [hT1.t], [hT1], 0)
            for cbk in range(4):
                w = wgb[gi % 2]; gi += 1
                b.dma(w.t[:], wg_d.t[:, cbk * 512:(cbk + 1) * 512].rearrange("(k p) c -> p k c", p=128), R=[wg_d], W=[w])
                bk = b.bank()
                for k in range(8):
                    b.mm(bk, bk.t[:, 0:512], hT1.t[:, k, :], w.t[:, k, :], R=[hT1, w], start=(k == 0), stop=(k == 7))
                b.act(sg.t[:, cbk * 512:(cbk + 1) * 512], bk.t[:, 0:512], AF.Sigmoid, R=[bk], W=[sg])
            for ((src, view), dst) in zip(srcs, (oaT, obT)):
                bk = b.bank()
                for cc in range(4):
                    b.tp(bk, bk.t[:, cc * 128:(cc + 1) * 128], view(cc), ident, R=[src, ct])
                b.cp(dst.t[:].rearrange("p a b -> p (a b)"), bk.t[:, 0:512], R=[bk], W=[dst])
            wogT = wgb[gi % 2]; gi += 1
            b.dma(wogT.t[:].rearrange("p k c -> p (k c)").rearrange("p (a f) -> p a f", a=4), wog_d.t[:, :].rearrange("(k p) c -> p k c", p=128), R=[wog_d], W=[wogT])
            wodT = wgb[gi % 2]; gi += 1
            b.dma(wodT.t[:].rearrange("p k c -> p (k c)").rearrange("p (a f) -> p a f", a=4), wod_d.t[:, :].rearrange("(k p) c -> p k c", p=128), R=[wod_d], W=[wodT])
            wog_v = wogT.t[:].rearrange("p k c -> p (k c)").rearrange("p (a f) -> p a f", a=4)
            wod_v = wodT.t[:].rearrange("p k c -> p (k c)").rearrange("p (a f) -> p a f", a=4)
            for hf in range(2):
                bk = b.bank()
                for cc in range(4):
                    b.mm(bk, bk.t[:, 0:512], oaT.t[:, cc, :], wog_v[:, cc, hf * 512:(hf + 1) * 512], R=[oaT, wogT], start=(cc == 0), stop=(cc == 3))
                b.tt(mg.t[:, hf * 512:(hf + 1) * 512], bk.t[:, 0:512], sg.t[:, hf * 512:(hf + 1) * 512], ALU.mult, R=[bk, sg], W=[mg])
                bk = b.bank()
                for cc in range(4):
                    b.mm(bk, bk.t[:, 0:512], obT.t[:, cc, :], wod_v[:, cc, hf * 512:(hf + 1) * 512], R=[obT, wodT], start=(cc == 0), stop=(cc == 3))
                b.tt(tmp.t[:], bk.t[:, 0:512], sg.t[:, 1024 + hf * 512:1024 + (hf + 1) * 512], ALU.mult, R=[bk, sg], W=[tmp])
                b.tt(mg.t[:, hf * 512:(hf + 1) * 512], mg.t[:, hf * 512:(hf + 1) * 512], tmp.t[:], ALU.add, R=[mg, tmp], W=[mg])
            for k2 in range(2):
                bk = b.bank()
                for kk in range(4):
                    k = k2 * 4 + kk
                    b.tp(bk, bk.t[:, kk * 128:(kk + 1) * 128], mg.t[:, k * 128:(k + 1) * 128], ident, R=[mg, ct])
                b.cp(mT.t[:, k2 * 4:(k2 + 1) * 4, :].rearrange("p a b -> p (a b)"), bk.t[:, 0:512], R=[bk], W=[mT])
            for hf in range(2):
                wo_ = wgb[gi % 2]; gi += 1
                b.dma(wo_.t[:], wout_d.t[:, hf * 512:(hf + 1) * 512].rearrange("(k p) c -> p k c", p=128), R=[wout_d], W=[wo_])
                bk = b.bank()
                for k in range(8):
                    b.mm(bk, bk.t[:, 0:512], mT.t[:, k, :], wo_.t[:, k, :], R=[mT, wo_], start=(k == 0), stop=(k == 7))
                b.tt(tmp.t[:], bk.t[:, 0:512], g1bc.t[:, hf * 512:(hf + 1) * 512], ALU.mult, R=[bk, g1bc], W=[tmp])
                b.tt(acc.t[:, tt, hf * 512:(hf + 1) * 512], tmp.t[:], x_.t[:, hf * 512:(hf + 1) * 512], ALU.add, R=[tmp, x_], W=[acc.sub(tt)])
            norm_tile2(b, c, acc.sub(tt), xn, ssx, scr, ab, 16, [h2f.t, h2b.t], [h2f, h2b.sub(tt // TPG)], 0, xap=acc.t[:, tt, :], cols=[0, tt * 128])
            bk = b.bank()
            for k in range(8):
                b.mm(bk, bk.t[:, 0:256], h2f.t[:, k, :], wr.t[:, k, :], R=[h2f, wr], start=(k == 0), stop=(k == 7))
            b.act(sc_.t[:], bk.t[:, 0:256], AF.Sigmoid, R=[bk], W=[sc_])
            b.tt(ch.t[:], sc_.t[:], rb.t[:], ALU.add, R=[sc_, rb], W=[ch])
            for g in range(8):
                b.S.op("dve", lambda e, g=g: e.max(out=m8.t[:], in_=ch.t[:, g * 32:(g + 1) * 32]), R=[ch], W=[m8])
                b.tt(gs.t[:, g:g + 1], m8.t[:, 0:1], m8.t[:, 1:2], ALU.add, R=[m8], W=[gs])
            b.S.op("dve", lambda e: e.max(out=m8.t[:], in_=gs.t[:]), R=[gs], W=[m8])
            b.ts(gm.t[:], gs.t[:], m8.t[:, 3:4], None, ALU.is_ge, None, R=[gs, m8], W=[gm])
            b.ts(pen.t[:], gm.t[:], -1.0, 1e9, ALU.add, ALU.mult, R=[gm], W=[pen])
            for g in range(8):
                b.ts(mc.t[:, g * 32:(g + 1) * 32], ch.t[:, g * 32:(g + 1) * 32], gm.t[:, g:g + 1], pen.t[:, g:g + 1], ALU.mult, ALU.add,
                     R=[ch, gm, pen], W=[mc])
            b.S.op("dve", lambda e: e.max(out=m8.t[:], in_=mc.t[:]), R=[mc], W=[m8])
            b.ts(mc.t[:], mc.t[:], m8.t[:, 7:8], None, ALU.is_ge, None, R=[mc, m8], W=[mc])
            b.tt(mc.t[:], mc.t[:], sc_.t[:], ALU.mult, R=[mc, sc_], W=[mc])
            b.S.op("dve", lambda e: e.tensor_reduce(out=rsum.t[:, 0:1], in_=mc.t[:], axis=AX.X, op=ALU.add), R=[mc], W=[rsum])
            b.S.op("dve", lambda e: e.reciprocal(out=rsum.t[:, 1:2], in_=rsum.t[:, 0:1]), R=[rsum], W=[rsum])
            b.ts(wt.t[:, tt, 0:256], mc.t[:], rsum.t[:, 1:2], 2.5, ALU.mult, ALU.mult, R=[mc, rsum], W=[wt])
    with S.scope():
        wgu_st = [S.sb(f"wgu_st{i}", [128, 8, 512]) for i in range(2)]
        wd_st = [S.sb(f"wd_st{i}", [128, 2, 1024]) for i in range(2)]
        wgu_bf = [S.sb(f"wgu_bf{i}", [128, 8, 512], BF16) for i in range(2)]
        wd_bf = [S.sb(f"wd_bf{i}", [128, 2, 1024], BF16) for i in range(2)]
        AT = [S.sb(f"AT{i}", [128, 2, MG], BF16) for i in range(2)]
        slu = [S.sb(f"slu{i}", [128, MG]) for i in range(2)]
        for e in range(n_exp):
            p = e % 2
            b.dma(wgu_st[p].t[:], wgu_d.t[e].rearrange("(k p) c -> p k c", p=128), R=[wgu_d], W=[wgu_st[p]])
            b.dma(wd_st[p].t[:], wdn_d.t[e].rearrange("(k p) c -> p k c", p=128), R=[wdn_d], W=[wd_st[p]])
            b.cp(wgu_bf[p].t[:, 0:4, :], wgu_st[p].t[:, 0:4, :], R=[wgu_st[p]], W=[wgu_bf[p]], eng="pool")
            b.cp(wgu_bf[p].t[:, 4:8, :], wgu_st[p].t[:, 4:8, :], R=[wgu_st[p]], W=[wgu_bf[p]], eng="pool")
            for c2 in range(2):
                b.tt(wd_bf[p].t[:, c2, :], wd_st[p].t[:, c2, :], g2bc.t[:], ALU.mult, R=[wd_st[p], g2bc], W=[wd_bf[p]], eng="pool")
            ai = 0
            for tg in range(NTG):
                bks = []
                for fc in range(4):
                    bk = b.bank()
                    bks.append(bk)
                    for k in range(8):
                        b.mm(bk, bk.t[:, 0:MG], wgu_bf[p].t[:, k, fc * 128:(fc + 1) * 128], h2b.t[:, k, tg * MG:(tg + 1) * MG],
                             R=[wgu_bf[p], h2b.sub(tg)], start=(k == 0), stop=(k == 7))
                A = AT[tg % 2]
                for c2 in range(2):
                    sl = slu[c2]
                    b.act(sl.t[:], bks[c2].t[:, 0:MG], AF.Silu, R=[bks[c2]], W=[sl])
                    b.tt(A.t[:, c2, :], sl.t[:], bks[2 + c2].t[:, 0:MG], ALU.mult, R=[sl, bks[2 + c2]], W=[A])
                for ti in range(TPG):
                    tile_ = tg * TPG + ti
                    for hf in range(2):
                        bk = b.bank()
                        for c2 in range(2):
                            b.mm(bk, bk.t[:, 0:512], A.t[:, c2, ti * 128:(ti + 1) * 128], wd_bf[p].t[:, c2, hf * 512:(hf + 1) * 512],
                                 R=[A, wd_bf[p]], start=(c2 == 0), stop=(c2 == 1))
                        b.stt(acc.t[:, tile_, hf * 512:(hf + 1) * 512], bk.t[:, 0:512], wt.t[:, tile_, e:e + 1],
                              acc.t[:, tile_, hf * 512:(hf + 1) * 512], ALU.mult, ALU.add, R=[bk, wt, acc.sub(tile_)], W=[acc.sub(tile_)])
        for tt in range(NT):
            b.dma(outd.t[tt * 128:(tt + 1) * 128, :], acc.t[:, tt, :], R=[acc.sub(tt)], W=[outd.sub(tt)], final=True)


def prep_d(inp, bidx, j, T, oa_b, ob_b):
    m = prep_common(inp, bidx)
    m["consts"] = host_consts(alibi_slope(0))
    w_in = inp["w_in"][0]
    m["xs"] = np.ascontiguousarray(inp["x"][bidx, j * T:(j + 1) * T])
    m["oa_in"] = np.ascontiguousarray(oa_b[j * T:(j + 1) * T])
    m["ob_in"] = np.ascontiguousarray(ob_b[j * T:(j + 1) * T])
    m["w_gates"] = np.ascontiguousarray(w_in[:, W_OFF["ga"]:W_OFF["ga"] + 2048])
    m["w_o_gdn"] = np.ascontiguousarray(inp["w_o_gdn"][0])
    m["w_o_diff"] = np.ascontiguousarray(inp["w_o_diff"][0])
    m["w_out"] = np.ascontiguousarray(inp["w_out"][0])
    m["w_router"] = np.ascontiguousarray(inp["w_router"][0])
    m["rbias"] = np.ascontiguousarray(np.tile(inp["router_bias"][0][None, :], (128, 1)))
    return m


_EXP_CACHE = {}


def expert_stack(inp):
    key = id(inp["w_exp_gate_up"])
    if key not in _EXP_CACHE:
        _EXP_CACHE.clear()
        wgu = np.concatenate([inp["w_exp_gate_up"][0], inp["w_shared_gate_up"][0][None]], axis=0)
        wdn = np.concatenate([inp["w_exp_down"][0], inp["w_shared_down"][0][None]], axis=0)
        _EXP_CACHE[key] = (np.ascontiguousarray(wgu), np.ascontiguousarray(wdn))
    return _EXP_CACHE[key]


def prep_fused(inp, cid, T):
    bi, j = cid // 4, cid % 4
    m = prep_ab(inp, bi, j)
    w_in = inp["w_in"][0]
    m["xs"] = np.ascontiguousarray(inp["x"][bi, j * T:(j + 1) * T])
    m["w_gates"] = np.ascontiguousarray(w_in[:, W_OFF["ga"]:W_OFF["ga"] + 2048])
    m["w_o_gdn"] = np.ascontiguousarray(inp["w_o_gdn"][0])
    m["w_o_diff"] = np.ascontiguousarray(inp["w_o_diff"][0])
    m["w_out"] = np.ascontiguousarray(inp["w_out"][0])
    m["w_router"] = np.ascontiguousarray(inp["w_router"][0])
    m["rbias"] = np.ascontiguousarray(np.tile(inp["router_bias"][0][None, :], (128, 1)))
    sel = np.zeros((128, 4), np.float32)
    sel[:, j] = 1.0
    m["sel"] = sel
    return m


def kernel(**inp):
    inp = {k: np.asarray(v) for k, v in inp.items()}
    B, Sq, _ = inp["x"].shape
    T = Sq // 4
    nc = build_fused(Sq)
    wgu, wdn = expert_stack(inp)
    maps = []
    for cid in range(8):
        m = prep_fused(inp, cid, T)
        m["w_gu"] = wgu
        m["w_dn"] = wdn
        maps.append(m)
    res = run_bass_kernel_spmd(nc, maps, core_ids=list(range(8)))
    out = np.zeros((B, Sq, 1024), np.float32)
    for cid in range(8):
        bi, j = cid // 4, cid % 4
        out[bi, j * T:(j + 1) * T] = res.results[cid]["out"]
    return out
```

```python
import contextlib
import numpy as np
import concourse.bass as bass
import concourse.mybir as mybir
from concourse.bass_utils import run_bass_kernel_spmd

F32 = mybir.dt.float32
BF16 = mybir.dt.bfloat16
I32 = mybir.dt.int32
AF = mybir.ActivationFunctionType
ALU = mybir.AluOpType


class Buf:
    __slots__ = ("name", "w", "r")

    def __init__(self, name):
        self.name = name
        self.w = None
        self.r = []


class Tn:
    def __init__(self, t, name):
        self.t = t
        self.name = name
        self.b = Buf(name)
        self.subs = {}

    def sub(self, key):
        if key not in self.subs:
            self.subs[key] = Buf(f"{self.name}:{key}")
        return self.subs[key]


class Op:
    __slots__ = ("eng", "fn", "deps", "is_dma", "sem", "val", "signals", "final", "inc")

    def __init__(self, eng, fn, is_dma=False):
        self.eng = eng
        self.fn = fn
        self.deps = []
        self.is_dma = is_dma
        self.sem = None
        self.val = None
        self.signals = False
        self.final = False
        self.inc = 1


def _buf(x):
    return x.b if hasattr(x, 'b') else x


class Sched:
    ENGS = ("pe", "act", "dve", "pool", "sp")

    def __init__(self, nc, n_dma_sems=None):
        self.nc = nc
        self.stack = contextlib.ExitStack()
        self.stacks = [self.stack]
        self.ops = {e: [] for e in self.ENGS}
        self.esem = {e: nc.alloc_semaphore(name=f"sem_{e}") for e in self.ENGS}
        nd = n_dma_sems or {"sp": 24, "pool": 12, "act": 4}
        self.dsem = {q: [nc.alloc_semaphore(name=f"dsem_{q}{i}") for i in range(n)] for q, n in nd.items()}
        self.dlast = {q: [None] * n for q, n in nd.items()}
        self.dcnt = {q: [0] * n for q, n in nd.items()}
        self.dnext = {q: 0 for q in nd}
        self.n_ops = 0

    def sb(self, name, shape, dtype=F32):
        t = self.stacks[-1].enter_context(self.nc.sbuf_tensor(name, list(shape), dtype))
        return Tn(t, name)

    @contextlib.contextmanager
    def scope(self):
        st = contextlib.ExitStack()
        self.stacks.append(st)
        try:
            yield
        finally:
            self.barrier()
            self.stacks.pop()
            st.close()

    def barrier(self):
        lasts = []
        for e in self.ENGS:
            for o in reversed(self.ops[e]):
                if o.fn is not None and not o.is_dma:
                    lasts.append(o)
                    break
        for q in self.dlast:
            lasts += [o for o in self.dlast[q] if o is not None]
        for e in self.ENGS:
            o = Op(e, None)
            o.sem = self.esem[e]
            o.deps = list(lasts)
            self.ops[e].append(o)

    def ps(self, name, shape, dtype=F32):
        t = self.stack.enter_context(self.nc.psum_tensor(name, list(shape), dtype))
        return Tn(t, name)

    def dram(self, ap, name):
        return Tn(ap, name)

    def scratch(self, name, shape, dtype=F32):
        t = self.nc.dram_tensor(name, list(shape), dtype, kind="Internal")
        return Tn(t.ap(), name)

    def _track(self, op, R, W):
        eng = op.eng
        deps = []

        def add(d, kind):
            if d is None:
                return
            if not d.is_dma and not op.is_dma and d.eng == eng:
                if eng == "pe":
                    return
            deps.append(d)

        Rb = [_buf(x) for x in R]
        Wb = [_buf(x) for x in W]
        for b in Rb:
            add(b.w, "raw")
        for b in Wb:
            add(b.w, "waw")
            for r in b.r:
                add(r, "war")
        for b in Rb:
            b.r.append(op)
        for b in Wb:
            b.w = op
            b.r = []
        seen = set()
        for d in deps:
            if id(d) not in seen and d is not op:
                seen.add(id(d))
                op.deps.append(d)

    def op(self, eng, fn, R=(), W=()):
        o = Op(eng, fn)
        o.sem = self.esem[eng]
        self._track(o, R, W)
        self.ops[eng].append(o)
        self.n_ops += 1
        return o

    def dma(self, q, out, in_, R=(), W=(), final=False, fn=None, **kw):
        if fn is None:
            fn = lambda e: e.dma_start(out=out, in_=in_, **kw)
        o = Op(q, fn, is_dma=True)
        o.inc = 16
        k = self.dnext[q]
        self.dnext[q] = (k + 1) % len(self.dsem[q])
        o.sem = self.dsem[q][k]
        self.dcnt[q][k] += 16
        o.val = self.dcnt[q][k]
        o.signals = True
        o.final = final
        self._track(o, R, W)
        prev = self.dlast[q][k]
        if prev is not None and prev not in o.deps:
            o.deps.append(prev)
        self.dlast[q][k] = o
        self.ops[q].append(o)
        self.n_ops += 1
        return o

    def cc(self, fn, R=(), W=()):
        if "cc" not in self.dsem:
            self.dsem["cc"] = [self.nc.alloc_semaphore(name="dsem_cc")]
            self.dlast["cc"] = [None]
            self.dcnt["cc"] = [0]
        o = Op("pool", fn, is_dma=True)
        o.inc = 1
        o.sem = self.dsem["cc"][0]
        self.dcnt["cc"][0] += 1
        o.val = self.dcnt["cc"][0]
        o.signals = True
        self._track(o, R, W)
        prev = self.dlast["cc"][0]
        if prev is not None and prev not in o.deps:
            o.deps.append(prev)
        self.dlast["cc"][0] = o
        self.ops["pool"].append(o)
        return o

    def emit(self):
        for e in self.ENGS:
            for o in self.ops[e]:
                for d in o.deps:
                    d.signals = True
        for e in self.ENGS:
            c = 0
            for o in self.ops[e]:
                if o.fn is None:
                    o.signals = False
                    o.val = c
                    continue
                if not o.is_dma and o.signals:
                    c += 1
                    o.val = c
        nc = self.nc

        def replay(ename):
            def run(e):
                waited = {}
                finals = []
                for o in self.ops[ename]:
                    need = {}
                    for d in o.deps:
                        k = id(d.sem)
                        if k not in need or need[k][1] < d.val:
                            need[k] = (d.sem, d.val)
                    for k, (sem, val) in need.items():
                        if waited.get(k, 0) >= val:
                            continue
                        e.wait_ge(sem, val)
                        waited[k] = val
                    if o.fn is None:
                        continue
                    ins = o.fn(e)
                    if o.signals:
                        ins.then_inc(o.sem, o.inc)
                    if o.final:
                        finals.append(o)
                for o in finals:
                    e.wait_ge(o.sem, o.val)
            return run

        with nc.Block() as block:
            block.sync(replay("sp"))
            block.tensor(replay("pe"))
            block.scalar(replay("act"))
            block.vector(replay("dve"))
            block.gpsimd(replay("pool"))
        self.stack.close()


AX = mybir.AxisListType
D = 1024
KC = 8
EPS = 1e-6
LAM_INIT = 0.2
W_OFF = dict(qA=0, kA=512, vA=1024, zA=1536, beta=2048, decay=2052, qB=2056, kB=2568, vB=3080, ga=3592, gb=4616)


class Bld:
    def __init__(self, nc):
        self.nc = nc
        self.S = Sched(nc)
        self.banks = [self.S.ps(f"bank{i}", [128, 512]) for i in range(8)]
        self.bi = 0
        self.ins = {}

    def inp(self, name, shape, dtype=F32):
        ap = self.nc.dram_tensor(name, list(shape), dtype, kind="ExternalInput").ap()
        t = self.S.dram(ap, name)
        self.ins[name] = t
        return t

    def outp(self, name, shape, dtype=F32):
        ap = self.nc.dram_tensor(name, list(shape), dtype, kind="ExternalOutput").ap()
        return self.S.dram(ap, name)

    def bank(self):
        b = self.banks[self.bi % 8]
        self.bi += 1
        return b

    def mm(self, bank, out, lhsT, rhs, R, start=True, stop=True):
        self.S.op("pe", lambda e: e.matmul(out, lhsT, rhs, start=start, stop=stop), R=R, W=[bank])

    def tp(self, bank, out, in_, ident, R):
        self.S.op("pe", lambda e: e.transpose(out, in_, ident), R=R, W=[bank])

    def act(self, out, in_, func, R, W, **kw):
        self.S.op("act", lambda e: e.activation(out=out, in_=in_, func=func, **kw), R=R, W=W)

    def ts(self, out, in0, s1, s2, op0, op1, R, W, eng="dve"):
        if op1 is None:
            self.S.op(eng, lambda e: e.tensor_scalar(out=out, in0=in0, scalar1=s1, scalar2=None, op0=op0), R=R, W=W)
        else:
            self.S.op(eng, lambda e: e.tensor_scalar(out=out, in0=in0, scalar1=s1, scalar2=s2, op0=op0, op1=op1), R=R, W=W)

    def tt(self, out, in0, in1, op, R, W, eng="dve"):
        self.S.op(eng, lambda e: e.tensor_tensor(out=out, in0=in0, in1=in1, op=op), R=R, W=W)

    def stt(self, out, in0, scalar, in1, op0, op1, R, W, eng="dve"):
        self.S.op(eng, lambda e: e.scalar_tensor_tensor(out=out, in0=in0, scalar=scalar, in1=in1, op0=op0, op1=op1), R=R, W=W)

    def cp(self, out, in_, R, W, eng="dve"):
        self.S.op(eng, lambda e: e.tensor_copy(out=out, in_=in_), R=R, W=W)

    def memset(self, ap, val, W, eng="dve"):
        self.S.op(eng, lambda e: e.memset(ap, val), R=[], W=W)

    def dma(self, out, in_, R, W, q="sp", final=False):
        self.S.dma(q, out, in_, R=R, W=W, final=final)

    def rstd(self, out, in_, R, W, scale, eps=EPS, lnbias=0.0):
        self.act(out, in_, AF.Ln, R=R, W=W, scale=scale, bias=eps)
        self.act(out, out, AF.Exp, R=W, W=W, scale=-0.5, bias=lnbias)


def build_consts(b, slope=None):
    S = b.S
    c = {}
    cin = b.inp("consts", [128, 128 * 6])
    ct = S.sb("consts_sb", [128, 128 * 6])
    b.dma(ct.t[:], cin.t[:, :], R=[cin], W=[ct])
    c["t"] = ct
    c["ident"] = ct.t[:, 0:128]
    c["ones"] = ct.t[:, 128:256]
    c["bd"] = ct.t[:, 256:384]
    c["UT"] = ct.t[0:64, 384:448]
    c["LTs"] = ct.t[0:64, 448:512]
    c["Cm"] = ct.t[:, 512:640]
    c["brel"] = ct.t[:, 640:704]
    return c


def host_consts(slope):
    c = np.zeros((128, 768), np.float32)
    c[:, 0:128] = np.eye(128)
    c[:, 128:256] = 1.0
    c[0:64, 256:320] = 1.0
    c[64:128, 320:384] = 1.0
    p = np.arange(64)[:, None]
    f = np.arange(64)[None, :]
    c[0:64, 384:448] = (p <= f)
    c[0:64, 448:512] = (p > f)
    k = np.arange(128)[:, None]
    q = np.arange(128)[None, :]
    cm = np.where(k <= q, 1.0, np.where((k // 64) == (q // 64), np.exp(-2.0 * slope * (k - q)), 0.0))
    c[:, 512:640] = cm
    r = np.arange(64)[None, :]
    c[:, 640:704] = slope * (k + 128.0 * (r - 63) - 127.0)
    return c


def emit_adaln(b, c, want):
    S = b.S
    ccol = b.inp("c_col", [128, 8])
    wada = b.inp("w_ada", [1024, 6144])
    bada = b.inp("b_ada", [1, 6144])
    g1c = b.inp("g1c", [128, 8])
    g2c = b.inp("g2c", [128, 8])
    modc = S.sb("modc", [128, 48])
    ab = S.sb("ab", [128, 32])
    out = {"modc": modc, "ab": ab}
    if want == "gates":
        out["g1bc"] = S.sb("g1bc", [128, 1024])
        out["g2bc"] = S.sb("g2bc", [128, 1024])
    with S.scope():
        _emit_adaln_body(b, c, want, out, ccol, wada, bada, g1c, g2c)
    return out


def _emit_adaln_body(b, c, want, out, ccol, wada, bada, g1c, g2c):
    S = b.S
    modc = out["modc"]
    ab = out["ab"]
    sc = S.sb("sc", [128, 8])
    gt = S.sb("gcols", [128, 16])
    brow = S.sb("brow", [1, 6144])
    modrow = S.sb("modrow", [1, 6144])
    wb = [S.sb(f"wadab{i}", [128, 8, 512]) for i in range(2)]
    b.dma(sc.t[:], ccol.t[:, :], R=[ccol], W=[sc])
    b.dma(gt.t[:, 0:8], g1c.t[:, :], R=[g1c], W=[gt])
    b.dma(gt.t[:, 8:16], g2c.t[:, :], R=[g2c], W=[gt])
    b.dma(brow.t[:], bada.t[:, :], R=[bada], W=[brow])
    b.act(sc.t[:], sc.t[:], AF.Silu, R=[sc], W=[sc])
    for jb in range(12):
        w = wb[jb % 2]
        b.dma(w.t[:], wada.t[:, jb * 512:(jb + 1) * 512].rearrange("(k p) c -> p k c", p=128), R=[wada], W=[w])
        bk = b.bank()
        for k in range(8):
            b.mm(bk, bk.t[0:1, 0:512], sc.t[:, k:k + 1], w.t[:, k, :], R=[sc, w], start=(k == 0), stop=(k == 7))
        b.tt(modrow.t[0:1, jb * 512:(jb + 1) * 512], bk.t[0:1, 0:512], brow.t[0:1, jb * 512:(jb + 1) * 512], ALU.add,
             R=[bk, brow], W=[modrow])
    bk = b.bank()
    for j in range(48):
        b.mm(bk, bk.t[:, j:j + 1], modrow.t[0:1, j * 128:(j + 1) * 128], c["ones"][0:1, 0:1], R=[modrow, c["t"]])
    b.cp(modc.t[:], bk.t[:, 0:48], R=[bk], W=[modc])
    b.ts(ab.t[:, 0:8], modc.t[:, 8:16], 1.0, None, ALU.add, None, R=[modc], W=[ab])
    b.tt(ab.t[:, 0:8], ab.t[:, 0:8], gt.t[:, 0:8], ALU.mult, R=[ab, gt], W=[ab])
    b.cp(ab.t[:, 8:16], modc.t[:, 0:8], R=[modc], W=[ab])
    b.ts(ab.t[:, 16:24], modc.t[:, 32:40], 1.0, None, ALU.add, None, R=[modc], W=[ab])
    b.tt(ab.t[:, 16:24], ab.t[:, 16:24], gt.t[:, 8:16], ALU.mult, R=[ab, gt], W=[ab])
    b.cp(ab.t[:, 24:32], modc.t[:, 24:32], R=[modc], W=[ab])
    if want == "gates":
        g1bc = out["g1bc"]
        g2bc = out["g2bc"]
        for (dst, off) in ((g1bc, 2048), (g2bc, 5120)):
            for hf in range(2):
                bk = b.bank()
                b.mm(bk, bk.t[:, 0:512], c["ones"][0:1, 0:128], modrow.t[0:1, off + hf * 512: off + (hf + 1) * 512],
                     R=[modrow, c["t"]])
                b.cp(dst.t[:, hf * 512:(hf + 1) * 512], bk.t[:, 0:512], R=[bk], W=[dst])
    return out


def emit_norm_hT(b, c, xsrc, row0, ntiles, acol, bcol, hT, xt_bufs, ssq, hT_dtype_ap=None, x_keep=None):
    S = b.S
    for tt in range(ntiles):
        xt = xt_bufs[tt % len(xt_bufs)] if x_keep is None else x_keep[tt]
        b.dma(xt.t[:], xsrc.t[row0 + tt * 128: row0 + (tt + 1) * 128, :], R=[xsrc], W=[xt])
    return


def norm_tile(b, c, xt, xn, ss, acol, bcol, hT, col0, scratch):
    b.act(scratch.t[:], xt.t[:], AF.Square, R=[xt], W=[scratch, ss], accum_out=ss.t[:, 0:1])
    b.rstd(ss.t[:, 0:1], ss.t[:, 0:1], R=[ss], W=[ss], scale=1.0 / D)
    b.ts(xn.t[:], xt.t[:], ss.t[:, 0:1], None, ALU.mult, None, R=[xt, ss], W=[xn])
    for k2 in range(2):
        bk = b.bank()
        for kk in range(4):
            k = k2 * 4 + kk
            b.tp(bk, bk.t[:, kk * 128:(kk + 1) * 128], xn.t[:, k * 128:(k + 1) * 128], c["ident"], R=[xn, c["t"]])
        for kk in range(4):
            k = k2 * 4 + kk
            b.act(hT[:, k, col0:col0 + 128], bk.t[:, kk * 128:(kk + 1) * 128], AF.Identity, R=[bk, acol[1]], W=[acol[2]],
                  scale=acol[0][:, k:k + 1], bias=bcol[0][:, k:k + 1])


def norm_tile2(b, c, xt, xn, ss, scratch, ab, aoff, hT, hTt, col0, xap=None, cols=None):
    if xap is None:
        xap = xt.t[:]
    b.act(scratch.t[:], xap, AF.Square, R=[xt], W=[scratch, ss], accum_out=ss.t[:, 0:1])
    b.rstd(ss.t[:, 0:1], ss.t[:, 0:1], R=[ss], W=[ss], scale=1.0 / D)
    b.ts(xn.t[:], xap, ss.t[:, 0:1], None, ALU.mult, None, R=[xt, ss], W=[xn])
    for k2 in range(2):
        bk = b.bank()
        for kk in range(4):
            k = k2 * 4 + kk
            b.tp(bk, bk.t[:, kk * 128:(kk + 1) * 128], xn.t[:, k * 128:(k + 1) * 128], c["ident"], R=[xn, c["t"]])
        for kk in range(4):
            k = k2 * 4 + kk
            for di, (dst, dstT) in enumerate(zip(hT, hTt)):
                cc0 = col0 if cols is None else cols[di]
                b.act(dst[:, k, cc0:cc0 + 128], bk.t[:, kk * 128:(kk + 1) * 128], AF.Identity, R=[bk, ab], W=[dstT],
                      scale=ab.t[:, aoff + k:aoff + k + 1], bias=ab.t[:, aoff + 8 + k:aoff + 9 + k])


def build_ab(Sq):
    nc = bass.Bass("TRN2", target_bir_lowering=False)
    b = Bld(nc)
    c = build_consts(b)
    ad = emit_adaln(b, c, want="none")
    send = b.outp("send", [Sq, 256])
    emit_ab(b, c, ad["ab"], Sq, lambda r0, n, c0, key: (send.t[r0:r0 + n, c0:c0 + 128], send.sub(key)))
    b.S.emit()
    return nc


def emit_ab(b, c, ab, Sq, send_at, after_group=None):
    with b.S.scope():
        _emit_ab_body(b, c, ab, Sq, send_at, after_group)


def _emit_ab_body(b, c, ab, Sq, send_at, after_group):
    S = b.S
    NG = Sq // 512
    xb = b.inp("xb", [Sq, 1024])
    wcm_d = b.inp("w_cm", [1024, 640])
    wzbd_d = b.inp("w_zbd", [1024, 130])
    wvb_d = b.inp("w_vb", [1024, 128])
    small_d = b.inp("small", [128, 384])
    lam_d = b.inp("lamrow", [1, 256])

    wcm = S.sb("wcm", [128, 8, 640])
    wzbd = S.sb("wzbd", [128, 8, 130])
    wvb = S.sb("wvb", [128, 8, 128])
    small = S.sb("small_sb", [128, 384])
    lamr = S.sb("lamr", [1, 256])
    b.dma(wcm.t[:], wcm_d.t[:, :].rearrange("(k p) c -> p k c", p=128), R=[wcm_d], W=[wcm])
    b.dma(wzbd.t[:], wzbd_d.t[:, :].rearrange("(k p) c -> p k c", p=128), R=[wzbd_d], W=[wzbd])
    b.dma(wvb.t[:], wvb_d.t[:, :].rearrange("(k p) c -> p k c", p=128), R=[wvb_d], W=[wvb])
    b.dma(small.t[:], small_d.t[:, :], R=[small_d], W=[small])
    b.dma(lamr.t[:], lam_d.t[:, :], R=[lam_d], W=[lamr])
    convw = lambda j, tap: small.t[:, j * 4 + tap: j * 4 + tap + 1]
    gng = small.t[0:64, 128:256]
    sub_g = S.sb("sub_g", [128, 128])
    b.ts(sub_g.t[:], small.t[:, 256:384], 1.0 - LAM_INIT, None, ALU.mult, None, R=[small], W=[sub_g])
    negA = S.sb("negA", [128, 1])
    b.act(negA.t[:], small.t[:, 12:13], AF.Exp, R=[small], W=[negA])
    b.ts(negA.t[:], negA.t[:], -1.0, None, ALU.mult, None, R=[negA], W=[negA])
    lt = S.sb("lam_t", [1, 136])
    b.tt(lt.t[0:1, 0:64], lamr.t[0:1, 0:64], lamr.t[0:1, 64:128], ALU.mult, R=[lamr], W=[lt])
    b.tt(lt.t[0:1, 64:128], lamr.t[0:1, 128:192], lamr.t[0:1, 192:256], ALU.mult, R=[lamr], W=[lt])
    b.S.op("dve", lambda e: e.tensor_reduce(out=lt.t[0:1, 128:129], in_=lt.t[0:1, 0:64], axis=AX.X, op=ALU.add), R=[lt], W=[lt])
    b.S.op("dve", lambda e: e.tensor_reduce(out=lt.t[0:1, 129:130], in_=lt.t[0:1, 64:128], axis=AX.X, op=ALU.add), R=[lt], W=[lt])
    b.act(lt.t[0:1, 128:130], lt.t[0:1, 128:130], AF.Exp, R=[lt], W=[lt])
    b.tt(lt.t[0:1, 130:131], lt.t[0:1, 129:130], lt.t[0:1, 128:129], ALU.subtract, R=[lt], W=[lt])
    b.ts(lt.t[0:1, 130:131], lt.t[0:1, 130:131], -LAM_INIT, None, ALU.add, None, R=[lt], W=[lt])
    neglam = S.sb("neglam", [128, 1])
    bk = b.bank()
    b.mm(bk, bk.t[:, 0:1], c["ones"][0:1, 0:128], lt.t[0:1, 130:131], R=[lt, c["t"]])
    b.cp(neglam.t[:], bk.t[:, 0:1], R=[bk], W=[neglam])

    qT = S.sb("qT_all", [128, Sq])
    kT = S.sb("kT_all", [128, Sq])
    Vx = S.sb("Vext", [128, Sq // 128, 130])
    b.memset(Vx.t[:, :, 128:130], 1.0, W=[Vx])
    hT = [S.sb(f"hT{i}", [128, 8, 512]) for i in range(1)]
    xt = [S.sb(f"xt{i}", [128, 1024]) for i in range(1)]
    xn = S.sb("xn", [128, 1024])
    scr = xn
    ssx = S.sb("ssx", [128, 1])
    pre = [S.sb(f"pre{j}", [128, 515]) for j in range(3)]
    for j in range(3):
        b.memset(pre[j].t[:, 0:3], 0.0, W=[pre[j]])
    cv = [S.sb(f"cv{j}", [128, 512]) for j in range(3)]
    sq = S.sb("sq", [128, 512])
    rs = S.sb("rs", [128, 512])
    qk_tmp = S.sb("qk_tmp", [128, 512])
    St = S.sb("state", [128, 128])
    b.memset(St.t[:], 0.0, W=[St])
    def cb(name, shape):
        return [S.sb(f"{name}{i}", shape) for i in range(2)]
    zb = cb("zb", [64, 132]); gcol = cb("gcol", [64, 8]); e1 = cb("e1", [64, 64]); dts = cb("dts", [64, 64])
    dcs = cb("dcs", [64, 64]); Mb = [cb("Ma", [64, 64]), cb("Mb", [64, 64])]; Nb = [cb("Na", [64, 64]), cb("Nb", [64, 64])]
    Tb = [cb("Ta", [64, 64]), cb("Tb", [64, 64])]; ktm = cb("ktm", [64, 128]); kbt = cb("kbt", [64, 128])
    vbt = cb("vbt", [64, 128]); kdec = cb("kdec", [64, 128]); Ub = cb("Ub", [64, 128]); WTb = cb("WTb", [128, 64])
    AqT = cb("AqT", [64, 64]); gl = cb("gl", [128, 2]); vnew = cb("vnew", [64, 128]); o1 = cb("o1", [64, 128])
    ob_ = cb("ob_", [64, 128]); szb = cb("szb", [64, 128]); tmp64 = cb("tmp64", [64, 64]); oast = cb("oast", [64, 128])
    ssg = cb("ssg", [64, 2])
    PT = [S.sb(f"PT{i}", [128, 512]) for i in range(2)]
    obacc = [S.sb(f"obacc{i}", [128, 128]) for i in range(4)]
    rz = S.sb("rz", [128, 8])
    obst = [S.sb(f"obst{i}", [128, 128]) for i in range(2)]
    sso = S.sb("sso", [128, 2])
    ident = c["ident"]; ct = c["t"]
    SCALE = 64 ** -0.5

    for g in range(NG):
        h = hT[0]
        for tt in range(4):
            x_ = xt[0]
            b.dma(x_.t[:], xb.t[g * 512 + tt * 128: g * 512 + (tt + 1) * 128, :], R=[xb], W=[x_])
            norm_tile2(b, c, x_, xn, ssx, scr, ab, 0, [h.t], [h], tt * 128)
        for j in range(5):
            bk = b.bank()
            for k in range(8):
                b.mm(bk, bk.t[:, 0:512], wcm.t[:, k, j * 128:(j + 1) * 128], h.t[:, k, :], R=[wcm, h], start=(k == 0), stop=(k == 7))
            if j < 3:
                b.act(pre[j].t[:, 3:515], bk.t[:, 0:512], AF.Copy, R=[bk], W=[pre[j]])
                y = cv[j]
                b.ts(y.t[:], pre[j].t[:, 0:512], convw(j, 0), None, ALU.mult, None, R=[pre[j], small], W=[y])
                for tap in range(1, 4):
                    b.stt(y.t[:], pre[j].t[:, tap:tap + 512], convw(j, tap), y.t[:], ALU.mult, ALU.add, R=[pre[j], small, y], W=[y])
                b.cp(pre[j].t[:, 0:3], pre[j].t[:, 512:515], R=[pre[j]], W=[pre[j]])
                b.act(y.t[:], y.t[:], AF.Silu, R=[y], W=[y])
                if j < 2:
                    b.tt(sq.t[:], y.t[:], y.t[:], ALU.mult, R=[y], W=[sq])
                    bk2 = b.bank()
                    b.mm(bk2, bk2.t[:, 0:512], c["ones"], sq.t[:], R=[sq, ct])
                    b.act(rs.t[:], bk2.t[:, 0:512], AF.Ln, R=[bk2], W=[rs], bias=EPS)
                    b.act(rs.t[:], rs.t[:], AF.Exp, R=[rs], W=[rs], scale=-0.5, bias=(float(np.log(128 ** -0.5)) if j == 0 else 0.0))
                    b.tt(y.t[:], y.t[:], rs.t[:], ALU.mult, R=[y, rs], W=[y])
            else:
                dst = qT if j == 3 else kT
                gcolm = small.t[:, 14:15] if j == 3 else small.t[:, 15:16]
                b.act(qk_tmp.t[:], bk.t[:, 0:512], AF.Copy, R=[bk], W=[qk_tmp])
                b.tt(sq.t[:], qk_tmp.t[:], qk_tmp.t[:], ALU.mult, R=[qk_tmp], W=[sq])
                bk2 = b.bank()
                b.mm(bk2, bk2.t[:, 0:512], c["bd"], sq.t[:], R=[sq, ct])
                b.act(rs.t[:], bk2.t[:, 0:512], AF.Ln, R=[bk2], W=[rs], scale=1.0 / 64, bias=EPS)
                b.act(rs.t[:], rs.t[:], AF.Exp, R=[rs], W=[rs], scale=-0.5)
                b.stt(dst.t[:, g * 512:(g + 1) * 512], qk_tmp.t[:], gcolm, rs.t[:], ALU.mult, ALU.mult, R=[qk_tmp, small, rs], W=[dst.sub(g)])
        for tt in range(4):
            bk = b.bank()
            for k in range(8):
                b.mm(bk, bk.t[:, 0:128], h.t[:, k, tt * 128:(tt + 1) * 128], wvb.t[:, k, :], R=[wvb, h], start=(k == 0), stop=(k == 7))
            b.cp(Vx.t[:, g * 4 + tt, 0:128], bk.t[:, 0:128], R=[bk], W=[Vx.sub(g)])
        qn, kn, vs = cv[0], cv[1], cv[2]
        def chunk_pre(ch):
            p = ch % 2
            t0 = ch * 64
            kTc = kn.t[:, t0:t0 + 64]; qTc = qn.t[:, t0:t0 + 64]
            bk = b.bank()
            for k in range(8):
                b.mm(bk, bk.t[0:64, 0:130], h.t[:, k, t0:t0 + 64], wzbd.t[:, k, :], R=[wzbd, h], start=(k == 0), stop=(k == 7))
            z = zb[p]
            b.cp(z.t[:, 0:130], bk.t[0:64, 0:130], R=[bk], W=[z])
            yield
            gc = gcol[p]
            b.act(gc.t[:, 0:1], z.t[:, 128:129], AF.Sigmoid, R=[z], W=[gc])
            yield
            b.act(gc.t[:, 1:2], z.t[:, 129:130], AF.Exp, R=[z, small], W=[gc], bias=small.t[0:64, 13:14])
            yield
            b.act(gc.t[:, 1:2], gc.t[:, 1:2], AF.Ln, R=[gc], W=[gc], bias=1.0)
            yield
            b.tt(gc.t[:, 1:2], gc.t[:, 1:2], negA.t[0:64, 0:1], ALU.mult, R=[gc, negA], W=[gc])
            yield
            bk = b.bank()
            b.mm(bk, bk.t[0:64, 0:1], c["UT"], gc.t[:, 1:2], R=[gc, ct])
            yield
            b.cp(gc.t[:, 2:3], bk.t[0:64, 0:1], R=[bk], W=[gc])
            yield
            t64 = tmp64[p]
            b.ts(t64.t[:], c["UT"], gc.t[:, 1:2], None, ALU.mult, None, R=[gc, ct], W=[t64])
            yield
            bk = b.bank()
            b.mm(bk, bk.t[0:64, 0:64], c["ones"][0:64, 0:64], t64.t[:], R=[t64, ct])
            yield
            E = e1[p]
            b.ts(E.t[:], bk.t[0:64, 0:64], gc.t[:, 2:3], None, ALU.subtract, None, R=[bk, gc], W=[E])
            yield
            Dt = dts[p]; Dc = dcs[p]
            b.ts(Dt.t[:], E.t[:], 0.0, None, ALU.min, None, R=[E], W=[Dt])
            yield
            b.act(Dt.t[:], Dt.t[:], AF.Exp, R=[Dt], W=[Dt])
            yield
            b.tt(Dt.t[:], Dt.t[:], c["UT"], ALU.mult, R=[Dt, ct], W=[Dt])
            yield
            b.ts(Dc.t[:], E.t[:], -1.0, 0.0, ALU.mult, ALU.min, R=[E], W=[Dc])
            yield
            b.act(Dc.t[:], Dc.t[:], AF.Exp, R=[Dc], W=[Dc])
            yield
            b.tt(Dc.t[:], Dc.t[:], c["LTs"], ALU.mult, R=[Dc, ct], W=[Dc])
            yield
            bk = b.bank()
            b.mm(bk, bk.t[0:64, 0:64], kTc, kTc, R=[kn])
            yield
            M = Mb[0][p]; N = Nb[0][p]; T = Tb[0][p]
            b.stt(M.t[:], bk.t[0:64, 0:64], gc.t[:, 0:1], Dc.t[:], ALU.mult, ALU.mult, R=[bk, gc, Dc], W=[M])
            yield
            bk = b.bank()
            b.tp(bk, bk.t[0:64, 0:64], M.t[:], ident[0:64, 0:64], R=[M, ct])
            yield
            b.cp(N.t[:], bk.t[0:64, 0:64], R=[bk], W=[N])
            yield
            b.tt(T.t[:], ident[0:64, 0:64], N.t[:], ALU.subtract, R=[N, ct], W=[T])
            yield
            for s in range(1, 6):
                M2 = Mb[s % 2][p]; N2 = Nb[s % 2][p]; T2 = Tb[s % 2][p]
                bk = b.bank()
                b.mm(bk, bk.t[0:64, 0:64], N.t[:], M.t[:], R=[N, M])
                yield
                b.act(M2.t[:], bk.t[0:64, 0:64], AF.Copy, R=[bk], W=[M2])
                yield
                if s < 5:
                    bk = b.bank()
                    b.mm(bk, bk.t[0:64, 0:64], M.t[:], N.t[:], R=[N, M])
                    yield
                    b.cp(N2.t[:], bk.t[0:64, 0:64], R=[bk], W=[N2])
                    yield
                bk = b.bank()
                b.mm(bk, bk.t[0:64, 0:64], M2.t[:], T.t[:], R=[M2, T])
                yield
                b.tt(T2.t[:], bk.t[0:64, 0:64], T.t[:], ALU.add, R=[bk, T], W=[T2])
                yield
                M, N, T = M2, N2, T2
            bk = b.bank()
            b.tp(bk, bk.t[0:64, 0:128], kTc, ident, R=[kn, ct])
            yield
            b.tp(bk, bk.t[0:64, 128:256], vs.t[:, t0:t0 + 64], ident, R=[vs, ct])
            yield
            b.act(gc.t[:, 3:4], gc.t[:, 2:3], AF.Exp, R=[gc], W=[gc])
            yield
            b.tt(gc.t[:, 4:5], gc.t[:, 3:4], gc.t[:, 0:1], ALU.mult, R=[gc], W=[gc])
            yield
            b.cp(ktm[p].t[:], bk.t[0:64, 0:128], R=[bk], W=[ktm[p]])
            yield
            b.ts(kbt[p].t[:], bk.t[0:64, 0:128], gc.t[:, 4:5], None, ALU.mult, None, R=[bk, gc], W=[kbt[p]])
            yield
            b.ts(vbt[p].t[:], bk.t[0:64, 128:256], gc.t[:, 0:1], None, ALU.mult, None, R=[bk, gc], W=[vbt[p]])
            yield
            bk = b.bank()
            b.mm(bk, bk.t[0:64, 0:128], T.t[:], vbt[p].t[:], R=[T, vbt[p]])
            yield
            b.cp(Ub[p].t[:], bk.t[0:64, 0:128], R=[bk], W=[Ub[p]])
            yield
            bk = b.bank()
            b.mm(bk, bk.t[:, 0:64], kbt[p].t[:], T.t[:], R=[T, kbt[p]])
            yield
            b.act(WTb[p].t[:], bk.t[:, 0:64], AF.Copy, R=[bk], W=[WTb[p]])
            yield
            bk = b.bank()
            b.mm(bk, bk.t[0:64, 0:64], kTc, qTc, R=[kn, qn])
            yield
            b.tt(AqT[p].t[:], bk.t[0:64, 0:64], Dt.t[:], ALU.mult, R=[bk, Dt], W=[AqT[p]])
            yield
            bk = b.bank()
            b.mm(bk, bk.t[:, 0:1], c["ones"][0:64, 0:128], gc.t[:, 1:2], R=[gc, ct])
            yield
            b.cp(gl[p].t[:, 0:1], bk.t[:, 0:1], R=[bk], W=[gl[p]])
            yield
            b.act(gl[p].t[:, 1:2], gl[p].t[:, 0:1], AF.Exp, R=[gl[p]], W=[gl[p]])
            yield
            b.act(gc.t[:, 5:6], gc.t[:, 2:3], AF.Exp, R=[gc, gl[p]], W=[gc], scale=-1.0, bias=gl[p].t[0:64, 0:1])
            yield
            b.ts(kdec[p].t[:], ktm[p].t[:], gc.t[:, 5:6], None, ALU.mult, None, R=[ktm[p], gc], W=[kdec[p]])
            yield
            yield

        def chunk_seq(ch):
            p = ch % 2
            t0 = ch * 64
            qTc = qn.t[:, t0:t0 + 64]
            z = zb[p]
            gc = gcol[p]
            bkv = b.bank()
            b.mm(bkv, bkv.t[0:64, 0:128], WTb[p].t[:], St.t[:], R=[WTb[p], St])
            b.tt(vnew[p].t[:], Ub[p].t[:], bkv.t[0:64, 0:128], ALU.subtract, R=[bkv, Ub[p]], W=[vnew[p]])
            bk1 = b.bank()
            b.mm(bk1, bk1.t[0:64, 0:128], qTc, St.t[:], R=[qn, St])
            b.act(o1[p].t[:], bk1.t[0:64, 0:128], AF.Copy, R=[bk1, gc], W=[o1[p]], scale=gc.t[:, 3:4])
            bk2 = b.bank()
            b.mm(bk2, bk2.t[0:64, 0:128], AqT[p].t[:], vnew[p].t[:], R=[AqT[p], vnew[p]])
            b.tt(ob_[p].t[:], bk2.t[0:64, 0:128], o1[p].t[:], ALU.add, R=[bk2, o1[p]], W=[ob_[p]])
            bks = b.bank()
            b.mm(bks, bks.t[:, 0:128], kdec[p].t[:], vnew[p].t[:], R=[kdec[p], vnew[p]])
            b.stt(St.t[:], St.t[:], gl[p].t[:, 1:2], bks.t[:, 0:128], ALU.mult, ALU.add, R=[St, gl[p], bks], W=[St])
            b.act(szb[p].t[:], ob_[p].t[:], AF.Square, R=[ob_[p]], W=[szb[p], ssg[p]], accum_out=ssg[p].t[:, 0:1])
            b.rstd(ssg[p].t[:, 0:1], ssg[p].t[:, 0:1], R=[ssg[p]], W=[ssg[p]], scale=1.0 / 128)
            b.stt(ob_[p].t[:], ob_[p].t[:], ssg[p].t[:, 0:1], gng, ALU.mult, ALU.mult, R=[ob_[p], ssg[p], small], W=[ob_[p]])
            b.act(szb[p].t[:], z.t[:, 0:128], AF.Silu, R=[z], W=[szb[p]])
            b.tt(oast[p].t[:], ob_[p].t[:], szb[p].t[:], ALU.mult, R=[ob_[p], szb[p]], W=[oast[p]])
            sap, sbuf_ = send_at(g * 512 + t0, 64, 0, ("a", g, ch))
            b.dma(sap, oast[p].t[:], R=[oast[p]], W=[sbuf_], final=True)
        for pr in range(4):
            gens = [chunk_pre(2 * pr), chunk_pre(2 * pr + 1)]
            while gens:
                for gnr in list(gens):
                    try:
                        next(gnr)
                    except StopIteration:
                        gens.remove(gnr)
            chunk_seq(2 * pr)
            chunk_seq(2 * pr + 1)
        for comp in range(2):
            lo, hi = comp * 64, (comp + 1) * 64
            nkb = 4 * g + 4
            for kb in range(nkb):
                rel = kb - 4 * g
                qlo = max(rel, 0) * 128
                ps = b.banks[kb % 2]
                b.mm(ps, ps.t[:, qlo:512], kT.t[lo:hi, kb * 128:(kb + 1) * 128], qT.t[lo:hi, g * 512 + qlo:(g + 1) * 512],
                     R=[kT.sub(kb // 4), qT.sub(g)])
                P = PT[kb % 2]
                for qs in range(max(rel, 0), 4):
                    r_ = kb - (4 * g + qs) + 63
                    b.act(P.t[:, qs * 128:(qs + 1) * 128], ps.t[:, qs * 128:(qs + 1) * 128], AF.Exp, R=[ps, ct], W=[P], scale=SCALE,
                          bias=c["brel"][:, r_:r_ + 1])
                if rel >= 0:
                    b.tt(P.t[:, rel * 128:(rel + 1) * 128], P.t[:, rel * 128:(rel + 1) * 128], c["Cm"], ALU.mult, R=[P, ct], W=[P])
                for qs in range(max(rel, 0), 4):
                    po = b.banks[4 + qs]
                    b.mm(po, po.t[:, 0:130], P.t[:, qs * 128:(qs + 1) * 128], Vx.t[:, kb, 0:130], R=[P, Vx.sub(kb // 4)],
                         start=(kb == 0), stop=(kb == 4 * g + qs))
            for qs in range(4):
                po = b.banks[4 + qs]
                rcol = rz.t[:, comp * 4 + qs: comp * 4 + qs + 1]
                b.S.op("dve", lambda e, rcol=rcol, po=po: e.reciprocal(out=rcol, in_=po.t[:, 128:129]), R=[po], W=[rz])
                if comp == 0:
                    b.ts(obacc[qs].t[:], po.t[:, 0:128], rcol, None, ALU.mult, None, R=[po, rz], W=[obacc[qs]])
                else:
                    b.tt(rcol, rcol, neglam.t[:, 0:1], ALU.mult, R=[rz, neglam], W=[rz])
                    b.stt(obacc[qs].t[:], po.t[:, 0:128], rcol, obacc[qs].t[:], ALU.mult, ALU.add, R=[po, rz, obacc[qs]], W=[obacc[qs]])
                    o_ = obst[qs % 2]
                    b.act(o_.t[:], obacc[qs].t[:], AF.Square, R=[obacc[qs]], W=[o_, sso], accum_out=sso.t[:, 0:1])
                    b.rstd(sso.t[:, 0:1], sso.t[:, 0:1], R=[sso], W=[sso], scale=1.0 / 128)
                    b.stt(o_.t[:], obacc[qs].t[:], sso.t[:, 0:1], sub_g.t[:], ALU.mult, ALU.mult, R=[obacc[qs], sso, sub_g], W=[o_])
                    sap, sbuf_ = send_at(g * 512 + qs * 128, 128, 128, ("b", g, qs))
                    b.dma(sap, o_.t[:], R=[o_], W=[sbuf_], final=True)
        if after_group is not None:
            after_group(g)


def _cols(v):
    return np.ascontiguousarray(np.asarray(v, np.float32).reshape(-1, 128).T)


def alibi_slope(h):
    return float(2.0 ** (-8.0 * (h + 1) / 4))


def prep_common(inp, bidx):
    return {
        "c_col": _cols(inp["c"][bidx]),
        "w_ada": np.ascontiguousarray(inp["w_ada"][0]),
        "b_ada": np.ascontiguousarray(inp["b_ada"][0].reshape(1, -1)),
        "g1c": _cols(inp["norm1_g"][0]),
        "g2c": _cols(inp["norm2_g"][0]),
    }


def prep_ab(inp, bidx, h):
    w_in = inp["w_in"][0]
    sl = lambda name, width=128: w_in[:, W_OFF[name] + h * width: W_OFF[name] + (h + 1) * width]
    m = prep_common(inp, bidx)
    m["consts"] = host_consts(alibi_slope(h))
    m["xb"] = np.ascontiguousarray(inp["x"][bidx])
    m["w_cm"] = np.ascontiguousarray(np.concatenate([sl("qA"), sl("kA"), sl("vA"), sl("qB"), sl("kB")], axis=1))
    m["w_zbd"] = np.ascontiguousarray(np.concatenate([sl("zA"), sl("beta", 1), sl("decay", 1)], axis=1))
    m["w_vb"] = np.ascontiguousarray(sl("vB"))
    small = np.zeros((128, 384), np.float32)
    cw = inp["conv_w"][0]
    for j in range(3):
        small[:, j * 4:(j + 1) * 4] = cw[:, j * 512 + h * 128: j * 512 + (h + 1) * 128].T
    small[:, 12] = inp["a_log"][0, h]
    small[:, 13] = inp["dt_bias"][0, h]
    small[:, 14] = np.tile(inp["q_norm_g"][0], 2)
    small[:, 15] = np.tile(inp["k_norm_g"][0], 2)
    small[:, 128:256] = np.tile(inp["gdn_norm_g"][0][None, :], (128, 1))
    small[:, 256:384] = np.tile(inp["subln_g"][0][None, :], (128, 1))
    m["small"] = small
    m["lamrow"] = np.ascontiguousarray(np.concatenate(
        [inp["lambda_q1"][0], inp["lambda_k1"][0], inp["lambda_q2"][0], inp["lambda_k2"][0]]).reshape(1, 256))
    return m


def build_d(Sq, n_exp=257):
    nc = bass.Bass("TRN2", target_bir_lowering=False)
    b = Bld(nc)
    c = build_consts(b)
    ad = emit_adaln(b, c, want="gates")
    emit_d(b, c, ad, Sq, None, n_exp)
    b.S.emit()
    return nc


def build_fused(Sq, n_exp=257):
    nc = bass.Bass("TRN2", target_bir_lowering=False)
    b = Bld(nc)
    S = b.S
    c = build_consts(b)
    ad = emit_adaln(b, c, want="gates")
    CH = min(1024, Sq)
    NCH = Sq // CH
    sends = [S.dram(nc.dram_tensor(f"send_bounce{i}", [CH, 256], F32).ap(), f"send{i}") for i in range(NCH)]
    gaths = [S.dram(nc.dram_tensor(f"gath_bounce{i}", [4 * CH, 256], F32).ap(), f"gath{i}") for i in range(NCH)]

    def send_at(r0, n, c0, key):
        ch, lr = r0 // CH, r0 % CH
        return sends[ch].t[lr:lr + n, c0:c0 + 128], sends[ch].sub(key)

    def after_group(g):
        if ((g + 1) * 512) % CH == 0:
            ch = ((g + 1) * 512) // CH - 1
            sd, gt = sends[ch], gaths[ch]
            S.cc(lambda e: e.collective_compute("AllGather", ALU.bypass, replica_groups=[[0, 1, 2, 3], [4, 5, 6, 7]],
                                                ins=[sd.t.opt()], outs=[gt.t.opt()]),
                 R=[sd] + list(sd.subs.values()), W=[gt])

    emit_ab(b, c, ad["ab"], Sq, send_at, after_group)
    emit_d(b, c, ad, Sq, (gaths, CH), n_exp)
    S.emit()
    return nc


def emit_d(b, c, ad, Sq, gath, n_exp):
    S = b.S
    nc = b.nc
    T = Sq // 4
    NT = T // 128
    MG = min(512, T)
    NTG = T // MG
    TPG = MG // 128
    ab, g1bc, g2bc = ad["ab"], ad["g1bc"], ad["g2bc"]
    ident = c["ident"]; ct = c["t"]
    xs = b.inp("xs", [T, 1024])
    if gath is None:
        oa_in = b.inp("oa_in", [T, 512])
        ob_in = b.inp("ob_in", [T, 512])
    else:
        sel_d = b.inp("sel", [128, 4])
    wg_d = b.inp("w_gates", [1024, 2048])
    wog_d = b.inp("w_o_gdn", [512, 1024])
    wod_d = b.inp("w_o_diff", [512, 1024])
    wout_d = b.inp("w_out", [1024, 1024])
    wr_d = b.inp("w_router", [1024, 256])
    rb_d = b.inp("rbias", [128, 256])
    wgu_d = b.inp("w_gu", [n_exp, 1024, 512])
    wdn_d = b.inp("w_dn", [n_exp, 256, 1024])
    outd = b.outp("out", [T, 1024])

    acc = S.sb("acc", [128, NT, 1024])
    h2b = S.sb("h2T_bf", [128, 8, T], BF16)
    wt = S.sb("wt_all", [128, NT, n_exp + 1])
    b.memset(wt.t[:, :, 256:n_exp + 1], 1.0, W=[wt])
    with S.scope():
        wr = S.sb("wr", [128, 8, 256]); rb = S.sb("rb", [128, 256])
        b.dma(wr.t[:], wr_d.t[:, :].rearrange("(k p) c -> p k c", p=128), R=[wr_d], W=[wr])
        b.dma(rb.t[:], rb_d.t[:, :], R=[rb_d], W=[rb])
        wgb = [S.sb(f"wgb{i}", [128, 8, 512]) for i in range(2)]
        xt = [S.sb(f"dxt{i}", [128, 1024]) for i in range(1)]
        xn = S.sb("dxn", [128, 1024]); ssx = S.sb("dssx", [128, 1])
        hT1 = S.sb("hT1", [128, 8, 128]); h2f = hT1
        sg = S.sb("sg", [128, 2048])
        if gath is None:
            oat = S.sb("oat", [128, 512]); obt = S.sb("obt", [128, 512])
        else:
            cand = [S.sb(f"cand{i}", [128, 4, 256]) for i in range(2)]
            cmb = S.sb("cmb", [128, 4, 256])
            sel = S.sb("sel_sb", [128, 4])
            b.dma(sel.t[:], sel_d.t[:, :], R=[sel_d], W=[sel])
            gaths, CH = gath
        oaT = S.sb("oaT", [128, 4, 128]); obT = S.sb("obT", [128, 4, 128])
        mg = S.sb("merged", [128, 1024]); tmp = S.sb("dtmp", [128, 512]); mT = hT1
        scr = mg
        class _V:
            def __init__(self, tn, lo):
                self.t = tn.t[:, lo:lo + 256]; self.b = tn.b
        sc_ = _V(sg, 0); ch = _V(sg, 256); mc = _V(sg, 512)
        m8 = S.sb("r_m8", [128, 8]); gs = S.sb("r_gs", [128, 8]); gm = S.sb("r_gm", [128, 8]); pen = S.sb("r_pen", [128, 8])
        rsum = S.sb("r_sum", [128, 2])
        gi = 0
        for tt in range(NT):
            x_ = xt[0]
            b.dma(x_.t[:], xs.t[tt * 128:(tt + 1) * 128, :], R=[xs], W=[x_])
            if gath is None:
                b.dma(oat.t[:], oa_in.t[tt * 128:(tt + 1) * 128, :], R=[oa_in], W=[oat])
                b.dma(obt.t[:], ob_in.t[tt * 128:(tt + 1) * 128, :], R=[ob_in], W=[obt])
                srcs = ((oat, lambda cc: oat.t[:, cc * 128:(cc + 1) * 128]), (obt, lambda cc: obt.t[:, cc * 128:(cc + 1) * 128]))
            else:
                cf = lambda t_: t_.t[:].rearrange("p a b -> p (a b)")
                for hp in range(4):
                    cd = cand[hp % 2]
                    gtok = hp * T + tt * 128
                    gsrc = gaths[gtok // CH]
                    lr = gtok % CH
                    b.dma(cd.t[:], gsrc.t.rearrange("(i s) c -> s i c", i=4)[lr:lr + 128, :, :], R=[gsrc], W=[cd])
                    if hp == 0:
                        b.ts(cf(cmb), cf(cd), sel.t[:, 0:1], None, ALU.mult, None, R=[cd, sel], W=[cmb])
                    else:
                        b.stt(cf(cmb), cf(cd), sel.t[:, hp:hp + 1], cf(cmb), ALU.mult, ALU.add, R=[cd, sel, cmb], W=[cmb])
                srcs = ((cmb, lambda cc: cmb.t[:, cc, 0:128]), (cmb, lambda cc: cmb.t[:, cc, 128:256]))
            norm_tile2(b, c, x_, xn, ssx, scr, ab, 0, [hT1.t], [hT1], 0)
            for cbk in range(4):
                w = wgb[gi % 2]; gi += 1
                b.dma(w.t[:], wg_d.t[:, cbk * 512:(cbk + 1) * 512].rearrange("(k p) c -> p k c", p=128), R=[wg_d], W=[w])
                bk = b.bank()
                for k in range(8):
                    b.mm(bk, bk.t[:, 0:512], hT1.t[:, k, :], w.t[:, k, :], R=[hT1, w], start=(k == 0), stop=(k == 7))
                b.act(sg.t[:, cbk * 512:(cbk + 1) * 512], bk.t[:, 0:512], AF.Sigmoid, R=[bk], W=[sg])
            for ((src, view), dst) in zip(srcs, (oaT, obT)):
                bk = b.bank()
                for cc in range(4):
                    b.tp(bk, bk.t[:, cc * 128:(cc + 1) * 128], view(cc), ident, R=[src, ct])
                b.cp(dst.t[:].rearrange("p a b -> p (a b)"), bk.t[:, 0:512], R=[bk], W=[dst])
            wogT = wgb[gi % 2]; gi += 1
            b.dma(wogT.t[:].rearrange("p k c -> p (k c)").rearrange("p (a f) -> p a f", a=4), wog_d.t[:, :].rearrange("(k p) c -> p k c", p=128), R=[wog_d], W=[wogT])
            wodT = wgb[gi % 2]; gi += 1
            b.dma(wodT.t[:].rearrange("p k c -> p (k c)").rearrange("p (a f) -> p a f", a=4), wod_d.t[:, :].rearrange("(k p) c -> p k c", p=128), R=[wod_d], W=[wodT])
            wog_v = wogT.t[:].rearrange("p k c -> p (k c)").rearrange("p (a f) -> p a f", a=4)
            wod_v = wodT.t[:].rearrange("p k c -> p (k c)").rearrange("p (a f) -> p a f", a=4)
            for hf in range(2):
                bk = b.bank()
                for cc in range(4):
                    b.mm(bk, bk.t[:, 0:512], oaT.t[:, cc, :], wog_v[:, cc, hf * 512:(hf + 1) * 512], R=[oaT, wogT], start=(cc == 0), stop=(cc == 3))
                b.tt(mg.t[:, hf * 512:(hf + 1) * 512], bk.t[:, 0:512], sg.t[:, hf * 512:(hf + 1) * 512], ALU.mult, R=[bk, sg], W=[mg])
                bk = b.bank()
                for cc in range(4):
                    b.mm(bk, bk.t[:, 0:512], obT.t[:, cc, :], wod_v[:, cc, hf * 512:(hf + 1) * 512], R=[obT, wodT], start=(cc == 0), stop=(cc == 3))
                b.tt(tmp.t[:], bk.t[:, 0:512], sg.t[:, 1024 + hf * 512:1024 + (hf + 1) * 512], ALU.mult, R=[bk, sg], W=[tmp])
                b.tt(mg.t[:, hf * 512:(hf + 1) * 512], mg.t[:, hf * 512:(hf + 1) * 512], tmp.t[:], ALU.add, R=[mg, tmp], W=[mg])
            for k2 in range(2):
                bk = b.bank()
                for kk in range(4):
                    k = k2 * 4 + kk
                    b.tp(bk, bk.t[:, kk * 128:(kk + 1) * 128], mg.t[:, k * 128:(k + 1) * 128], ident, R=[mg, ct])
                b.cp(mT.t[:, k2 * 4:(k2 + 1) * 4, :].rearrange("p a b -> p (a b)"), bk.t[:, 0:512], R=[bk], W=[mT])
            for hf in range(2):
                wo_ = wgb[gi % 2]; gi += 1
                b.dma(wo_.t[:], wout_d.t[:, hf * 512:(hf + 1) * 512].rearrange("(k p) c -> p k c", p=128), R=[wout_d], W=[wo_])
                bk = b.bank()
                for k in range(8):
                    b.mm(bk, bk.t[:, 0:512], mT.t[:, k, :], wo_.t[:, k, :], R=[mT, wo_], start=(k == 0), stop=(k == 7))
                b.tt(tmp.t[:], bk.t[:, 0:512], g1bc.t[:, hf * 512:(hf + 1) * 512], ALU.mult, R=[bk, g1bc], W=[tmp])
                b.tt(acc.t[:, tt, hf * 512:(hf + 1) * 512], tmp.t[:], x_.t[:, hf * 512:(hf + 1) * 512], ALU.add, R=[tmp, x_], W=[acc.sub(tt)])
            norm_tile2(b, c, acc.sub(tt), xn, ssx, scr, ab, 16, [h2f.t, h2b.t], [h2f, h2b.sub(tt // TPG)], 0, xap=acc.t[:, tt, :], cols=[0, tt * 128])
            bk = b.bank()
            for k in range(8):
                b.mm(bk, bk.t[:, 0:256], h2f.t[:, k, :], wr.t[:, k, :], R=[h2f, wr], start=(k == 0), stop=(k == 7))
            b.act(sc_.t[:], bk.t[:, 0:256], AF.Sigmoid, R=[bk], W=[sc_])
            b.tt(ch.t[:], sc_.t[:], rb.t[:], ALU.add, R=[sc_, rb], W=[ch])
            for g in range(8):
                b.S.op("dve", lambda e, g=g: e.max(out=m8.t[:], in_=ch.t[:, g * 32:(g + 1) * 32]), R=[ch], W=[m8])
                b.tt(gs.t[:, g:g + 1], m8.t[:, 0:1], m8.t[:, 1:2], ALU.add, R=[m8], W=[gs])
            b.S.op("dve", lambda e: e.max(out=m8.t[:], in_=gs.t[:]), R=[gs], W=[m8])
            b.ts(gm.t[:], gs.t[:], m8.t[:, 3:4], None, ALU.is_ge, None, R=[gs, m8], W=[gm])
            b.ts(pen.t[:], gm.t[:], -1.0, 1e9, ALU.add, ALU.mult, R=[gm], W=[pen])
            for g in range(8):
                b.ts(mc.t[:, g * 32:(g + 1) * 32], ch.t[:, g * 32:(g + 1) * 32], gm.t[:, g:g + 1], pen.t[:, g:g + 1], ALU.mult, ALU.add,
                     R=[ch, gm, pen], W=[mc])
            b.S.op("dve", lambda e: e.max(out=m8.t[:], in_=mc.t[:]), R=[mc], W=[m8])
            b.ts(mc.t[:], mc.t[:], m8.t[:, 7:8], None, ALU.is_ge, None, R=[mc, m8], W=[mc])
            b.tt(mc.t[:], mc.t[:], sc_.t[:], ALU.mult, R=[mc, sc_], W=[mc])
            b.S.op("dve", lambda e: e.tensor_reduce(out=rsum.t[:, 0:1], in_=mc.t[:], axis=AX.X, op=ALU.add), R=[mc], W=[rsum])
            b.S.op("dve", lambda e: e.reciprocal(out=rsum.t[:, 1:2], in_=rsum.t[:, 0:1]), R=[rsum], W=[rsum])
            b.ts(wt.t[:, tt, 0:256], mc.t[:], rsum.t[:, 1:2], 2.5, ALU.mult, ALU.mult, R=[mc, rsum], W=[wt])
    with S.scope():
        wgu_st = [S.sb(f"wgu_st{i}", [128, 8, 512]) for i in range(2)]
        wd_st = [S.sb(f"wd_st{i}", [128, 2, 1024]) for i in range(2)]
        wgu_bf = [S.sb(f"wgu_bf{i}", [128, 8, 512], BF16) for i in range(2)]
        wd_bf = [S.sb(f"wd_bf{i}", [128, 2, 1024], BF16) for i in range(2)]
        AT = [S.sb(f"AT{i}", [128, 2, MG], BF16) for i in range(2)]
        slu = [S.sb(f"slu{i}", [128, MG]) for i in range(2)]
        for e in range(n_exp):
            p = e % 2
            b.dma(wgu_st[p].t[:], wgu_d.t[e].rearrange("(k p) c -> p k c", p=128), R=[wgu_d], W=[wgu_st[p]])
            b.dma(wd_st[p].t[:], wdn_d.t[e].rearrange("(k p) c -> p k c", p=128), R=[wdn_d], W=[wd_st[p]])
            b.cp(wgu_bf[p].t[:, 0:4, :], wgu_st[p].t[:, 0:4, :], R=[wgu_st[p]], W=[wgu_bf[p]], eng="pool")
            b.cp(wgu_bf[p].t[:, 4:8, :], wgu_st[p].t[:, 4:8, :], R=[wgu_st[p]], W=[wgu_bf[p]], eng="pool")
            for c2 in range(2):
                b.tt(wd_bf[p].t[:, c2, :], wd_st[p].t[:, c2, :], g2bc.t[:], ALU.mult, R=[wd_st[p], g2bc], W=[wd_bf[p]], eng="pool")
            ai = 0
            for tg in range(NTG):
                bks = []
                for fc in range(4):
                    bk = b.bank()
                    bks.append(bk)
                    for k in range(8):
                        b.mm(bk, bk.t[:, 0:MG], wgu_bf[p].t[:, k, fc * 128:(fc + 1) * 128], h2b.t[:, k, tg * MG:(tg + 1) * MG],
                             R=[wgu_bf[p], h2b.sub(tg)], start=(k == 0), stop=(k == 7))
                A = AT[tg % 2]
                for c2 in range(2):
                    sl = slu[c2]
                    b.act(sl.t[:], bks[c2].t[:, 0:MG], AF.Silu, R=[bks[c2]], W=[sl])
                    b.tt(A.t[:, c2, :], sl.t[:], bks[2 + c2].t[:, 0:MG], ALU.mult, R=[sl, bks[2 + c2]], W=[A])
                for ti in range(TPG):
                    tile_ = tg * TPG + ti
                    for hf in range(2):
                        bk = b.bank()
                        for c2 in range(2):
                            b.mm(bk, bk.t[:, 0:512], A.t[:, c2, ti * 128:(ti + 1) * 128], wd_bf[p].t[:, c2, hf * 512:(hf + 1) * 512],
                                 R=[A, wd_bf[p]], start=(c2 == 0), stop=(c2 == 1))
                        b.stt(acc.t[:, tile_, hf * 512:(hf + 1) * 512], bk.t[:, 0:512], wt.t[:, tile_, e:e + 1],
                              acc.t[:, tile_, hf * 512:(hf + 1) * 512], ALU.mult, ALU.add, R=[bk, wt, acc.sub(tile_)], W=[acc.sub(tile_)])
        for tt in range(NT):
            b.dma(outd.t[tt * 128:(tt + 1) * 128, :], acc.t[:, tt, :], R=[acc.sub(tt)], W=[outd.sub(tt)], final=True)


def prep_d(inp, bidx, j, T, oa_b, ob_b):
    m = prep_common(inp, bidx)
    m["consts"] = host_consts(alibi_slope(0))
    w_in = inp["w_in"][0]
    m["xs"] = np.ascontiguousarray(inp["x"][bidx, j * T:(j + 1) * T])
    m["oa_in"] = np.ascontiguousarray(oa_b[j * T:(j + 1) * T])
    m["ob_in"] = np.ascontiguousarray(ob_b[j * T:(j + 1) * T])
    m["w_gates"] = np.ascontiguousarray(w_in[:, W_OFF["ga"]:W_OFF["ga"] + 2048])
    m["w_o_gdn"] = np.ascontiguousarray(inp["w_o_gdn"][0])
    m["w_o_diff"] = np.ascontiguousarray(inp["w_o_diff"][0])
    m["w_out"] = np.ascontiguousarray(inp["w_out"][0])
    m["w_router"] = np.ascontiguousarray(inp["w_router"][0])
    m["rbias"] = np.ascontiguousarray(np.tile(inp["router_bias"][0][None, :], (128, 1)))
    return m


_EXP_CACHE = {}


def expert_stack(inp):
    key = id(inp["w_exp_gate_up"])
    if key not in _EXP_CACHE:
        _EXP_CACHE.clear()
        wgu = np.concatenate([inp["w_exp_gate_up"][0], inp["w_shared_gate_up"][0][None]], axis=0)
        wdn = np.concatenate([inp["w_exp_down"][0], inp["w_shared_down"][0][None]], axis=0)
        _EXP_CACHE[key] = (np.ascontiguousarray(wgu), np.ascontiguousarray(wdn))
    return _EXP_CACHE[key]


def prep_fused(inp, cid, T):
    bi, j = cid // 4, cid % 4
    m = prep_ab(inp, bi, j)
    w_in = inp["w_in"][0]
    m["xs"] = np.ascontiguousarray(inp["x"][bi, j * T:(j + 1) * T])
    m["w_gates"] = np.ascontiguousarray(w_in[:, W_OFF["ga"]:W_OFF["ga"] + 2048])
    m["w_o_gdn"] = np.ascontiguousarray(inp["w_o_gdn"][0])
    m["w_o_diff"] = np.ascontiguousarray(inp["w_o_diff"][0])
    m["w_out"] = np.ascontiguousarray(inp["w_out"][0])
    m["w_router"] = np.ascontiguousarray(inp["w_router"][0])
    m["rbias"] = np.ascontiguousarray(np.tile(inp["router_bias"][0][None, :], (128, 1)))
    sel = np.zeros((128, 4), np.float32)
    sel[:, j] = 1.0
    m["sel"] = sel
    return m


def kernel(**inp):
    inp = {k: np.asarray(v) for k, v in inp.items()}
    B, Sq, _ = inp["x"].shape
    T = Sq // 4
    nc = build_fused(Sq)
    wgu, wdn = expert_stack(inp)
    maps = []
    for cid in range(8):
        m = prep_fused(inp, cid, T)
        m["w_gu"] = wgu
        m["w_dn"] = wdn
        maps.append(m)
    res = run_bass_kernel_spmd(nc, maps, core_ids=list(range(8)))
    out = np.zeros((B, Sq, 1024), np.float32)
    for cid in range(8):
        bi, j = cid // 4, cid % 4
        out[bi, j * T:(j + 1) * T] = res.results[cid]["out"]
    return out
```

```python
import contextlib
import numpy as np
import concourse.bass as bass
import concourse.mybir as mybir
from concourse.bass_utils import run_bass_kernel_spmd

F32 = mybir.dt.float32
BF16 = mybir.dt.bfloat16
I32 = mybir.dt.int32
AF = mybir.ActivationFunctionType
ALU = mybir.AluOpType


class Buf:
    __slots__ = ("name", "w", "r")

    def __init__(self, name):
        self.name = name
        self.w = None
        self.r = []


class Tn:
    def __init__(self, t, name):
        self.t = t
        self.name = name
        self.b = Buf(name)
        self.subs = {}

    def sub(self, key):
        if key not in self.subs:
            self.subs[key] = Buf(f"{self.name}:{key}")
        return self.subs[key]


class Op:
    __slots__ = ("eng", "fn", "deps", "is_dma", "sem", "val", "signals", "final", "inc")

    def __init__(self, eng, fn, is_dma=False):
        self.eng = eng
        self.fn = fn
        self.deps = []
        self.is_dma = is_dma
        self.sem = None
        self.val = None
        self.signals = False
        self.final = False
        self.inc = 1


def _buf(x):
    return x.b if hasattr(x, 'b') else x


class Sched:
    ENGS = ("pe", "act", "dve", "pool", "sp")

    def __init__(self, nc, n_dma_sems=None):
        self.nc = nc
        self.stack = contextlib.ExitStack()
        self.stacks = [self.stack]
        self.ops = {e: [] for e in self.ENGS}
        self.esem = {e: nc.alloc_semaphore(name=f"sem_{e}") for e in self.ENGS}
        nd = n_dma_sems or {"sp": 24, "pool": 12, "act": 4}
        self.dsem = {q: [nc.alloc_semaphore(name=f"dsem_{q}{i}") for i in range(n)] for q, n in nd.items()}
        self.dlast = {q: [None] * n for q, n in nd.items()}
        self.dcnt = {q: [0] * n for q, n in nd.items()}
        self.dnext = {q: 0 for q in nd}
        self.n_ops = 0

    def sb(self, name, shape, dtype=F32):
        t = self.stacks[-1].enter_context(self.nc.sbuf_tensor(name, list(shape), dtype))
        return Tn(t, name)

    @contextlib.contextmanager
    def scope(self):
        st = contextlib.ExitStack()
        self.stacks.append(st)
        try:
            yield
        finally:
            self.barrier()
            self.stacks.pop()
            st.close()

    def barrier(self):
        lasts = []
        for e in self.ENGS:
            for o in reversed(self.ops[e]):
                if o.fn is not None and not o.is_dma:
                    lasts.append(o)
                    break
        for q in self.dlast:
            lasts += [o for o in self.dlast[q] if o is not None]
        for e in self.ENGS:
            o = Op(e, None)
            o.sem = self.esem[e]
            o.deps = list(lasts)
            self.ops[e].append(o)

    def ps(self, name, shape, dtype=F32):
        t = self.stack.enter_context(self.nc.psum_tensor(name, list(shape), dtype))
        return Tn(t, name)

    def dram(self, ap, name):
        return Tn(ap, name)

    def scratch(self, name, shape, dtype=F32):
        t = self.nc.dram_tensor(name, list(shape), dtype, kind="Internal")
        return Tn(t.ap(), name)

    def _track(self, op, R, W):
        eng = op.eng
        deps = []

        def add(d, kind):
            if d is None:
                return
            if not d.is_dma and not op.is_dma and d.eng == eng:
                if eng == "pe":
                    return
            deps.append(d)

        Rb = [_buf(x) for x in R]
        Wb = [_buf(x) for x in W]
        for b in Rb:
            add(b.w, "raw")
        for b in Wb:
            add(b.w, "waw")
            for r in b.r:
                add(r, "war")
        for b in Rb:
            b.r.append(op)
        for b in Wb:
            b.w = op
            b.r = []
        seen = set()
        for d in deps:
            if id(d) not in seen and d is not op:
                seen.add(id(d))
                op.deps.append(d)

    def op(self, eng, fn, R=(), W=()):
        o = Op(eng, fn)
        o.sem = self.esem[eng]
        self._track(o, R, W)
        self.ops[eng].append(o)
        self.n_ops += 1
        return o

    def dma(self, q, out, in_, R=(), W=(), final=False, fn=None, **kw):
        if fn is None:
            fn = lambda e: e.dma_start(out=out, in_=in_, **kw)
        o = Op(q, fn, is_dma=True)
        o.inc = 16
        k = self.dnext[q]
        self.dnext[q] = (k + 1) % len(self.dsem[q])
        o.sem = self.dsem[q][k]
        self.dcnt[q][k] += 16
        o.val = self.dcnt[q][k]
        o.signals = True
        o.final = final
        self._track(o, R, W)
        prev = self.dlast[q][k]
        if prev is not None and prev not in o.deps:
            o.deps.append(prev)
        self.dlast[q][k] = o
        self.ops[q].append(o)
        self.n_ops += 1
        return o

    def cc(self, fn, R=(), W=()):
        if "cc" not in self.dsem:
            self.dsem["cc"] = [self.nc.alloc_semaphore(name="dsem_cc")]
            self.dlast["cc"] = [None]
            self.dcnt["cc"] = [0]
        o = Op("pool", fn, is_dma=True)
        o.inc = 1
        o.sem = self.dsem["cc"][0]
        self.dcnt["cc"][0] += 1
        o.val = self.dcnt["cc"][0]
        o.signals = True
        self._track(o, R, W)
        prev = self.dlast["cc"][0]
        if prev is not None and prev not in o.deps:
            o.deps.append(prev)
        self.dlast["cc"][0] = o
        self.ops["pool"].append(o)
        return o

    def emit(self):
        for e in self.ENGS:
            for o in self.ops[e]:
                for d in o.deps:
                    d.signals = True
        for e in self.ENGS:
            c = 0
            for o in self.ops[e]:
                if o.fn is None:
                    o.signals = False
                    o.val = c
                    continue
                if not o.is_dma and o.signals:
                    c += 1
                    o.val = c
        nc = self.nc

        def replay(ename):
            def run(e):
                waited = {}
                finals = []
                for o in self.ops[ename]:
                    need = {}
                    for d in o.deps:
                        k = id(d.sem)
                        if k not in need or need[k][1] < d.val:
                            need[k] = (d.sem, d.val)
                    for k, (sem, val) in need.items():
                        if waited.get(k, 0) >= val:
                            continue
                        e.wait_ge(sem, val)
                        waited[k] = val
                    if o.fn is None:
                        continue
                    ins = o.fn(e)
                    if o.signals:
                        ins.then_inc(o.sem, o.inc)
                    if o.final:
                        finals.append(o)
                for o in finals:
                    e.wait_ge(o.sem, o.val)
            return run

        with nc.Block() as block:
            block.sync(replay("sp"))
            block.tensor(replay("pe"))
            block.scalar(replay("act"))
            block.vector(replay("dve"))
            block.gpsimd(replay("pool"))
        self.stack.close()


AX = mybir.AxisListType
D = 1024
KC = 8
EPS = 1e-6
LAM_INIT = 0.2
W_OFF = dict(qA=0, kA=512, vA=1024, zA=1536, beta=2048, decay=2052, qB=2056, kB=2568, vB=3080, ga=3592, gb=4616)


class Bld:
    def __init__(self, nc):
        self.nc = nc
        self.S = Sched(nc)
        self.banks = [self.S.ps(f"bank{i}", [128, 512]) for i in range(8)]
        self.bi = 0
        self.bank_pool = list(range(8))
        self.ins = {}

    def inp(self, name, shape, dtype=F32):
        ap = self.nc.dram_tensor(name, list(shape), dtype, kind="ExternalInput").ap()
        t = self.S.dram(ap, name)
        self.ins[name] = t
        return t

    def outp(self, name, shape, dtype=F32):
        ap = self.nc.dram_tensor(name, list(shape), dtype, kind="ExternalOutput").ap()
        return self.S.dram(ap, name)

    def bank(self):
        b = self.banks[self.bank_pool[self.bi % len(self.bank_pool)]]
        self.bi += 1
        return b

    def mm(self, bank, out, lhsT, rhs, R, start=True, stop=True):
        self.S.op("pe", lambda e: e.matmul(out, lhsT, rhs, start=start, stop=stop), R=R, W=[bank])

    def tp(self, bank, out, in_, ident, R):
        self.S.op("pe", lambda e: e.transpose(out, in_, ident), R=R, W=[bank])

    def act(self, out, in_, func, R, W, **kw):
        self.S.op("act", lambda e: e.activation(out=out, in_=in_, func=func, **kw), R=R, W=W)

    def ts(self, out, in0, s1, s2, op0, op1, R, W, eng="dve"):
        if op1 is None:
            self.S.op(eng, lambda e: e.tensor_scalar(out=out, in0=in0, scalar1=s1, scalar2=None, op0=op0), R=R, W=W)
        else:
            self.S.op(eng, lambda e: e.tensor_scalar(out=out, in0=in0, scalar1=s1, scalar2=s2, op0=op0, op1=op1), R=R, W=W)

    def tt(self, out, in0, in1, op, R, W, eng="dve"):
        self.S.op(eng, lambda e: e.tensor_tensor(out=out, in0=in0, in1=in1, op=op), R=R, W=W)

    def stt(self, out, in0, scalar, in1, op0, op1, R, W, eng="dve"):
        self.S.op(eng, lambda e: e.scalar_tensor_tensor(out=out, in0=in0, scalar=scalar, in1=in1, op0=op0, op1=op1), R=R, W=W)

    def cp(self, out, in_, R, W, eng="dve"):
        self.S.op(eng, lambda e: e.tensor_copy(out=out, in_=in_), R=R, W=W)

    def memset(self, ap, val, W, eng="dve"):
        self.S.op(eng, lambda e: e.memset(ap, val), R=[], W=W)

    def dma(self, out, in_, R, W, q="sp", final=False):
        self.S.dma(q, out, in_, R=R, W=W, final=final)

    def rstd(self, out, in_, R, W, scale, eps=EPS, lnbias=0.0):
        self.act(out, in_, AF.Ln, R=R, W=W, scale=scale, bias=eps)
        self.act(out, out, AF.Exp, R=W, W=W, scale=-0.5, bias=lnbias)


def build_consts(b, slope=None):
    S = b.S
    c = {}
    cin = b.inp("consts", [128, 128 * 6])
    ct = S.sb("consts_sb", [128, 128 * 6])
    b.dma(ct.t[:], cin.t[:, :], R=[cin], W=[ct])
    c["t"] = ct
    c["ident"] = ct.t[:, 0:128]
    c["ones"] = ct.t[:, 128:256]
    c["bd"] = ct.t[:, 256:384]
    c["UT"] = ct.t[0:64, 384:448]
    c["LTs"] = ct.t[0:64, 448:512]
    c["Cm"] = ct.t[:, 512:640]
    c["brel"] = ct.t[:, 640:704]
    return c


def host_consts(slope):
    c = np.zeros((128, 768), np.float32)
    c[:, 0:128] = np.eye(128)
    c[:, 128:256] = 1.0
    c[0:64, 256:320] = 1.0
    c[64:128, 320:384] = 1.0
    p = np.arange(64)[:, None]
    f = np.arange(64)[None, :]
    c[0:64, 384:448] = (p <= f)
    c[0:64, 448:512] = (p > f)
    k = np.arange(128)[:, None]
    q = np.arange(128)[None, :]
    cm = np.where(k <= q, 1.0, np.where((k // 64) == (q // 64), np.exp(-2.0 * slope * (k - q)), 0.0))
    c[:, 512:640] = cm
    r = np.arange(64)[None, :]
    c[:, 640:704] = slope * (k + 128.0 * (r - 63) - 127.0)
    return c


def emit_adaln(b, c, want):
    S = b.S
    ccol = b.inp("c_col", [128, 8])
    wada = b.inp("w_ada", [1024, 6144])
    bada = b.inp("b_ada", [1, 6144])
    g1c = b.inp("g1c", [128, 8])
    g2c = b.inp("g2c", [128, 8])
    modc = S.sb("modc", [128, 48])
    ab = S.sb("ab", [128, 32])
    out = {"modc": modc, "ab": ab}
    if want == "gates":
        out["g1bc"] = S.sb("g1bc", [128, 1024])
        out["g2bc"] = S.sb("g2bc", [128, 1024])
    with S.scope():
        _emit_adaln_body(b, c, want, out, ccol, wada, bada, g1c, g2c)
    return out


def _emit_adaln_body(b, c, want, out, ccol, wada, bada, g1c, g2c):
    S = b.S
    modc = out["modc"]
    ab = out["ab"]
    sc = S.sb("sc", [128, 8])
    gt = S.sb("gcols", [128, 16])
    brow = S.sb("brow", [1, 6144])
    modrow = S.sb("modrow", [1, 6144])
    wb = [S.sb(f"wadab{i}", [128, 8, 512]) for i in range(2)]
    b.dma(sc.t[:], ccol.t[:, :], R=[ccol], W=[sc])
    b.dma(gt.t[:, 0:8], g1c.t[:, :], R=[g1c], W=[gt])
    b.dma(gt.t[:, 8:16], g2c.t[:, :], R=[g2c], W=[gt])
    b.dma(brow.t[:], bada.t[:, :], R=[bada], W=[brow])
    b.act(sc.t[:], sc.t[:], AF.Silu, R=[sc], W=[sc])
    for jb in range(12):
        w = wb[jb % 2]
        b.dma(w.t[:], wada.t[:, jb * 512:(jb + 1) * 512].rearrange("(k p) c -> p k c", p=128), R=[wada], W=[w])
        bk = b.bank()
        for k in range(8):
            b.mm(bk, bk.t[0:1, 0:512], sc.t[:, k:k + 1], w.t[:, k, :], R=[sc, w], start=(k == 0), stop=(k == 7))
        b.tt(modrow.t[0:1, jb * 512:(jb + 1) * 512], bk.t[0:1, 0:512], brow.t[0:1, jb * 512:(jb + 1) * 512], ALU.add,
             R=[bk, brow], W=[modrow])
    bk = b.bank()
    for j in range(48):
        b.mm(bk, bk.t[:, j:j + 1], modrow.t[0:1, j * 128:(j + 1) * 128], c["ones"][0:1, 0:1], R=[modrow, c["t"]])
    b.cp(modc.t[:], bk.t[:, 0:48], R=[bk], W=[modc])
    b.ts(ab.t[:, 0:8], modc.t[:, 8:16], 1.0, None, ALU.add, None, R=[modc], W=[ab])
    b.tt(ab.t[:, 0:8], ab.t[:, 0:8], gt.t[:, 0:8], ALU.mult, R=[ab, gt], W=[ab])
    b.cp(ab.t[:, 8:16], modc.t[:, 0:8], R=[modc], W=[ab])
    b.ts(ab.t[:, 16:24], modc.t[:, 32:40], 1.0, None, ALU.add, None, R=[modc], W=[ab])
    b.tt(ab.t[:, 16:24], ab.t[:, 16:24], gt.t[:, 8:16], ALU.mult, R=[ab, gt], W=[ab])
    b.cp(ab.t[:, 24:32], modc.t[:, 24:32], R=[modc], W=[ab])
    if want == "gates":
        g1bc = out["g1bc"]
        g2bc = out["g2bc"]
        for (dst, off) in ((g1bc, 2048), (g2bc, 5120)):
            for hf in range(2):
                bk = b.bank()
                b.mm(bk, bk.t[:, 0:512], c["ones"][0:1, 0:128], modrow.t[0:1, off + hf * 512: off + (hf + 1) * 512],
                     R=[modrow, c["t"]])
                b.cp(dst.t[:, hf * 512:(hf + 1) * 512], bk.t[:, 0:512], R=[bk], W=[dst])
    return out


def emit_norm_hT(b, c, xsrc, row0, ntiles, acol, bcol, hT, xt_bufs, ssq, hT_dtype_ap=None, x_keep=None):
    S = b.S
    for tt in range(ntiles):
        xt = xt_bufs[tt % len(xt_bufs)] if x_keep is None else x_keep[tt]
        b.dma(xt.t[:], xsrc.t[row0 + tt * 128: row0 + (tt + 1) * 128, :], R=[xsrc], W=[xt])
    return


def norm_tile(b, c, xt, xn, ss, acol, bcol, hT, col0, scratch):
    b.act(scratch.t[:], xt.t[:], AF.Square, R=[xt], W=[scratch, ss], accum_out=ss.t[:, 0:1])
    b.rstd(ss.t[:, 0:1], ss.t[:, 0:1], R=[ss], W=[ss], scale=1.0 / D)
    b.ts(xn.t[:], xt.t[:], ss.t[:, 0:1], None, ALU.mult, None, R=[xt, ss], W=[xn])
    for k2 in range(2):
        bk = b.bank()
        for kk in range(4):
            k = k2 * 4 + kk
            b.tp(bk, bk.t[:, kk * 128:(kk + 1) * 128], xn.t[:, k * 128:(k + 1) * 128], c["ident"], R=[xn, c["t"]])
        for kk in range(4):
            k = k2 * 4 + kk
            b.act(hT[:, k, col0:col0 + 128], bk.t[:, kk * 128:(kk + 1) * 128], AF.Identity, R=[bk, acol[1]], W=[acol[2]],
                  scale=acol[0][:, k:k + 1], bias=bcol[0][:, k:k + 1])


def norm_tile2(b, c, xt, xn, ss, scratch, ab, aoff, hT, hTt, col0, xap=None, cols=None):
    if xap is None:
        xap = xt.t[:]
    b.act(scratch.t[:], xap, AF.Square, R=[xt], W=[scratch, ss], accum_out=ss.t[:, 0:1])
    b.rstd(ss.t[:, 0:1], ss.t[:, 0:1], R=[ss], W=[ss], scale=1.0 / D)
    b.ts(xn.t[:], xap, ss.t[:, 0:1], None, ALU.mult, None, R=[xt, ss], W=[xn])
    for k2 in range(2):
        bk = b.bank()
        for kk in range(4):
            k = k2 * 4 + kk
            b.tp(bk, bk.t[:, kk * 128:(kk + 1) * 128], xn.t[:, k * 128:(k + 1) * 128], c["ident"], R=[xn, c["t"]])
        for kk in range(4):
            k = k2 * 4 + kk
            for di, (dst, dstT) in enumerate(zip(hT, hTt)):
                cc0 = col0 if cols is None else cols[di]
                b.act(dst[:, k, cc0:cc0 + 128], bk.t[:, kk * 128:(kk + 1) * 128], AF.Identity, R=[bk, ab], W=[dstT],
                      scale=ab.t[:, aoff + k:aoff + k + 1], bias=ab.t[:, aoff + 8 + k:aoff + 9 + k])


def build_ab(Sq):
    nc = bass.Bass("TRN2", target_bir_lowering=False)
    b = Bld(nc)
    c = build_consts(b)
    ad = emit_adaln(b, c, want="none")
    send = b.outp("send", [Sq, 256])
    emit_ab(b, c, ad["ab"], Sq, lambda r0, n, c0, key: (send.t[r0:r0 + n, c0:c0 + 128], send.sub(key)))
    b.S.emit()
    return nc


def emit_ab(b, c, ab, Sq, send_at, after_group=None):
    with b.S.scope():
        _emit_ab_body(b, c, ab, Sq, send_at, after_group)


def _emit_ab_body(b, c, ab, Sq, send_at, after_group):
    S = b.S
    NG = Sq // 512
    xb = b.inp("xb", [Sq, 1024])
    wcm_d = b.inp("w_cm", [1024, 640])
    wzbd_d = b.inp("w_zbd", [1024, 130])
    wvb_d = b.inp("w_vb", [1024, 128])
    small_d = b.inp("small", [128, 384])
    lam_d = b.inp("lamrow", [1, 256])

    wcm = S.sb("wcm", [128, 8, 640])
    wzbd = S.sb("wzbd", [128, 8, 130])
    wvb = S.sb("wvb", [128, 8, 128])
    small = S.sb("small_sb", [128, 384])
    lamr = S.sb("lamr", [1, 256])
    b.dma(wcm.t[:], wcm_d.t[:, :].rearrange("(k p) c -> p k c", p=128), R=[wcm_d], W=[wcm])
    b.dma(wzbd.t[:], wzbd_d.t[:, :].rearrange("(k p) c -> p k c", p=128), R=[wzbd_d], W=[wzbd])
    b.dma(wvb.t[:], wvb_d.t[:, :].rearrange("(k p) c -> p k c", p=128), R=[wvb_d], W=[wvb])
    b.dma(small.t[:], small_d.t[:, :], R=[small_d], W=[small])
    b.dma(lamr.t[:], lam_d.t[:, :], R=[lam_d], W=[lamr])
    convw = lambda j, tap: small.t[:, j * 4 + tap: j * 4 + tap + 1]
    gng = small.t[0:64, 128:256]
    sub_g = S.sb("sub_g", [128, 128])
    b.ts(sub_g.t[:], small.t[:, 256:384], 1.0 - LAM_INIT, None, ALU.mult, None, R=[small], W=[sub_g])
    negA = S.sb("negA", [128, 1])
    b.act(negA.t[:], small.t[:, 12:13], AF.Exp, R=[small], W=[negA])
    b.ts(negA.t[:], negA.t[:], -1.0, None, ALU.mult, None, R=[negA], W=[negA])
    lt = S.sb("lam_t", [1, 136])
    b.tt(lt.t[0:1, 0:64], lamr.t[0:1, 0:64], lamr.t[0:1, 64:128], ALU.mult, R=[lamr], W=[lt])
    b.tt(lt.t[0:1, 64:128], lamr.t[0:1, 128:192], lamr.t[0:1, 192:256], ALU.mult, R=[lamr], W=[lt])
    b.S.op("dve", lambda e: e.tensor_reduce(out=lt.t[0:1, 128:129], in_=lt.t[0:1, 0:64], axis=AX.X, op=ALU.add), R=[lt], W=[lt])
    b.S.op("dve", lambda e: e.tensor_reduce(out=lt.t[0:1, 129:130], in_=lt.t[0:1, 64:128], axis=AX.X, op=ALU.add), R=[lt], W=[lt])
    b.act(lt.t[0:1, 128:130], lt.t[0:1, 128:130], AF.Exp, R=[lt], W=[lt])
    b.tt(lt.t[0:1, 130:131], lt.t[0:1, 129:130], lt.t[0:1, 128:129], ALU.subtract, R=[lt], W=[lt])
    b.ts(lt.t[0:1, 130:131], lt.t[0:1, 130:131], -LAM_INIT, None, ALU.add, None, R=[lt], W=[lt])
    neglam = S.sb("neglam", [128, 1])
    bk = b.bank()
    b.mm(bk, bk.t[:, 0:1], c["ones"][0:1, 0:128], lt.t[0:1, 130:131], R=[lt, c["t"]])
    b.cp(neglam.t[:], bk.t[:, 0:1], R=[bk], W=[neglam])

    qT = S.sb("qT_all", [128, Sq])
    kT = S.sb("kT_all", [128, Sq])
    Vx = S.sb("Vext", [128, Sq // 128, 130])
    b.memset(Vx.t[:, :, 128:130], 1.0, W=[Vx])
    hT = [S.sb(f"hT{i}", [128, 8, 512]) for i in range(1)]
    xt = [S.sb(f"xt{i}", [128, 1024]) for i in range(1)]
    xn = S.sb("xn", [128, 1024])
    scr = xn
    ssx = S.sb("ssx", [128, 1])
    pre = [S.sb(f"pre{j}", [128, 515]) for j in range(3)]
    for j in range(3):
        b.memset(pre[j].t[:, 0:3], 0.0, W=[pre[j]])
    cv = [S.sb(f"cv{j}", [128, 512]) for j in range(3)]
    sq = S.sb("sq", [128, 512])
    rs = S.sb("rs", [128, 512])
    qk_tmp = S.sb("qk_tmp", [128, 512])
    St = S.sb("state", [128, 128])
    b.memset(St.t[:], 0.0, W=[St])
    def cb(name, shape):
        return [S.sb(f"{name}{i}", shape) for i in range(2)]
    zb = cb("zb", [64, 132]); gcol = cb("gcol", [64, 8]); e1 = cb("e1", [64, 64]); dts = cb("dts", [64, 64])
    dcs = cb("dcs", [64, 64]); Mb = [cb("Ma", [64, 64]), cb("Mb", [64, 64])]; Nb = [cb("Na", [64, 64]), cb("Nb", [64, 64])]
    Tb = [cb("Ta", [64, 64]), cb("Tb", [64, 64])]; ktm = cb("ktm", [64, 128]); kbt = cb("kbt", [64, 128])
    vbt = cb("vbt", [64, 128]); kdec = cb("kdec", [64, 128]); Ub = cb("Ub", [64, 128]); WTb = cb("WTb", [128, 64])
    AqT = cb("AqT", [64, 64]); gl = cb("gl", [128, 2]); vnew = cb("vnew", [64, 128]); o1 = cb("o1", [64, 128])
    ob_ = cb("ob_", [64, 128]); szb = cb("szb", [64, 128]); tmp64 = cb("tmp64", [64, 64]); oast = cb("oast", [64, 128])
    ssg = cb("ssg", [64, 2])
    PT = [S.sb(f"PT{i}", [128, 512]) for i in range(2)]
    obacc = [S.sb(f"obacc{i}", [128, 128]) for i in range(4)]
    rz = S.sb("rz", [128, 8])
    obst = [S.sb(f"obst{i}", [128, 128]) for i in range(2)]
    sso = S.sb("sso", [128, 2])
    ident = c["ident"]; ct = c["t"]
    SCALE = 64 ** -0.5

    for g in range(NG):
        h = hT[0]
        for tt in range(4):
            x_ = xt[0]
            b.dma(x_.t[:], xb.t[g * 512 + tt * 128: g * 512 + (tt + 1) * 128, :], R=[xb], W=[x_])
            norm_tile2(b, c, x_, xn, ssx, scr, ab, 0, [h.t], [h], tt * 128)
        for j in range(5):
            bk = b.bank()
            for k in range(8):
                b.mm(bk, bk.t[:, 0:512], wcm.t[:, k, j * 128:(j + 1) * 128], h.t[:, k, :], R=[wcm, h], start=(k == 0), stop=(k == 7))
            if j < 3:
                b.act(pre[j].t[:, 3:515], bk.t[:, 0:512], AF.Copy, R=[bk], W=[pre[j]])
                y = cv[j]
                b.ts(y.t[:], pre[j].t[:, 0:512], convw(j, 0), None, ALU.mult, None, R=[pre[j], small], W=[y])
                for tap in range(1, 4):
                    b.stt(y.t[:], pre[j].t[:, tap:tap + 512], convw(j, tap), y.t[:], ALU.mult, ALU.add, R=[pre[j], small, y], W=[y])
                b.cp(pre[j].t[:, 0:3], pre[j].t[:, 512:515], R=[pre[j]], W=[pre[j]])
                b.act(y.t[:], y.t[:], AF.Silu, R=[y], W=[y])
                if j < 2:
                    b.tt(sq.t[:], y.t[:], y.t[:], ALU.mult, R=[y], W=[sq])
                    bk2 = b.bank()
                    b.mm(bk2, bk2.t[:, 0:512], c["ones"], sq.t[:], R=[sq, ct])
                    b.act(rs.t[:], bk2.t[:, 0:512], AF.Ln, R=[bk2], W=[rs], bias=EPS)
                    b.act(rs.t[:], rs.t[:], AF.Exp, R=[rs], W=[rs], scale=-0.5, bias=(float(np.log(128 ** -0.5)) if j == 0 else 0.0))
                    b.tt(y.t[:], y.t[:], rs.t[:], ALU.mult, R=[y, rs], W=[y])
            else:
                dst = qT if j == 3 else kT
                gcolm = small.t[:, 14:15] if j == 3 else small.t[:, 15:16]
                b.act(qk_tmp.t[:], bk.t[:, 0:512], AF.Copy, R=[bk], W=[qk_tmp])
                b.tt(sq.t[:], qk_tmp.t[:], qk_tmp.t[:], ALU.mult, R=[qk_tmp], W=[sq])
                bk2 = b.bank()
                b.mm(bk2, bk2.t[:, 0:512], c["bd"], sq.t[:], R=[sq, ct])
                b.act(rs.t[:], bk2.t[:, 0:512], AF.Ln, R=[bk2], W=[rs], scale=1.0 / 64, bias=EPS)
                b.act(rs.t[:], rs.t[:], AF.Exp, R=[rs], W=[rs], scale=-0.5)
                b.stt(dst.t[:, g * 512:(g + 1) * 512], qk_tmp.t[:], gcolm, rs.t[:], ALU.mult, ALU.mult, R=[qk_tmp, small, rs], W=[dst.sub(g)])
        for tt in range(4):
            bk = b.bank()
            for k in range(8):
                b.mm(bk, bk.t[:, 0:128], h.t[:, k, tt * 128:(tt + 1) * 128], wvb.t[:, k, :], R=[wvb, h], start=(k == 0), stop=(k == 7))
            b.cp(Vx.t[:, g * 4 + tt, 0:128], bk.t[:, 0:128], R=[bk], W=[Vx.sub(g)])
        qn, kn, vs = cv[0], cv[1], cv[2]
        def chunk_pre(ch):
            p = ch % 2
            t0 = ch * 64
            kTc = kn.t[:, t0:t0 + 64]; qTc = qn.t[:, t0:t0 + 64]
            bk = b.bank()
            for k in range(8):
                b.mm(bk, bk.t[0:64, 0:130], h.t[:, k, t0:t0 + 64], wzbd.t[:, k, :], R=[wzbd, h], start=(k == 0), stop=(k == 7))
            z = zb[p]
            b.cp(z.t[:, 0:130], bk.t[0:64, 0:130], R=[bk], W=[z])
            yield
            gc = gcol[p]
            b.act(gc.t[:, 0:1], z.t[:, 128:129], AF.Sigmoid, R=[z], W=[gc])
            yield
            b.act(gc.t[:, 1:2], z.t[:, 129:130], AF.Exp, R=[z, small], W=[gc], bias=small.t[0:64, 13:14])
            yield
            b.act(gc.t[:, 1:2], gc.t[:, 1:2], AF.Ln, R=[gc], W=[gc], bias=1.0)
            yield
            b.tt(gc.t[:, 1:2], gc.t[:, 1:2], negA.t[0:64, 0:1], ALU.mult, R=[gc, negA], W=[gc])
            yield
            bk = b.bank()
            b.mm(bk, bk.t[0:64, 0:1], c["UT"], gc.t[:, 1:2], R=[gc, ct])
            yield
            b.cp(gc.t[:, 2:3], bk.t[0:64, 0:1], R=[bk], W=[gc])
            yield
            t64 = tmp64[p]
            b.ts(t64.t[:], c["UT"], gc.t[:, 1:2], None, ALU.mult, None, R=[gc, ct], W=[t64])
            yield
            bk = b.bank()
            b.mm(bk, bk.t[0:64, 0:64], c["ones"][0:64, 0:64], t64.t[:], R=[t64, ct])
            yield
            E = e1[p]
            b.ts(E.t[:], bk.t[0:64, 0:64], gc.t[:, 2:3], None, ALU.subtract, None, R=[bk, gc], W=[E])
            yield
            Dt = dts[p]; Dc = dcs[p]
            b.ts(Dt.t[:], E.t[:], 0.0, None, ALU.min, None, R=[E], W=[Dt])
            yield
            b.act(Dt.t[:], Dt.t[:], AF.Exp, R=[Dt], W=[Dt])
            yield
            b.tt(Dt.t[:], Dt.t[:], c["UT"], ALU.mult, R=[Dt, ct], W=[Dt])
            yield
            b.ts(Dc.t[:], E.t[:], -1.0, 0.0, ALU.mult, ALU.min, R=[E], W=[Dc])
            yield
            b.act(Dc.t[:], Dc.t[:], AF.Exp, R=[Dc], W=[Dc])
            yield
            b.tt(Dc.t[:], Dc.t[:], c["LTs"], ALU.mult, R=[Dc, ct], W=[Dc])
            yield
            bk = b.bank()
            b.mm(bk, bk.t[0:64, 0:64], kTc, kTc, R=[kn])
            yield
            M = Mb[0][p]; N = Nb[0][p]; T = Tb[0][p]
            b.stt(M.t[:], bk.t[0:64, 0:64], gc.t[:, 0:1], Dc.t[:], ALU.mult, ALU.mult, R=[bk, gc, Dc], W=[M])
            yield
            bk = b.bank()
            b.tp(bk, bk.t[0:64, 0:64], M.t[:], ident[0:64, 0:64], R=[M, ct])
            yield
            b.cp(N.t[:], bk.t[0:64, 0:64], R=[bk], W=[N])
            yield
            b.tt(T.t[:], ident[0:64, 0:64], N.t[:], ALU.subtract, R=[N, ct], W=[T])
            yield
            for s in range(1, 6):
                M2 = Mb[s % 2][p]; N2 = Nb[s % 2][p]; T2 = Tb[s % 2][p]
                bk = b.bank()
                b.mm(bk, bk.t[0:64, 0:64], N.t[:], M.t[:], R=[N, M])
                yield
                b.act(M2.t[:], bk.t[0:64, 0:64], AF.Copy, R=[bk], W=[M2])
                yield
                if s < 5:
                    bk = b.bank()
                    b.mm(bk, bk.t[0:64, 0:64], M.t[:], N.t[:], R=[N, M])
                    yield
                    b.cp(N2.t[:], bk.t[0:64, 0:64], R=[bk], W=[N2])
                    yield
                bk = b.bank()
                b.mm(bk, bk.t[0:64, 0:64], M2.t[:], T.t[:], R=[M2, T])
                yield
                b.tt(T2.t[:], bk.t[0:64, 0:64], T.t[:], ALU.add, R=[bk, T], W=[T2])
                yield
                M, N, T = M2, N2, T2
            bk = b.bank()
            b.tp(bk, bk.t[0:64, 0:128], kTc, ident, R=[kn, ct])
            yield
            b.tp(bk, bk.t[0:64, 128:256], vs.t[:, t0:t0 + 64], ident, R=[vs, ct])
            yield
            b.act(gc.t[:, 3:4], gc.t[:, 2:3], AF.Exp, R=[gc], W=[gc])
            yield
            b.tt(gc.t[:, 4:5], gc.t[:, 3:4], gc.t[:, 0:1], ALU.mult, R=[gc], W=[gc])
            yield
            b.cp(ktm[p].t[:], bk.t[0:64, 0:128], R=[bk], W=[ktm[p]])
            yield
            b.ts(kbt[p].t[:], bk.t[0:64, 0:128], gc.t[:, 4:5], None, ALU.mult, None, R=[bk, gc], W=[kbt[p]])
            yield
            b.ts(vbt[p].t[:], bk.t[0:64, 128:256], gc.t[:, 0:1], None, ALU.mult, None, R=[bk, gc], W=[vbt[p]])
            yield
            bk = b.bank()
            b.mm(bk, bk.t[0:64, 0:128], T.t[:], vbt[p].t[:], R=[T, vbt[p]])
            yield
            b.cp(Ub[p].t[:], bk.t[0:64, 0:128], R=[bk], W=[Ub[p]])
            yield
            bk = b.bank()
            b.mm(bk, bk.t[:, 0:64], kbt[p].t[:], T.t[:], R=[T, kbt[p]])
            yield
            b.act(WTb[p].t[:], bk.t[:, 0:64], AF.Copy, R=[bk], W=[WTb[p]])
            yield
            bk = b.bank()
            b.mm(bk, bk.t[0:64, 0:64], kTc, qTc, R=[kn, qn])
            yield
            b.tt(AqT[p].t[:], bk.t[0:64, 0:64], Dt.t[:], ALU.mult, R=[bk, Dt], W=[AqT[p]])
            yield
            bk = b.bank()
            b.mm(bk, bk.t[:, 0:1], c["ones"][0:64, 0:128], gc.t[:, 1:2], R=[gc, ct])
            yield
            b.cp(gl[p].t[:, 0:1], bk.t[:, 0:1], R=[bk], W=[gl[p]])
            yield
            b.act(gl[p].t[:, 1:2], gl[p].t[:, 0:1], AF.Exp, R=[gl[p]], W=[gl[p]])
            yield
            b.act(gc.t[:, 5:6], gc.t[:, 2:3], AF.Exp, R=[gc, gl[p]], W=[gc], scale=-1.0, bias=gl[p].t[0:64, 0:1])
            yield
            b.ts(kdec[p].t[:], ktm[p].t[:], gc.t[:, 5:6], None, ALU.mult, None, R=[ktm[p], gc], W=[kdec[p]])
            yield
            yield

        def chunk_seq(ch):
            p = ch % 2
            t0 = ch * 64
            qTc = qn.t[:, t0:t0 + 64]
            z = zb[p]
            gc = gcol[p]
            bkv = b.bank()
            b.mm(bkv, bkv.t[0:64, 0:128], WTb[p].t[:], St.t[:], R=[WTb[p], St])
            b.tt(vnew[p].t[:], Ub[p].t[:], bkv.t[0:64, 0:128], ALU.subtract, R=[bkv, Ub[p]], W=[vnew[p]])
            bk1 = b.bank()
            b.mm(bk1, bk1.t[0:64, 0:128], qTc, St.t[:], R=[qn, St])
            b.act(o1[p].t[:], bk1.t[0:64, 0:128], AF.Copy, R=[bk1, gc], W=[o1[p]], scale=gc.t[:, 3:4])
            bk2 = b.bank()
            b.mm(bk2, bk2.t[0:64, 0:128], AqT[p].t[:], vnew[p].t[:], R=[AqT[p], vnew[p]])
            b.tt(ob_[p].t[:], bk2.t[0:64, 0:128], o1[p].t[:], ALU.add, R=[bk2, o1[p]], W=[ob_[p]])
            bks = b.bank()
            b.mm(bks, bks.t[:, 0:128], kdec[p].t[:], vnew[p].t[:], R=[kdec[p], vnew[p]])
            b.stt(St.t[:], St.t[:], gl[p].t[:, 1:2], bks.t[:, 0:128], ALU.mult, ALU.add, R=[St, gl[p], bks], W=[St])
            b.act(szb[p].t[:], ob_[p].t[:], AF.Square, R=[ob_[p]], W=[szb[p], ssg[p]], accum_out=ssg[p].t[:, 0:1])
            b.rstd(ssg[p].t[:, 0:1], ssg[p].t[:, 0:1], R=[ssg[p]], W=[ssg[p]], scale=1.0 / 128)
            b.stt(ob_[p].t[:], ob_[p].t[:], ssg[p].t[:, 0:1], gng, ALU.mult, ALU.mult, R=[ob_[p], ssg[p], small], W=[ob_[p]])
            b.act(szb[p].t[:], z.t[:, 0:128], AF.Silu, R=[z], W=[szb[p]])
            b.tt(oast[p].t[:], ob_[p].t[:], szb[p].t[:], ALU.mult, R=[ob_[p], szb[p]], W=[oast[p]])
            sap, sbuf_ = send_at(g * 512 + t0, 64, 0, ("a", g, ch))
            b.dma(sap, oast[p].t[:], R=[oast[p]], W=[sbuf_], final=True)
        def gdn_gen():
            for pr in range(4):
                gens = [chunk_pre(2 * pr), chunk_pre(2 * pr + 1)]
                while gens:
                    for gnr in list(gens):
                        try:
                            next(gnr)
                        except StopIteration:
                            gens.remove(gnr)
                    yield
                chunk_seq(2 * pr)
                yield
                chunk_seq(2 * pr + 1)
                yield

        def attn_gen():
            for comp in range(2):
                lo, hi = comp * 64, (comp + 1) * 64
                nkb = 4 * g + 4
                for kb in range(nkb):
                    rel = kb - 4 * g
                    qlo = max(rel, 0) * 128
                    ps = b.banks[0]
                    b.mm(ps, ps.t[:, qlo:512], kT.t[lo:hi, kb * 128:(kb + 1) * 128], qT.t[lo:hi, g * 512 + qlo:(g + 1) * 512],
                         R=[kT.sub(kb // 4), qT.sub(g)])
                    P = PT[kb % 2]
                    for qs in range(max(rel, 0), 4):
                        r_ = kb - (4 * g + qs) + 63
                        b.act(P.t[:, qs * 128:(qs + 1) * 128], ps.t[:, qs * 128:(qs + 1) * 128], AF.Exp, R=[ps, ct], W=[P], scale=SCALE,
                              bias=c["brel"][:, r_:r_ + 1])
                    if rel >= 0:
                        b.tt(P.t[:, rel * 128:(rel + 1) * 128], P.t[:, rel * 128:(rel + 1) * 128], c["Cm"], ALU.mult, R=[P, ct], W=[P])
                    yield
                    for qs in range(max(rel, 0), 4):
                        po = b.banks[4 + qs]
                        b.mm(po, po.t[:, 0:130], P.t[:, qs * 128:(qs + 1) * 128], Vx.t[:, kb, 0:130], R=[P, Vx.sub(kb // 4)],
                             start=(kb == 0), stop=(kb == 4 * g + qs))
                    yield
                for qs in range(4):
                    po = b.banks[4 + qs]
                    rcol = rz.t[:, comp * 4 + qs: comp * 4 + qs + 1]
                    b.S.op("dve", lambda e, rcol=rcol, po=po: e.reciprocal(out=rcol, in_=po.t[:, 128:129]), R=[po], W=[rz])
                    if comp == 0:
                        b.ts(obacc[qs].t[:], po.t[:, 0:128], rcol, None, ALU.mult, None, R=[po, rz], W=[obacc[qs]])
                    else:
                        b.tt(rcol, rcol, neglam.t[:, 0:1], ALU.mult, R=[rz, neglam], W=[rz])
                        b.stt(obacc[qs].t[:], po.t[:, 0:128], rcol, obacc[qs].t[:], ALU.mult, ALU.add, R=[po, rz, obacc[qs]], W=[obacc[qs]])
                        o_ = obst[qs % 2]
                        b.act(o_.t[:], obacc[qs].t[:], AF.Square, R=[obacc[qs]], W=[o_, sso], accum_out=sso.t[:, 0:1])
                        b.rstd(sso.t[:, 0:1], sso.t[:, 0:1], R=[sso], W=[sso], scale=1.0 / 128)
                        b.stt(o_.t[:], obacc[qs].t[:], sso.t[:, 0:1], sub_g.t[:], ALU.mult, ALU.mult, R=[obacc[qs], sso, sub_g], W=[o_])
                        sap, sbuf_ = send_at(g * 512 + qs * 128, 128, 128, ("b", g, qs))
                        b.dma(sap, o_.t[:], R=[o_], W=[sbuf_], final=True)
        b.bank_pool = [1, 2, 3]
        gG, gA = gdn_gen(), attn_gen()
        nA_est = 2 * (4 * g + 4) * 2 + 2
        nG_est = 4 * 75
        ratio = nG_est / nA_est
        accr = 0.0
        aliveG = aliveA = True
        while aliveG or aliveA:
            if aliveA:
                try:
                    next(gA)
                except StopIteration:
                    aliveA = False
            accr += ratio
            n_g = int(accr) if aliveA else 1
            accr -= int(accr)
            for _ in range(max(n_g, 0)):
                if not aliveG:
                    break
                try:
                    next(gG)
                except StopIteration:
                    aliveG = False
        b.bank_pool = list(range(8))
        if after_group is not None:
            after_group(g)


def _cols(v):
    return np.ascontiguousarray(np.asarray(v, np.float32).reshape(-1, 128).T)


def alibi_slope(h):
    return float(2.0 ** (-8.0 * (h + 1) / 4))


def prep_common(inp, bidx):
    return {
        "c_col": _cols(inp["c"][bidx]),
        "w_ada": np.ascontiguousarray(inp["w_ada"][0]),
        "b_ada": np.ascontiguousarray(inp["b_ada"][0].reshape(1, -1)),
        "g1c": _cols(inp["norm1_g"][0]),
        "g2c": _cols(inp["norm2_g"][0]),
    }


def prep_ab(inp, bidx, h):
    w_in = inp["w_in"][0]
    sl = lambda name, width=128: w_in[:, W_OFF[name] + h * width: W_OFF[name] + (h + 1) * width]
    m = prep_common(inp, bidx)
    m["consts"] = host_consts(alibi_slope(h))
    m["xb"] = np.ascontiguousarray(inp["x"][bidx])
    m["w_cm"] = np.ascontiguousarray(np.concatenate([sl("qA"), sl("kA"), sl("vA"), sl("qB"), sl("kB")], axis=1))
    m["w_zbd"] = np.ascontiguousarray(np.concatenate([sl("zA"), sl("beta", 1), sl("decay", 1)], axis=1))
    m["w_vb"] = np.ascontiguousarray(sl("vB"))
    small = np.zeros((128, 384), np.float32)
    cw = inp["conv_w"][0]
    for j in range(3):
        small[:, j * 4:(j + 1) * 4] = cw[:, j * 512 + h * 128: j * 512 + (h + 1) * 128].T
    small[:, 12] = inp["a_log"][0, h]
    small[:, 13] = inp["dt_bias"][0, h]
    small[:, 14] = np.tile(inp["q_norm_g"][0], 2)
    small[:, 15] = np.tile(inp["k_norm_g"][0], 2)
    small[:, 128:256] = np.tile(inp["gdn_norm_g"][0][None, :], (128, 1))
    small[:, 256:384] = np.tile(inp["subln_g"][0][None, :], (128, 1))
    m["small"] = small
    m["lamrow"] = np.ascontiguousarray(np.concatenate(
        [inp["lambda_q1"][0], inp["lambda_k1"][0], inp["lambda_q2"][0], inp["lambda_k2"][0]]).reshape(1, 256))
    return m


def build_d(Sq, n_exp=257):
    nc = bass.Bass("TRN2", target_bir_lowering=False)
    b = Bld(nc)
    c = build_consts(b)
    ad = emit_adaln(b, c, want="gates")
    emit_d(b, c, ad, Sq, None, n_exp)
    b.S.emit()
    return nc


def build_fused(Sq, n_exp=257):
    nc = bass.Bass("TRN2", target_bir_lowering=False)
    b = Bld(nc)
    S = b.S
    c = build_consts(b)
    ad = emit_adaln(b, c, want="gates")
    CH = min(1024, Sq)
    NCH = Sq // CH
    sends = [S.dram(nc.dram_tensor(f"send_bounce{i}", [CH, 256], F32).ap(), f"send{i}") for i in range(NCH)]
    gaths = [S.dram(nc.dram_tensor(f"gath_bounce{i}", [4 * CH, 256], F32).ap(), f"gath{i}") for i in range(NCH)]

    def send_at(r0, n, c0, key):
        ch, lr = r0 // CH, r0 % CH
        return sends[ch].t[lr:lr + n, c0:c0 + 128], sends[ch].sub(key)

    def after_group(g):
        if ((g + 1) * 512) % CH == 0:
            ch = ((g + 1) * 512) // CH - 1
            sd, gt = sends[ch], gaths[ch]
            S.cc(lambda e: e.collective_compute("AllGather", ALU.bypass, replica_groups=[[0, 1, 2, 3], [4, 5, 6, 7]],
                                                ins=[sd.t.opt()], outs=[gt.t.opt()]),
                 R=[sd] + list(sd.subs.values()), W=[gt])

    emit_ab(b, c, ad["ab"], Sq, send_at, after_group)
    emit_d(b, c, ad, Sq, (gaths, CH), n_exp)
    S.emit()
    return nc


def emit_d(b, c, ad, Sq, gath, n_exp):
    S = b.S
    nc = b.nc
    T = Sq // 4
    NT = T // 128
    MG = min(512, T)
    NTG = T // MG
    TPG = MG // 128
    ab, g1bc, g2bc = ad["ab"], ad["g1bc"], ad["g2bc"]
    ident = c["ident"]; ct = c["t"]
    xs = b.inp("xs", [T, 1024])
    if gath is None:
        oa_in = b.inp("oa_in", [T, 512])
        ob_in = b.inp("ob_in", [T, 512])
    else:
        sel_d = b.inp("sel", [128, 4])
    wg_d = b.inp("w_gates", [1024, 2048])
    wog_d = b.inp("w_o_gdn", [512, 1024])
    wod_d = b.inp("w_o_diff", [512, 1024])
    wout_d = b.inp("w_out", [1024, 1024])
    wr_d = b.inp("w_router", [1024, 256])
    rb_d = b.inp("rbias", [128, 256])
    wgu_d = b.inp("w_gu", [n_exp, 1024, 512])
    wdn_d = b.inp("w_dn", [n_exp, 256, 1024])
    outd = b.outp("out", [T, 1024])

    acc = S.sb("acc", [128, NT, 1024])
    h2b = S.sb("h2T_bf", [128, 8, T], BF16)
    wt = S.sb("wt_all", [128, NT, n_exp + 1])
    b.memset(wt.t[:, :, 256:n_exp + 1], 1.0, W=[wt])
    with S.scope():
        wr = S.sb("wr", [128, 8, 256]); rb = S.sb("rb", [128, 256])
        b.dma(wr.t[:], wr_d.t[:, :].rearrange("(k p) c -> p k c", p=128), R=[wr_d], W=[wr])
        b.dma(rb.t[:], rb_d.t[:, :], R=[rb_d], W=[rb])
        wgb = [S.sb(f"wgb{i}", [128, 8, 512]) for i in range(2)]
        xt = [S.sb(f"dxt{i}", [128, 1024]) for i in range(1)]
        xn = S.sb("dxn", [128, 1024]); ssx = S.sb("dssx", [128, 1])
        hT1 = S.sb("hT1", [128, 8, 128]); h2f = hT1
        sg = S.sb("sg", [128, 2048])
        if gath is None:
            oat = S.sb("oat", [128, 512]); obt = S.sb("obt", [128, 512])
        else:
            cand = [S.sb(f"cand{i}", [128, 4, 256]) for i in range(2)]
            cmb = S.sb("cmb", [128, 4, 256])
            sel = S.sb("sel_sb", [128, 4])
            b.dma(sel.t[:], sel_d.t[:, :], R=[sel_d], W=[sel])
            gaths, CH = gath
        oaT = S.sb("oaT", [128, 4, 128]); obT = S.sb("obT", [128, 4, 128])
        mg = S.sb("merged", [128, 1024]); tmp = S.sb("dtmp", [128, 512]); mT = hT1
        scr = mg
        class _V:
            def __init__(self, tn, lo):
                self.t = tn.t[:, lo:lo + 256]; self.b = tn.b
        sc_ = _V(sg, 0); ch = _V(sg, 256); mc = _V(sg, 512)
        m8 = S.sb("r_m8", [128, 8]); gs = S.sb("r_gs", [128, 8]); gm = S.sb("r_gm", [128, 8]); pen = S.sb("r_pen", [128, 8])
        rsum = S.sb("r_sum", [128, 2])
        gi = 0
        for tt in range(NT):
            x_ = xt[0]
            b.dma(x_.t[:], xs.t[tt * 128:(tt + 1) * 128, :], R=[xs], W=[x_])
            if gath is None:
                b.dma(oat.t[:], oa_in.t[tt * 128:(tt + 1) * 128, :], R=[oa_in], W=[oat])
                b.dma(obt.t[:], ob_in.t[tt * 128:(tt + 1) * 128, :], R=[ob_in], W=[obt])
                srcs = ((oat, lambda cc: oat.t[:, cc * 128:(cc + 1) * 128]), (obt, lambda cc: obt.t[:, cc * 128:(cc + 1) * 128]))
            else:
                cf = lambda t_: t_.t[:].rearrange("p a b -> p (a b)")
                for hp in range(4):
                    cd = cand[hp % 2]
                    gtok = hp * T + tt * 128
                    gsrc = gaths[gtok // CH]
                    lr = gtok % CH
                    b.dma(cd.t[:], gsrc.t.rearrange("(i s) c -> s i c", i=4)[lr:lr + 128, :, :], R=[gsrc], W=[cd])
                    if hp == 0:
                        b.ts(cf(cmb), cf(cd), sel.t[:, 0:1], None, ALU.mult, None, R=[cd, sel], W=[cmb])
                    else:
                        b.stt(cf(cmb), cf(cd), sel.t[:, hp:hp + 1], cf(cmb), ALU.mult, ALU.add, R=[cd, sel, cmb], W=[cmb])
                srcs = ((cmb, lambda cc: cmb.t[:, cc, 0:128]), (cmb, lambda cc: cmb.t[:, cc, 128:256]))
            norm_tile2(b, c, x_, xn, ssx, scr, ab, 0, [hT1.t], [hT1], 0)
            for cbk in range(4):
                w = wgb[gi % 2]; gi += 1
                b.dma(w.t[:], wg_d.t[:, cbk * 512:(cbk + 1) * 512].rearrange("(k p) c -> p k c", p=128), R=[wg_d], W=[w])
                bk = b.bank()
                for k in range(8):
                    b.mm(bk, bk.t[:, 0:512], hT1.t[:, k, :], w.t[:, k, :], R=[hT1, w], start=(k == 0), stop=(k == 7))
                b.act(sg.t[:, cbk * 512:(cbk + 1) * 512], bk.t[:, 0:512], AF.Sigmoid, R=[bk], W=[sg])
            for ((src, view), dst) in zip(srcs, (oaT, obT)):
                bk = b.bank()
                for cc in range(4):
                    b.tp(bk, bk.t[:, cc * 128:(cc + 1) * 128], view(cc), ident, R=[src, ct])
                b.cp(dst.t[:].rearrange("p a b -> p (a b)"), bk.t[:, 0:512], R=[bk], W=[dst])
            wogT = wgb[gi % 2]; gi += 1
            b.dma(wogT.t[:].rearrange("p k c -> p (k c)").rearrange("p (a f) -> p a f", a=4), wog_d.t[:, :].rearrange("(k p) c -> p k c", p=128), R=[wog_d], W=[wogT])
            wodT = wgb[gi % 2]; gi += 1
            b.dma(wodT.t[:].rearrange("p k c -> p (k c)").rearrange("p (a f) -> p a f", a=4), wod_d.t[:, :].rearrange("(k p) c -> p k c", p=128), R=[wod_d], W=[wodT])
            wog_v = wogT.t[:].rearrange("p k c -> p (k c)").rearrange("p (a f) -> p a f", a=4)
            wod_v = wodT.t[:].rearrange("p k c -> p (k c)").rearrange("p (a f) -> p a f", a=4)
            for hf in range(2):
                bk = b.bank()
                for cc in range(4):
                    b.mm(bk, bk.t[:, 0:512], oaT.t[:, cc, :], wog_v[:, cc, hf * 512:(hf + 1) * 512], R=[oaT, wogT], start=(cc == 0), stop=(cc == 3))
                b.tt(mg.t[:, hf * 512:(hf + 1) * 512], bk.t[:, 0:512], sg.t[:, hf * 512:(hf + 1) * 512], ALU.mult, R=[bk, sg], W=[mg])
                bk = b.bank()
                for cc in range(4):
                    b.mm(bk, bk.t[:, 0:512], obT.t[:, cc, :], wod_v[:, cc, hf * 512:(hf + 1) * 512], R=[obT, wodT], start=(cc == 0), stop=(cc == 3))
                b.tt(tmp.t[:], bk.t[:, 0:512], sg.t[:, 1024 + hf * 512:1024 + (hf + 1) * 512], ALU.mult, R=[bk, sg], W=[tmp])
                b.tt(mg.t[:, hf * 512:(hf + 1) * 512], mg.t[:, hf * 512:(hf + 1) * 512], tmp.t[:], ALU.add, R=[mg, tmp], W=[mg])
            for k2 in range(2):
                bk = b.bank()
                for kk in range(4):
                    k = k2 * 4 + kk
                    b.tp(bk, bk.t[:, kk * 128:(kk + 1) * 128], mg.t[:, k * 128:(k + 1) * 128], ident, R=[mg, ct])
                b.cp(mT.t[:, k2 * 4:(k2 + 1) * 4, :].rearrange("p a b -> p (a b)"), bk.t[:, 0:512], R=[bk], W=[mT])
            for hf in range(2):
                wo_ = wgb[gi % 2]; gi += 1
                b.dma(wo_.t[:], wout_d.t[:, hf * 512:(hf + 1) * 512].rearrange("(k p) c -> p k c", p=128), R=[wout_d], W=[wo_])
                bk = b.bank()
                for k in range(8):
                    b.mm(bk, bk.t[:, 0:512], mT.t[:, k, :], wo_.t[:, k, :], R=[mT, wo_], start=(k == 0), stop=(k == 7))
                b.tt(tmp.t[:], bk.t[:, 0:512], g1bc.t[:, hf * 512:(hf + 1) * 512], ALU.mult, R=[bk, g1bc], W=[tmp])
                b.tt(acc.t[:, tt, hf * 512:(hf + 1) * 512], tmp.t[:], x_.t[:, hf * 512:(hf + 1) * 512], ALU.add, R=[tmp, x_], W=[acc.sub(tt)])
            norm_tile2(b, c, acc.sub(tt), xn, ssx, scr, ab, 16, [h2f.t, h2b.t], [h2f, h2b.sub(tt // TPG)], 0, xap=acc.t[:, tt, :], cols=[0, tt * 128])
            bk = b.bank()
            for k in range(8):
                b.mm(bk, bk.t[:, 0:256], h2f.t[:, k, :], wr.t[:, k, :], R=[h2f, wr], start=(k == 0), stop=(k == 7))
            b.act(sc_.t[:], bk.t[:, 0:256], AF.Sigmoid, R=[bk], W=[sc_])
            b.tt(ch.t[:], sc_.t[:], rb.t[:], ALU.add, R=[sc_, rb], W=[ch])
            for g in range(8):
                b.S.op("dve", lambda e, g=g: e.max(out=m8.t[:], in_=ch.t[:, g * 32:(g + 1) * 32]), R=[ch], W=[m8])
                b.tt(gs.t[:, g:g + 1], m8.t[:, 0:1], m8.t[:, 1:2], ALU.add, R=[m8], W=[gs])
            b.S.op("dve", lambda e: e.max(out=m8.t[:], in_=gs.t[:]), R=[gs], W=[m8])
            b.ts(gm.t[:], gs.t[:], m8.t[:, 3:4], None, ALU.is_ge, None, R=[gs, m8], W=[gm])
            b.ts(pen.t[:], gm.t[:], -1.0, 1e9, ALU.add, ALU.mult, R=[gm], W=[pen])
            for g in range(8):
                b.ts(mc.t[:, g * 32:(g + 1) * 32], ch.t[:, g * 32:(g + 1) * 32], gm.t[:, g:g + 1], pen.t[:, g:g + 1], ALU.mult, ALU.add,
                     R=[ch, gm, pen], W=[mc])
            b.S.op("dve", lambda e: e.max(out=m8.t[:], in_=mc.t[:]), R=[mc], W=[m8])
            b.ts(mc.t[:], mc.t[:], m8.t[:, 7:8], None, ALU.is_ge, None, R=[mc, m8], W=[mc])
            b.tt(mc.t[:], mc.t[:], sc_.t[:], ALU.mult, R=[mc, sc_], W=[mc])
            b.S.op("dve", lambda e: e.tensor_reduce(out=rsum.t[:, 0:1], in_=mc.t[:], axis=AX.X, op=ALU.add), R=[mc], W=[rsum])
            b.S.op("dve", lambda e: e.reciprocal(out=rsum.t[:, 1:2], in_=rsum.t[:, 0:1]), R=[rsum], W=[rsum])
            b.ts(wt.t[:, tt, 0:256], mc.t[:], rsum.t[:, 1:2], 2.5, ALU.mult, ALU.mult, R=[mc, rsum], W=[wt])
    with S.scope():
        wgu_st = [S.sb(f"wgu_st{i}", [128, 8, 512]) for i in range(2)]
        wd_st = [S.sb(f"wd_st{i}", [128, 2, 1024]) for i in range(2)]
        wgu_bf = [S.sb(f"wgu_bf{i}", [128, 8, 512], BF16) for i in range(2)]
        wd_bf = [S.sb(f"wd_bf{i}", [128, 2, 1024], BF16) for i in range(2)]
        AT = [S.sb(f"AT{i}", [128, 2, MG], BF16) for i in range(2)]
        slu = [S.sb(f"slu{i}", [128, MG]) for i in range(2)]
        for e in range(n_exp):
            p = e % 2
            b.dma(wgu_st[p].t[:], wgu_d.t[e].rearrange("(k p) c -> p k c", p=128), R=[wgu_d], W=[wgu_st[p]])
            b.dma(wd_st[p].t[:], wdn_d.t[e].rearrange("(k p) c -> p k c", p=128), R=[wdn_d], W=[wd_st[p]])
            b.cp(wgu_bf[p].t[:, 0:4, :], wgu_st[p].t[:, 0:4, :], R=[wgu_st[p]], W=[wgu_bf[p]], eng="pool")
            b.cp(wgu_bf[p].t[:, 4:8, :], wgu_st[p].t[:, 4:8, :], R=[wgu_st[p]], W=[wgu_bf[p]], eng="pool")
            for c2 in range(2):
                b.tt(wd_bf[p].t[:, c2, :], wd_st[p].t[:, c2, :], g2bc.t[:], ALU.mult, R=[wd_st[p], g2bc], W=[wd_bf[p]], eng="pool")
            ai = 0
            for tg in range(NTG):
                bks = []
                for fc in range(4):
                    bk = b.bank()
                    bks.append(bk)
                    for k in range(8):
                        b.mm(bk, bk.t[:, 0:MG], wgu_bf[p].t[:, k, fc * 128:(fc + 1) * 128], h2b.t[:, k, tg * MG:(tg + 1) * MG],
                             R=[wgu_bf[p], h2b.sub(tg)], start=(k == 0), stop=(k == 7))
                A = AT[tg % 2]
                for c2 in range(2):
                    sl = slu[c2]
                    b.act(sl.t[:], bks[c2].t[:, 0:MG], AF.Silu, R=[bks[c2]], W=[sl])
                    b.tt(A.t[:, c2, :], sl.t[:], bks[2 + c2].t[:, 0:MG], ALU.mult, R=[sl, bks[2 + c2]], W=[A])
                for ti in range(TPG):
                    tile_ = tg * TPG + ti
                    for hf in range(2):
                        bk = b.bank()
                        for c2 in range(2):
                            b.mm(bk, bk.t[:, 0:512], A.t[:, c2, ti * 128:(ti + 1) * 128], wd_bf[p].t[:, c2, hf * 512:(hf + 1) * 512],
                                 R=[A, wd_bf[p]], start=(c2 == 0), stop=(c2 == 1))
                        b.stt(acc.t[:, tile_, hf * 512:(hf + 1) * 512], bk.t[:, 0:512], wt.t[:, tile_, e:e + 1],
                              acc.t[:, tile_, hf * 512:(hf + 1) * 512], ALU.mult, ALU.add, R=[bk, wt, acc.sub(tile_)], W=[acc.sub(tile_)])
        for tt in range(NT):
            b.dma(outd.t[tt * 128:(tt + 1) * 128, :], acc.t[:, tt, :], R=[acc.sub(tt)], W=[outd.sub(tt)], final=True)


def prep_d(inp, bidx, j, T, oa_b, ob_b):
    m = prep_common(inp, bidx)
    m["consts"] = host_consts(alibi_slope(0))
    w_in = inp["w_in"][0]
    m["xs"] = np.ascontiguousarray(inp["x"][bidx, j * T:(j + 1) * T])
    m["oa_in"] = np.ascontiguousarray(oa_b[j * T:(j + 1) * T])
    m["ob_in"] = np.ascontiguousarray(ob_b[j * T:(j + 1) * T])
    m["w_gates"] = np.ascontiguousarray(w_in[:, W_OFF["ga"]:W_OFF["ga"] + 2048])
    m["w_o_gdn"] = np.ascontiguousarray(inp["w_o_gdn"][0])
    m["w_o_diff"] = np.ascontiguousarray(inp["w_o_diff"][0])
    m["w_out"] = np.ascontiguousarray(inp["w_out"][0])
    m["w_router"] = np.ascontiguousarray(inp["w_router"][0])
    m["rbias"] = np.ascontiguousarray(np.tile(inp["router_bias"][0][None, :], (128, 1)))
    return m


_EXP_CACHE = {}


def expert_stack(inp):
    key = id(inp["w_exp_gate_up"])
    if key not in _EXP_CACHE:
        _EXP_CACHE.clear()
        wgu = np.concatenate([inp["w_exp_gate_up"][0], inp["w_shared_gate_up"][0][None]], axis=0)
        wdn = np.concatenate([inp["w_exp_down"][0], inp["w_shared_down"][0][None]], axis=0)
        _EXP_CACHE[key] = (np.ascontiguousarray(wgu), np.ascontiguousarray(wdn))
    return _EXP_CACHE[key]


def prep_fused(inp, cid, T):
    bi, j = cid // 4, cid % 4
    m = prep_ab(inp, bi, j)
    w_in = inp["w_in"][0]
    m["xs"] = np.ascontiguousarray(inp["x"][bi, j * T:(j + 1) * T])
    m["w_gates"] = np.ascontiguousarray(w_in[:, W_OFF["ga"]:W_OFF["ga"] + 2048])
    m["w_o_gdn"] = np.ascontiguousarray(inp["w_o_gdn"][0])
    m["w_o_diff"] = np.ascontiguousarray(inp["w_o_diff"][0])
    m["w_out"] = np.ascontiguousarray(inp["w_out"][0])
    m["w_router"] = np.ascontiguousarray(inp["w_router"][0])
    m["rbias"] = np.ascontiguousarray(np.tile(inp["router_bias"][0][None, :], (128, 1)))
    sel = np.zeros((128, 4), np.float32)
    sel[:, j] = 1.0
    m["sel"] = sel
    return m


def kernel(**inp):
    inp = {k: np.asarray(v) for k, v in inp.items()}
    B, Sq, _ = inp["x"].shape
    T = Sq // 4
    nc = build_fused(Sq)
    wgu, wdn = expert_stack(inp)
    maps = []
    for cid in range(8):
        m = prep_fused(inp, cid, T)
        m["w_gu"] = wgu
        m["w_dn"] = wdn
        maps.append(m)
    res = run_bass_kernel_spmd(nc, maps, core_ids=list(range(8)))
    out = np.zeros((B, Sq, 1024), np.float32)
    for cid in range(8):
        bi, j = cid // 4, cid % 4
        out[bi, j * T:(j + 1) * T] = res.results[cid]["out"]
    return out
```

```python
import contextlib
import numpy as np
import concourse.bass as bass
import concourse.mybir as mybir
from concourse.bass_utils import run_bass_kernel_spmd

F32 = mybir.dt.float32
BF16 = mybir.dt.bfloat16
I32 = mybir.dt.int32
AF = mybir.ActivationFunctionType
ALU = mybir.AluOpType


class Buf:
    __slots__ = ("name", "w", "r")

    def __init__(self, name):
        self.name = name
        self.w = None
        self.r = []


class Tn:
    def __init__(self, t, name):
        self.t = t
        self.name = name
        self.b = Buf(name)
        self.subs = {}

    def sub(self, key):
        if key not in self.subs:
            self.subs[key] = Buf(f"{self.name}:{key}")
        return self.subs[key]


class Op:
    __slots__ = ("eng", "fn", "deps", "is_dma", "sem", "val", "signals", "final", "inc")

    def __init__(self, eng, fn, is_dma=False):
        self.eng = eng
        self.fn = fn
        self.deps = []
        self.is_dma = is_dma
        self.sem = None
        self.val = None
        self.signals = False
        self.final = False
        self.inc = 1


def _buf(x):
    return x.b if hasattr(x, 'b') else x


class Sched:
    ENGS = ("pe", "act", "dve", "pool", "sp")

    def __init__(self, nc, n_dma_sems=None):
        self.nc = nc
        self.stack = contextlib.ExitStack()
        self.stacks = [self.stack]
        self.ops = {e: [] for e in self.ENGS}
        self.esem = {e: nc.alloc_semaphore(name=f"sem_{e}") for e in self.ENGS}
        nd = n_dma_sems or {"sp": 24, "pool": 12, "act": 4}
        self.dsem = {q: [nc.alloc_semaphore(name=f"dsem_{q}{i}") for i in range(n)] for q, n in nd.items()}
        self.dlast = {q: [None] * n for q, n in nd.items()}
        self.dcnt = {q: [0] * n for q, n in nd.items()}
        self.dnext = {q: 0 for q in nd}
        self.n_ops = 0

    def sb(self, name, shape, dtype=F32):
        t = self.stacks[-1].enter_context(self.nc.sbuf_tensor(name, list(shape), dtype))
        return Tn(t, name)

    @contextlib.contextmanager
    def scope(self):
        st = contextlib.ExitStack()
        self.stacks.append(st)
        try:
            yield
        finally:
            self.barrier()
            self.stacks.pop()
            st.close()

    def barrier(self):
        lasts = []
        for e in self.ENGS:
            for o in reversed(self.ops[e]):
                if o.fn is not None and not o.is_dma:
                    lasts.append(o)
                    break
        for q in self.dlast:
            lasts += [o for o in self.dlast[q] if o is not None]
        for e in self.ENGS:
            o = Op(e, None)
            o.sem = self.esem[e]
            o.deps = list(lasts)
            self.ops[e].append(o)

    def ps(self, name, shape, dtype=F32):
        t = self.stack.enter_context(self.nc.psum_tensor(name, list(shape), dtype))
        return Tn(t, name)

    def dram(self, ap, name):
        return Tn(ap, name)

    def scratch(self, name, shape, dtype=F32):
        t = self.nc.dram_tensor(name, list(shape), dtype, kind="Internal")
        return Tn(t.ap(), name)

    def _track(self, op, R, W):
        eng = op.eng
        deps = []

        def add(d, kind):
            if d is None:
                return
            if not d.is_dma and not op.is_dma and d.eng == eng:
                if eng == "pe":
                    return
            deps.append(d)

        Rb = [_buf(x) for x in R]
        Wb = [_buf(x) for x in W]
        for b in Rb:
            add(b.w, "raw")
        for b in Wb:
            add(b.w, "waw")
            for r in b.r:
                add(r, "war")
        for b in Rb:
            b.r.append(op)
        for b in Wb:
            b.w = op
            b.r = []
        seen = set()
        for d in deps:
            if id(d) not in seen and d is not op:
                seen.add(id(d))
                op.deps.append(d)

    def op(self, eng, fn, R=(), W=()):
        o = Op(eng, fn)
        o.sem = self.esem[eng]
        self._track(o, R, W)
        self.ops[eng].append(o)
        self.n_ops += 1
        return o

    def dma(self, q, out, in_, R=(), W=(), final=False, fn=None, **kw):
        if fn is None:
            fn = lambda e: e.dma_start(out=out, in_=in_, **kw)
        o = Op(q, fn, is_dma=True)
        o.inc = 16
        k = self.dnext[q]
        self.dnext[q] = (k + 1) % len(self.dsem[q])
        o.sem = self.dsem[q][k]
        self.dcnt[q][k] += 16
        o.val = self.dcnt[q][k]
        o.signals = True
        o.final = final
        self._track(o, R, W)
        prev = self.dlast[q][k]
        if prev is not None and prev not in o.deps:
            o.deps.append(prev)
        self.dlast[q][k] = o
        self.ops[q].append(o)
        self.n_ops += 1
        return o

    def cc(self, fn, R=(), W=()):
        if "cc" not in self.dsem:
            self.dsem["cc"] = [self.nc.alloc_semaphore(name="dsem_cc")]
            self.dlast["cc"] = [None]
            self.dcnt["cc"] = [0]
        o = Op("pool", fn, is_dma=True)
        o.inc = 1
        o.sem = self.dsem["cc"][0]
        self.dcnt["cc"][0] += 1
        o.val = self.dcnt["cc"][0]
        o.signals = True
        self._track(o, R, W)
        prev = self.dlast["cc"][0]
        if prev is not None and prev not in o.deps:
            o.deps.append(prev)
        self.dlast["cc"][0] = o
        self.ops["pool"].append(o)
        return o

    def emit(self):
        for e in self.ENGS:
            for o in self.ops[e]:
                for d in o.deps:
                    d.signals = True
        for e in self.ENGS:
            c = 0
            for o in self.ops[e]:
                if o.fn is None:
                    o.signals = False
                    o.val = c
                    continue
                if not o.is_dma and o.signals:
                    c += 1
                    o.val = c
        nc = self.nc

        def replay(ename):
            def run(e):
                waited = {}
                finals = []
                for o in self.ops[ename]:
                    need = {}
                    for d in o.deps:
                        k = id(d.sem)
                        if k not in need or need[k][1] < d.val:
                            need[k] = (d.sem, d.val)
                    for k, (sem, val) in need.items():
                        if waited.get(k, 0) >= val:
                            continue
                        e.wait_ge(sem, val)
                        waited[k] = val
                    if o.fn is None:
                        continue
                    ins = o.fn(e)
                    if o.signals:
                        ins.then_inc(o.sem, o.inc)
                    if o.final:
                        finals.append(o)
                for o in finals:
                    e.wait_ge(o.sem, o.val)
            return run

        with nc.Block() as block:
            block.sync(replay("sp"))
            block.tensor(replay("pe"))
            block.scalar(replay("act"))
            block.vector(replay("dve"))
            block.gpsimd(replay("pool"))
        self.stack.close()


AX = mybir.AxisListType
D = 1024
KC = 8
EPS = 1e-6
LAM_INIT = 0.2
W_OFF = dict(qA=0, kA=512, vA=1024, zA=1536, beta=2048, decay=2052, qB=2056, kB=2568, vB=3080, ga=3592, gb=4616)


class Bld:
    def __init__(self, nc):
        self.nc = nc
        self.S = Sched(nc)
        self.banks = [self.S.ps(f"bank{i}", [128, 512]) for i in range(8)]
        self.bi = 0
        self.bank_pool = list(range(8))
        self.ins = {}

    def inp(self, name, shape, dtype=F32):
        ap = self.nc.dram_tensor(name, list(shape), dtype, kind="ExternalInput").ap()
        t = self.S.dram(ap, name)
        self.ins[name] = t
        return t

    def outp(self, name, shape, dtype=F32):
        ap = self.nc.dram_tensor(name, list(shape), dtype, kind="ExternalOutput").ap()
        return self.S.dram(ap, name)

    def bank(self):
        b = self.banks[self.bank_pool[self.bi % len(self.bank_pool)]]
        self.bi += 1
        return b

    def mm(self, bank, out, lhsT, rhs, R, start=True, stop=True):
        self.S.op("pe", lambda e: e.matmul(out, lhsT, rhs, start=start, stop=stop), R=R, W=[bank])

    def tp(self, bank, out, in_, ident, R):
        self.S.op("pe", lambda e: e.transpose(out, in_, ident), R=R, W=[bank])

    def act(self, out, in_, func, R, W, **kw):
        self.S.op("act", lambda e: e.activation(out=out, in_=in_, func=func, **kw), R=R, W=W)

    def ts(self, out, in0, s1, s2, op0, op1, R, W, eng="dve"):
        if op1 is None:
            self.S.op(eng, lambda e: e.tensor_scalar(out=out, in0=in0, scalar1=s1, scalar2=None, op0=op0), R=R, W=W)
        else:
            self.S.op(eng, lambda e: e.tensor_scalar(out=out, in0=in0, scalar1=s1, scalar2=s2, op0=op0, op1=op1), R=R, W=W)

    def tt(self, out, in0, in1, op, R, W, eng="dve"):
        self.S.op(eng, lambda e: e.tensor_tensor(out=out, in0=in0, in1=in1, op=op), R=R, W=W)

    def stt(self, out, in0, scalar, in1, op0, op1, R, W, eng="dve"):
        self.S.op(eng, lambda e: e.scalar_tensor_tensor(out=out, in0=in0, scalar=scalar, in1=in1, op0=op0, op1=op1), R=R, W=W)

    def cp(self, out, in_, R, W, eng="dve"):
        self.S.op(eng, lambda e: e.tensor_copy(out=out, in_=in_), R=R, W=W)

    def memset(self, ap, val, W, eng="dve"):
        self.S.op(eng, lambda e: e.memset(ap, val), R=[], W=W)

    def dma(self, out, in_, R, W, q="sp", final=False):
        self.S.dma(q, out, in_, R=R, W=W, final=final)

    def rstd(self, out, in_, R, W, scale, eps=EPS, lnbias=0.0):
        self.act(out, in_, AF.Ln, R=R, W=W, scale=scale, bias=eps)
        self.act(out, out, AF.Exp, R=W, W=W, scale=-0.5, bias=lnbias)


def build_consts(b, slope=None):
    S = b.S
    c = {}
    cin = b.inp("consts", [128, 128 * 6])
    ct = S.sb("consts_sb", [128, 128 * 6])
    b.dma(ct.t[:], cin.t[:, :], R=[cin], W=[ct])
    c["t"] = ct
    c["ident"] = ct.t[:, 0:128]
    c["ones"] = ct.t[:, 128:256]
    c["bd"] = ct.t[:, 256:384]
    c["UT"] = ct.t[0:64, 384:448]
    c["LTs"] = ct.t[0:64, 448:512]
    c["Cm"] = ct.t[:, 512:640]
    c["brel"] = ct.t[:, 640:704]
    return c


def host_consts(slope):
    c = np.zeros((128, 768), np.float32)
    c[:, 0:128] = np.eye(128)
    c[:, 128:256] = 1.0
    c[0:64, 256:320] = 1.0
    c[64:128, 320:384] = 1.0
    p = np.arange(64)[:, None]
    f = np.arange(64)[None, :]
    c[0:64, 384:448] = (p <= f)
    c[0:64, 448:512] = (p > f)
    k = np.arange(128)[:, None]
    q = np.arange(128)[None, :]
    cm = np.where(k <= q, 1.0, np.where((k // 64) == (q // 64), np.exp(-2.0 * slope * (k - q)), 0.0))
    c[:, 512:640] = cm
    r = np.arange(64)[None, :]
    c[:, 640:704] = slope * (k + 128.0 * (r - 63) - 127.0)
    return c


def emit_adaln(b, c, want):
    S = b.S
    ccol = b.inp("c_col", [128, 8])
    wada = b.inp("w_ada", [1024, 6144])
    bada = b.inp("b_ada", [1, 6144])
    g1c = b.inp("g1c", [128, 8])
    g2c = b.inp("g2c", [128, 8])
    modc = S.sb("modc", [128, 48])
    ab = S.sb("ab", [128, 32])
    out = {"modc": modc, "ab": ab}
    if want == "gates":
        out["g1bc"] = S.sb("g1bc", [128, 1024])
        out["g2bc"] = S.sb("g2bc", [128, 1024])
    with S.scope():
        _emit_adaln_body(b, c, want, out, ccol, wada, bada, g1c, g2c)
    return out


def _emit_adaln_body(b, c, want, out, ccol, wada, bada, g1c, g2c):
    S = b.S
    modc = out["modc"]
    ab = out["ab"]
    sc = S.sb("sc", [128, 8])
    gt = S.sb("gcols", [128, 16])
    brow = S.sb("brow", [1, 6144])
    modrow = S.sb("modrow", [1, 6144])
    wb = [S.sb(f"wadab{i}", [128, 8, 512]) for i in range(2)]
    b.dma(sc.t[:], ccol.t[:, :], R=[ccol], W=[sc])
    b.dma(gt.t[:, 0:8], g1c.t[:, :], R=[g1c], W=[gt])
    b.dma(gt.t[:, 8:16], g2c.t[:, :], R=[g2c], W=[gt])
    b.dma(brow.t[:], bada.t[:, :], R=[bada], W=[brow])
    b.act(sc.t[:], sc.t[:], AF.Silu, R=[sc], W=[sc])
    for jb in range(12):
        w = wb[jb % 2]
        b.dma(w.t[:], wada.t[:, jb * 512:(jb + 1) * 512].rearrange("(k p) c -> p k c", p=128), R=[wada], W=[w])
        bk = b.bank()
        for k in range(8):
            b.mm(bk, bk.t[0:1, 0:512], sc.t[:, k:k + 1], w.t[:, k, :], R=[sc, w], start=(k == 0), stop=(k == 7))
        b.tt(modrow.t[0:1, jb * 512:(jb + 1) * 512], bk.t[0:1, 0:512], brow.t[0:1, jb * 512:(jb + 1) * 512], ALU.add,
             R=[bk, brow], W=[modrow])
    bk = b.bank()
    for j in range(48):
        b.mm(bk, bk.t[:, j:j + 1], modrow.t[0:1, j * 128:(j + 1) * 128], c["ones"][0:1, 0:1], R=[modrow, c["t"]])
    b.cp(modc.t[:], bk.t[:, 0:48], R=[bk], W=[modc])
    b.ts(ab.t[:, 0:8], modc.t[:, 8:16], 1.0, None, ALU.add, None, R=[modc], W=[ab])
    b.tt(ab.t[:, 0:8], ab.t[:, 0:8], gt.t[:, 0:8], ALU.mult, R=[ab, gt], W=[ab])
    b.cp(ab.t[:, 8:16], modc.t[:, 0:8], R=[modc], W=[ab])
    b.ts(ab.t[:, 16:24], modc.t[:, 32:40], 1.0, None, ALU.add, None, R=[modc], W=[ab])
    b.tt(ab.t[:, 16:24], ab.t[:, 16:24], gt.t[:, 8:16], ALU.mult, R=[ab, gt], W=[ab])
    b.cp(ab.t[:, 24:32], modc.t[:, 24:32], R=[modc], W=[ab])
    if want == "gates":
        g1bc = out["g1bc"]
        g2bc = out["g2bc"]
        for (dst, off) in ((g1bc, 2048), (g2bc, 5120)):
            for hf in range(2):
                bk = b.bank()
                b.mm(bk, bk.t[:, 0:512], c["ones"][0:1, 0:128], modrow.t[0:1, off + hf * 512: off + (hf + 1) * 512],
                     R=[modrow, c["t"]])
                b.cp(dst.t[:, hf * 512:(hf + 1) * 512], bk.t[:, 0:512], R=[bk], W=[dst])
    return out


def emit_norm_hT(b, c, xsrc, row0, ntiles, acol, bcol, hT, xt_bufs, ssq, hT_dtype_ap=None, x_keep=None):
    S = b.S
    for tt in range(ntiles):
        xt = xt_bufs[tt % len(xt_bufs)] if x_keep is None else x_keep[tt]
        b.dma(xt.t[:], xsrc.t[row0 + tt * 128: row0 + (tt + 1) * 128, :], R=[xsrc], W=[xt])
    return


def norm_tile(b, c, xt, xn, ss, acol, bcol, hT, col0, scratch):
    b.act(scratch.t[:], xt.t[:], AF.Square, R=[xt], W=[scratch, ss], accum_out=ss.t[:, 0:1])
    b.rstd(ss.t[:, 0:1], ss.t[:, 0:1], R=[ss], W=[ss], scale=1.0 / D)
    b.ts(xn.t[:], xt.t[:], ss.t[:, 0:1], None, ALU.mult, None, R=[xt, ss], W=[xn])
    for k2 in range(2):
        bk = b.bank()
        for kk in range(4):
            k = k2 * 4 + kk
            b.tp(bk, bk.t[:, kk * 128:(kk + 1) * 128], xn.t[:, k * 128:(k + 1) * 128], c["ident"], R=[xn, c["t"]])
        for kk in range(4):
            k = k2 * 4 + kk
            b.act(hT[:, k, col0:col0 + 128], bk.t[:, kk * 128:(kk + 1) * 128], AF.Identity, R=[bk, acol[1]], W=[acol[2]],
                  scale=acol[0][:, k:k + 1], bias=bcol[0][:, k:k + 1])


def norm_tile2(b, c, xt, xn, ss, scratch, ab, aoff, hT, hTt, col0, xap=None, cols=None):
    if xap is None:
        xap = xt.t[:]
    b.act(scratch.t[:], xap, AF.Square, R=[xt], W=[scratch, ss], accum_out=ss.t[:, 0:1])
    b.rstd(ss.t[:, 0:1], ss.t[:, 0:1], R=[ss], W=[ss], scale=1.0 / D)
    b.ts(xn.t[:], xap, ss.t[:, 0:1], None, ALU.mult, None, R=[xt, ss], W=[xn])
    for k2 in range(2):
        bk = b.bank()
        for kk in range(4):
            k = k2 * 4 + kk
            b.tp(bk, bk.t[:, kk * 128:(kk + 1) * 128], xn.t[:, k * 128:(k + 1) * 128], c["ident"], R=[xn, c["t"]])
        for kk in range(4):
            k = k2 * 4 + kk
            for di, (dst, dstT) in enumerate(zip(hT, hTt)):
                cc0 = col0 if cols is None else cols[di]
                b.act(dst[:, k, cc0:cc0 + 128], bk.t[:, kk * 128:(kk + 1) * 128], AF.Identity, R=[bk, ab], W=[dstT],
                      scale=ab.t[:, aoff + k:aoff + k + 1], bias=ab.t[:, aoff + 8 + k:aoff + 9 + k])


def build_ab(Sq):
    nc = bass.Bass("TRN2", target_bir_lowering=False)
    b = Bld(nc)
    c = build_consts(b)
    ad = emit_adaln(b, c, want="none")
    send = b.outp("send", [Sq, 256])
    emit_ab(b, c, ad["ab"], Sq, lambda r0, n, c0, key: (send.t[r0:r0 + n, c0:c0 + 128], send.sub(key)))
    b.S.emit()
    return nc


def emit_ab(b, c, ab, Sq, send_at, after_group=None):
    with b.S.scope():
        _emit_ab_body(b, c, ab, Sq, send_at, after_group)


def _emit_ab_body(b, c, ab, Sq, send_at, after_group):
    S = b.S
    NG = Sq // 512
    xb = b.inp("xb", [Sq, 1024])
    wcm_d = b.inp("w_cm", [1024, 640])
    wzbd_d = b.inp("w_zbd", [1024, 130])
    wvb_d = b.inp("w_vb", [1024, 128])
    small_d = b.inp("small", [128, 384])
    lam_d = b.inp("lamrow", [1, 256])

    wcm = S.sb("wcm", [128, 8, 640])
    wzbd = S.sb("wzbd", [128, 8, 130])
    wvb = S.sb("wvb", [128, 8, 128])
    small = S.sb("small_sb", [128, 384])
    lamr = S.sb("lamr", [1, 256])
    b.dma(wcm.t[:], wcm_d.t[:, :].rearrange("(k p) c -> p k c", p=128), R=[wcm_d], W=[wcm])
    b.dma(wzbd.t[:], wzbd_d.t[:, :].rearrange("(k p) c -> p k c", p=128), R=[wzbd_d], W=[wzbd])
    b.dma(wvb.t[:], wvb_d.t[:, :].rearrange("(k p) c -> p k c", p=128), R=[wvb_d], W=[wvb])
    b.dma(small.t[:], small_d.t[:, :], R=[small_d], W=[small])
    b.dma(lamr.t[:], lam_d.t[:, :], R=[lam_d], W=[lamr])
    convw = lambda j, tap: small.t[:, j * 4 + tap: j * 4 + tap + 1]
    gng = small.t[0:64, 128:256]
    sub_g = S.sb("sub_g", [128, 128])
    b.ts(sub_g.t[:], small.t[:, 256:384], 1.0 - LAM_INIT, None, ALU.mult, None, R=[small], W=[sub_g])
    negA = S.sb("negA", [128, 1])
    b.act(negA.t[:], small.t[:, 12:13], AF.Exp, R=[small], W=[negA])
    b.ts(negA.t[:], negA.t[:], -1.0, None, ALU.mult, None, R=[negA], W=[negA])
    lt = S.sb("lam_t", [1, 136])
    b.tt(lt.t[0:1, 0:64], lamr.t[0:1, 0:64], lamr.t[0:1, 64:128], ALU.mult, R=[lamr], W=[lt])
    b.tt(lt.t[0:1, 64:128], lamr.t[0:1, 128:192], lamr.t[0:1, 192:256], ALU.mult, R=[lamr], W=[lt])
    b.S.op("dve", lambda e: e.tensor_reduce(out=lt.t[0:1, 128:129], in_=lt.t[0:1, 0:64], axis=AX.X, op=ALU.add), R=[lt], W=[lt])
    b.S.op("dve", lambda e: e.tensor_reduce(out=lt.t[0:1, 129:130], in_=lt.t[0:1, 64:128], axis=AX.X, op=ALU.add), R=[lt], W=[lt])
    b.act(lt.t[0:1, 128:130], lt.t[0:1, 128:130], AF.Exp, R=[lt], W=[lt])
    b.tt(lt.t[0:1, 130:131], lt.t[0:1, 129:130], lt.t[0:1, 128:129], ALU.subtract, R=[lt], W=[lt])
    b.ts(lt.t[0:1, 130:131], lt.t[0:1, 130:131], -LAM_INIT, None, ALU.add, None, R=[lt], W=[lt])
    neglam = S.sb("neglam", [128, 1])
    bk = b.bank()
    b.mm(bk, bk.t[:, 0:1], c["ones"][0:1, 0:128], lt.t[0:1, 130:131], R=[lt, c["t"]])
    b.cp(neglam.t[:], bk.t[:, 0:1], R=[bk], W=[neglam])

    qT = S.sb("qT_all", [128, Sq])
    kT = S.sb("kT_all", [128, Sq])
    Vx = S.sb("Vext", [128, Sq // 128, 130])
    b.memset(Vx.t[:, :, 128:130], 1.0, W=[Vx])
    hT = [S.sb(f"hT{i}", [128, 8, 512]) for i in range(1)]
    xt = [S.sb(f"xt{i}", [128, 1024]) for i in range(1)]
    xn = S.sb("xn", [128, 1024])
    scr = xn
    ssx = S.sb("ssx", [128, 1])
    pre = [S.sb(f"pre{j}", [128, 515]) for j in range(3)]
    for j in range(3):
        b.memset(pre[j].t[:, 0:3], 0.0, W=[pre[j]])
    cv = [S.sb(f"cv{j}", [128, 512]) for j in range(3)]
    sq = S.sb("sq", [128, 512])
    rs = S.sb("rs", [128, 512])
    qk_tmp = S.sb("qk_tmp", [128, 512])
    St = S.sb("state", [128, 128])
    b.memset(St.t[:], 0.0, W=[St])
    def cb(name, shape):
        return [S.sb(f"{name}{i}", shape) for i in range(2)]
    zb = cb("zb", [64, 132]); gcol = cb("gcol", [64, 8]); e1 = cb("e1", [64, 64]); dts = cb("dts", [64, 64])
    dcs = cb("dcs", [64, 64]); Mb = [cb("Ma", [64, 64]), cb("Mb", [64, 64])]; Nb = [cb("Na", [64, 64]), cb("Nb", [64, 64])]
    Tb = [cb("Ta", [64, 64]), cb("Tb", [64, 64])]; ktm = cb("ktm", [64, 128]); kbt = cb("kbt", [64, 128])
    vbt = cb("vbt", [64, 128]); kdec = cb("kdec", [64, 128]); Ub = cb("Ub", [64, 128]); WTb = cb("WTb", [128, 64])
    AqT = cb("AqT", [64, 64]); gl = cb("gl", [128, 2]); vnew = cb("vnew", [64, 128]); o1 = cb("o1", [64, 128])
    ob_ = cb("ob_", [64, 128]); szb = cb("szb", [64, 128]); tmp64 = cb("tmp64", [64, 64]); oast = cb("oast", [64, 128])
    ssg = cb("ssg", [64, 2])
    PT = [S.sb(f"PT{i}", [128, 512]) for i in range(2)]
    obacc = [S.sb(f"obacc{i}", [128, 128]) for i in range(4)]
    rz = S.sb("rz", [128, 8])
    obst = [S.sb(f"obst{i}", [128, 128]) for i in range(2)]
    sso = S.sb("sso", [128, 2])
    ident = c["ident"]; ct = c["t"]
    SCALE = 64 ** -0.5

    for g in range(NG):
        h = hT[0]
        for tt in range(4):
            x_ = xt[0]
            b.dma(x_.t[:], xb.t[g * 512 + tt * 128: g * 512 + (tt + 1) * 128, :], R=[xb], W=[x_])
            norm_tile2(b, c, x_, xn, ssx, scr, ab, 0, [h.t], [h], tt * 128)
        for j in range(5):
            bk = b.bank()
            for k in range(8):
                b.mm(bk, bk.t[:, 0:512], wcm.t[:, k, j * 128:(j + 1) * 128], h.t[:, k, :], R=[wcm, h], start=(k == 0), stop=(k == 7))
            if j < 3:
                b.act(pre[j].t[:, 3:515], bk.t[:, 0:512], AF.Copy, R=[bk], W=[pre[j]])
                y = cv[j]
                b.ts(y.t[:], pre[j].t[:, 0:512], convw(j, 0), None, ALU.mult, None, R=[pre[j], small], W=[y])
                for tap in range(1, 4):
                    b.stt(y.t[:], pre[j].t[:, tap:tap + 512], convw(j, tap), y.t[:], ALU.mult, ALU.add, R=[pre[j], small, y], W=[y])
                b.cp(pre[j].t[:, 0:3], pre[j].t[:, 512:515], R=[pre[j]], W=[pre[j]])
                b.act(y.t[:], y.t[:], AF.Silu, R=[y], W=[y])
                if j < 2:
                    b.tt(sq.t[:], y.t[:], y.t[:], ALU.mult, R=[y], W=[sq])
                    bk2 = b.bank()
                    b.mm(bk2, bk2.t[:, 0:512], c["ones"], sq.t[:], R=[sq, ct])
                    b.act(rs.t[:], bk2.t[:, 0:512], AF.Ln, R=[bk2], W=[rs], bias=EPS)
                    b.act(rs.t[:], rs.t[:], AF.Exp, R=[rs], W=[rs], scale=-0.5, bias=(float(np.log(128 ** -0.5)) if j == 0 else 0.0))
                    b.tt(y.t[:], y.t[:], rs.t[:], ALU.mult, R=[y, rs], W=[y])
            else:
                dst = qT if j == 3 else kT
                gcolm = small.t[:, 14:15] if j == 3 else small.t[:, 15:16]
                b.act(qk_tmp.t[:], bk.t[:, 0:512], AF.Copy, R=[bk], W=[qk_tmp])
                b.tt(sq.t[:], qk_tmp.t[:], qk_tmp.t[:], ALU.mult, R=[qk_tmp], W=[sq])
                bk2 = b.bank()
                b.mm(bk2, bk2.t[:, 0:512], c["bd"], sq.t[:], R=[sq, ct])
                b.act(rs.t[:], bk2.t[:, 0:512], AF.Ln, R=[bk2], W=[rs], scale=1.0 / 64, bias=EPS)
                b.act(rs.t[:], rs.t[:], AF.Exp, R=[rs], W=[rs], scale=-0.5)
                b.stt(dst.t[:, g * 512:(g + 1) * 512], qk_tmp.t[:], gcolm, rs.t[:], ALU.mult, ALU.mult, R=[qk_tmp, small, rs], W=[dst.sub(g)])
        for tt in range(4):
            bk = b.bank()
            for k in range(8):
                b.mm(bk, bk.t[:, 0:128], h.t[:, k, tt * 128:(tt + 1) * 128], wvb.t[:, k, :], R=[wvb, h], start=(k == 0), stop=(k == 7))
            b.cp(Vx.t[:, g * 4 + tt, 0:128], bk.t[:, 0:128], R=[bk], W=[Vx.sub(g)])
        qn, kn, vs = cv[0], cv[1], cv[2]
        def chunk_pre(ch):
            p = ch % 2
            t0 = ch * 64
            kTc = kn.t[:, t0:t0 + 64]; qTc = qn.t[:, t0:t0 + 64]
            bk = b.bank()
            for k in range(8):
                b.mm(bk, bk.t[0:64, 0:130], h.t[:, k, t0:t0 + 64], wzbd.t[:, k, :], R=[wzbd, h], start=(k == 0), stop=(k == 7))
            z = zb[p]
            b.cp(z.t[:, 0:130], bk.t[0:64, 0:130], R=[bk], W=[z])
            yield
            gc = gcol[p]
            b.act(gc.t[:, 0:1], z.t[:, 128:129], AF.Sigmoid, R=[z], W=[gc])
            yield
            b.act(gc.t[:, 1:2], z.t[:, 129:130], AF.Exp, R=[z, small], W=[gc], bias=small.t[0:64, 13:14])
            yield
            b.act(gc.t[:, 1:2], gc.t[:, 1:2], AF.Ln, R=[gc], W=[gc], bias=1.0)
            yield
            b.tt(gc.t[:, 1:2], gc.t[:, 1:2], negA.t[0:64, 0:1], ALU.mult, R=[gc, negA], W=[gc])
            yield
            bk = b.bank()
            b.mm(bk, bk.t[0:64, 0:1], c["UT"], gc.t[:, 1:2], R=[gc, ct])
            yield
            b.cp(gc.t[:, 2:3], bk.t[0:64, 0:1], R=[bk], W=[gc])
            yield
            t64 = tmp64[p]
            b.ts(t64.t[:], c["UT"], gc.t[:, 1:2], None, ALU.mult, None, R=[gc, ct], W=[t64])
            yield
            bk = b.bank()
            b.mm(bk, bk.t[0:64, 0:64], c["ones"][0:64, 0:64], t64.t[:], R=[t64, ct])
            yield
            E = e1[p]
            b.ts(E.t[:], bk.t[0:64, 0:64], gc.t[:, 2:3], None, ALU.subtract, None, R=[bk, gc], W=[E])
            yield
            Dt = dts[p]; Dc = dcs[p]
            b.ts(Dt.t[:], E.t[:], 0.0, None, ALU.min, None, R=[E], W=[Dt])
            yield
            b.act(Dt.t[:], Dt.t[:], AF.Exp, R=[Dt], W=[Dt])
            yield
            b.tt(Dt.t[:], Dt.t[:], c["UT"], ALU.mult, R=[Dt, ct], W=[Dt])
            yield
            b.ts(Dc.t[:], E.t[:], -1.0, 0.0, ALU.mult, ALU.min, R=[E], W=[Dc])
            yield
            b.act(Dc.t[:], Dc.t[:], AF.Exp, R=[Dc], W=[Dc])
            yield
            b.tt(Dc.t[:], Dc.t[:], c["LTs"], ALU.mult, R=[Dc, ct], W=[Dc])
            yield
            bk = b.bank()
            b.mm(bk, bk.t[0:64, 0:64], kTc, kTc, R=[kn])
            yield
            M = Mb[0][p]; N = Nb[0][p]; T = Tb[0][p]
            b.stt(M.t[:], bk.t[0:64, 0:64], gc.t[:, 0:1], Dc.t[:], ALU.mult, ALU.mult, R=[bk, gc, Dc], W=[M])
            yield
            bk = b.bank()
            b.tp(bk, bk.t[0:64, 0:64], M.t[:], ident[0:64, 0:64], R=[M, ct])
            yield
            b.cp(N.t[:], bk.t[0:64, 0:64], R=[bk], W=[N])
            yield
            b.tt(T.t[:], ident[0:64, 0:64], N.t[:], ALU.subtract, R=[N, ct], W=[T])
            yield
            for s in range(1, 6):
                M2 = Mb[s % 2][p]; N2 = Nb[s % 2][p]; T2 = Tb[s % 2][p]
                bk = b.bank()
                b.mm(bk, bk.t[0:64, 0:64], N.t[:], M.t[:], R=[N, M])
                yield
                b.act(M2.t[:], bk.t[0:64, 0:64], AF.Copy, R=[bk], W=[M2])
                yield
                if s < 5:
                    bk = b.bank()
                    b.mm(bk, bk.t[0:64, 0:64], M.t[:], N.t[:], R=[N, M])
                    yield
                    b.cp(N2.t[:], bk.t[0:64, 0:64], R=[bk], W=[N2])
                    yield
                bk = b.bank()
                b.mm(bk, bk.t[0:64, 0:64], M2.t[:], T.t[:], R=[M2, T])
                yield
                b.tt(T2.t[:], bk.t[0:64, 0:64], T.t[:], ALU.add, R=[bk, T], W=[T2])
                yield
                M, N, T = M2, N2, T2
            bk = b.bank()
            b.tp(bk, bk.t[0:64, 0:128], kTc, ident, R=[kn, ct])
            yield
            b.tp(bk, bk.t[0:64, 128:256], vs.t[:, t0:t0 + 64], ident, R=[vs, ct])
            yield
            b.act(gc.t[:, 3:4], gc.t[:, 2:3], AF.Exp, R=[gc], W=[gc])
            yield
            b.tt(gc.t[:, 4:5], gc.t[:, 3:4], gc.t[:, 0:1], ALU.mult, R=[gc], W=[gc])
            yield
            b.cp(ktm[p].t[:], bk.t[0:64, 0:128], R=[bk], W=[ktm[p]])
            yield
            b.ts(kbt[p].t[:], bk.t[0:64, 0:128], gc.t[:, 4:5], None, ALU.mult, None, R=[bk, gc], W=[kbt[p]])
            yield
            b.ts(vbt[p].t[:], bk.t[0:64, 128:256], gc.t[:, 0:1], None, ALU.mult, None, R=[bk, gc], W=[vbt[p]])
            yield
            bk = b.bank()
            b.mm(bk, bk.t[0:64, 0:128], T.t[:], vbt[p].t[:], R=[T, vbt[p]])
            yield
            b.cp(Ub[p].t[:], bk.t[0:64, 0:128], R=[bk], W=[Ub[p]])
            yield
            bk = b.bank()
            b.mm(bk, bk.t[:, 0:64], kbt[p].t[:], T.t[:], R=[T, kbt[p]])
            yield
            b.act(WTb[p].t[:], bk.t[:, 0:64], AF.Copy, R=[bk], W=[WTb[p]])
            yield
            bk = b.bank()
            b.mm(bk, bk.t[0:64, 0:64], kTc, qTc, R=[kn, qn])
            yield
            b.tt(AqT[p].t[:], bk.t[0:64, 0:64], Dt.t[:], ALU.mult, R=[bk, Dt], W=[AqT[p]])
            yield
            bk = b.bank()
            b.mm(bk, bk.t[:, 0:1], c["ones"][0:64, 0:128], gc.t[:, 1:2], R=[gc, ct])
            yield
            b.cp(gl[p].t[:, 0:1], bk.t[:, 0:1], R=[bk], W=[gl[p]])
            yield
            b.act(gl[p].t[:, 1:2], gl[p].t[:, 0:1], AF.Exp, R=[gl[p]], W=[gl[p]])
            yield
            b.act(gc.t[:, 5:6], gc.t[:, 2:3], AF.Exp, R=[gc, gl[p]], W=[gc], scale=-1.0, bias=gl[p].t[0:64, 0:1])
            yield
            b.ts(kdec[p].t[:], ktm[p].t[:], gc.t[:, 5:6], None, ALU.mult, None, R=[ktm[p], gc], W=[kdec[p]])
            yield
            yield

        def chunk_seq(ch):
            p = ch % 2
            t0 = ch * 64
            qTc = qn.t[:, t0:t0 + 64]
            z = zb[p]
            gc = gcol[p]
            bkv = b.bank()
            b.mm(bkv, bkv.t[0:64, 0:128], WTb[p].t[:], St.t[:], R=[WTb[p], St])
            b.tt(vnew[p].t[:], Ub[p].t[:], bkv.t[0:64, 0:128], ALU.subtract, R=[bkv, Ub[p]], W=[vnew[p]])
            bk1 = b.bank()
            b.mm(bk1, bk1.t[0:64, 0:128], qTc, St.t[:], R=[qn, St])
            b.act(o1[p].t[:], bk1.t[0:64, 0:128], AF.Copy, R=[bk1, gc], W=[o1[p]], scale=gc.t[:, 3:4])
            bk2 = b.bank()
            b.mm(bk2, bk2.t[0:64, 0:128], AqT[p].t[:], vnew[p].t[:], R=[AqT[p], vnew[p]])
            b.tt(ob_[p].t[:], bk2.t[0:64, 0:128], o1[p].t[:], ALU.add, R=[bk2, o1[p]], W=[ob_[p]])
            bks = b.bank()
            b.mm(bks, bks.t[:, 0:128], kdec[p].t[:], vnew[p].t[:], R=[kdec[p], vnew[p]])
            b.stt(St.t[:], St.t[:], gl[p].t[:, 1:2], bks.t[:, 0:128], ALU.mult, ALU.add, R=[St, gl[p], bks], W=[St])
            b.act(szb[p].t[:], ob_[p].t[:], AF.Square, R=[ob_[p]], W=[szb[p], ssg[p]], accum_out=ssg[p].t[:, 0:1])
            b.rstd(ssg[p].t[:, 0:1], ssg[p].t[:, 0:1], R=[ssg[p]], W=[ssg[p]], scale=1.0 / 128)
            b.stt(ob_[p].t[:], ob_[p].t[:], ssg[p].t[:, 0:1], gng, ALU.mult, ALU.mult, R=[ob_[p], ssg[p], small], W=[ob_[p]])
            b.act(szb[p].t[:], z.t[:, 0:128], AF.Silu, R=[z], W=[szb[p]])
            b.tt(oast[p].t[:], ob_[p].t[:], szb[p].t[:], ALU.mult, R=[ob_[p], szb[p]], W=[oast[p]])
            sap, sbuf_ = send_at(g * 512 + t0, 64, 0, ("a", g, ch))
            b.dma(sap, oast[p].t[:], R=[oast[p]], W=[sbuf_], final=True)
        def gdn_gen():
            for pr in range(4):
                gens = [chunk_pre(2 * pr), chunk_pre(2 * pr + 1)]
                while gens:
                    for gnr in list(gens):
                        try:
                            next(gnr)
                        except StopIteration:
                            gens.remove(gnr)
                    yield
                chunk_seq(2 * pr)
                yield
                chunk_seq(2 * pr + 1)
                yield

        def attn_gen():
            for comp in range(2):
                lo, hi = comp * 64, (comp + 1) * 64
                nkb = 4 * g + 4
                for kb in range(nkb):
                    rel = kb - 4 * g
                    qlo = max(rel, 0) * 128
                    ps = b.banks[0]
                    b.mm(ps, ps.t[:, qlo:512], kT.t[lo:hi, kb * 128:(kb + 1) * 128], qT.t[lo:hi, g * 512 + qlo:(g + 1) * 512],
                         R=[kT.sub(kb // 4), qT.sub(g)])
                    P = PT[kb % 2]
                    for qs in range(max(rel, 0), 4):
                        r_ = kb - (4 * g + qs) + 63
                        b.act(P.t[:, qs * 128:(qs + 1) * 128], ps.t[:, qs * 128:(qs + 1) * 128], AF.Exp, R=[ps, ct], W=[P], scale=SCALE,
                              bias=c["brel"][:, r_:r_ + 1])
                    if rel >= 0:
                        b.tt(P.t[:, rel * 128:(rel + 1) * 128], P.t[:, rel * 128:(rel + 1) * 128], c["Cm"], ALU.mult, R=[P, ct], W=[P])
                    yield
                    for qs in range(max(rel, 0), 4):
                        po = b.banks[4 + qs]
                        b.mm(po, po.t[:, 0:130], P.t[:, qs * 128:(qs + 1) * 128], Vx.t[:, kb, 0:130], R=[P, Vx.sub(kb // 4)],
                             start=(kb == 0), stop=(kb == 4 * g + qs))
                    yield
                for qs in range(4):
                    po = b.banks[4 + qs]
                    rcol = rz.t[:, comp * 4 + qs: comp * 4 + qs + 1]
                    b.S.op("dve", lambda e, rcol=rcol, po=po: e.reciprocal(out=rcol, in_=po.t[:, 128:129]), R=[po], W=[rz])
                    if comp == 0:
                        b.ts(obacc[qs].t[:], po.t[:, 0:128], rcol, None, ALU.mult, None, R=[po, rz], W=[obacc[qs]])
                    else:
                        b.tt(rcol, rcol, neglam.t[:, 0:1], ALU.mult, R=[rz, neglam], W=[rz])
                        b.stt(obacc[qs].t[:], po.t[:, 0:128], rcol, obacc[qs].t[:], ALU.mult, ALU.add, R=[po, rz, obacc[qs]], W=[obacc[qs]])
                        o_ = obst[qs % 2]
                        b.act(o_.t[:], obacc[qs].t[:], AF.Square, R=[obacc[qs]], W=[o_, sso], accum_out=sso.t[:, 0:1])
                        b.rstd(sso.t[:, 0:1], sso.t[:, 0:1], R=[sso], W=[sso], scale=1.0 / 128)
                        b.stt(o_.t[:], obacc[qs].t[:], sso.t[:, 0:1], sub_g.t[:], ALU.mult, ALU.mult, R=[obacc[qs], sso, sub_g], W=[o_])
                        sap, sbuf_ = send_at(g * 512 + qs * 128, 128, 128, ("b", g, qs))
                        b.dma(sap, o_.t[:], R=[o_], W=[sbuf_], final=True)
        b.bank_pool = [1, 2, 3]
        gG, gA = gdn_gen(), attn_gen()
        nA_est = 2 * (4 * g + 4) * 2 + 2
        nG_est = 4 * 75
        ratio = nG_est / nA_est
        accr = 0.0
        aliveG = aliveA = True
        while aliveG or aliveA:
            if aliveA:
                try:
                    next(gA)
                except StopIteration:
                    aliveA = False
            accr += ratio
            n_g = int(accr) if aliveA else 1
            accr -= int(accr)
            for _ in range(max(n_g, 0)):
                if not aliveG:
                    break
                try:
                    next(gG)
                except StopIteration:
                    aliveG = False
        b.bank_pool = list(range(8))
        if after_group is not None:
            after_group(g)


def _cols(v):
    return np.ascontiguousarray(np.asarray(v, np.float32).reshape(-1, 128).T)


def alibi_slope(h):
    return float(2.0 ** (-8.0 * (h + 1) / 4))


def prep_common(inp, bidx):
    return {
        "c_col": _cols(inp["c"][bidx]),
        "w_ada": np.ascontiguousarray(inp["w_ada"][0]),
        "b_ada": np.ascontiguousarray(inp["b_ada"][0].reshape(1, -1)),
        "g1c": _cols(inp["norm1_g"][0]),
        "g2c": _cols(inp["norm2_g"][0]),
    }


def prep_ab(inp, bidx, h):
    w_in = inp["w_in"][0]
    sl = lambda name, width=128: w_in[:, W_OFF[name] + h * width: W_OFF[name] + (h + 1) * width]
    m = prep_common(inp, bidx)
    m["consts"] = host_consts(alibi_slope(h))
    m["xb"] = np.ascontiguousarray(inp["x"][bidx])
    m["w_cm"] = np.ascontiguousarray(np.concatenate([sl("qA"), sl("kA"), sl("vA"), sl("qB"), sl("kB")], axis=1))
    m["w_zbd"] = np.ascontiguousarray(np.concatenate([sl("zA"), sl("beta", 1), sl("decay", 1)], axis=1))
    m["w_vb"] = np.ascontiguousarray(sl("vB"))
    small = np.zeros((128, 384), np.float32)
    cw = inp["conv_w"][0]
    for j in range(3):
        small[:, j * 4:(j + 1) * 4] = cw[:, j * 512 + h * 128: j * 512 + (h + 1) * 128].T
    small[:, 12] = inp["a_log"][0, h]
    small[:, 13] = inp["dt_bias"][0, h]
    small[:, 14] = np.tile(inp["q_norm_g"][0], 2)
    small[:, 15] = np.tile(inp["k_norm_g"][0], 2)
    small[:, 128:256] = np.tile(inp["gdn_norm_g"][0][None, :], (128, 1))
    small[:, 256:384] = np.tile(inp["subln_g"][0][None, :], (128, 1))
    m["small"] = small
    m["lamrow"] = np.ascontiguousarray(np.concatenate(
        [inp["lambda_q1"][0], inp["lambda_k1"][0], inp["lambda_q2"][0], inp["lambda_k2"][0]]).reshape(1, 256))
    return m


def build_d(Sq, n_exp=257):
    nc = bass.Bass("TRN2", target_bir_lowering=False)
    b = Bld(nc)
    c = build_consts(b)
    ad = emit_adaln(b, c, want="gates")
    emit_d(b, c, ad, Sq, None, n_exp)
    b.S.emit()
    return nc


def build_fused(Sq, n_exp=257):
    nc = bass.Bass("TRN2", target_bir_lowering=False)
    b = Bld(nc)
    S = b.S
    c = build_consts(b)
    ad = emit_adaln(b, c, want="gates")
    CH = min(1024, Sq)
    NCH = Sq // CH
    sends = [S.dram(nc.dram_tensor(f"send_bounce{i}", [CH, 256], F32).ap(), f"send{i}") for i in range(NCH)]
    gaths = [S.dram(nc.dram_tensor(f"gath_bounce{i}", [4 * CH, 256], F32).ap(), f"gath{i}") for i in range(NCH)]

    def send_at(r0, n, c0, key):
        ch, lr = r0 // CH, r0 % CH
        return sends[ch].t[lr:lr + n, c0:c0 + 128], sends[ch].sub(key)

    def after_group(g):
        if ((g + 1) * 512) % CH == 0:
            ch = ((g + 1) * 512) // CH - 1
            sd, gt = sends[ch], gaths[ch]
            S.cc(lambda e: e.collective_compute("AllGather", ALU.bypass, replica_groups=[[0, 1, 2, 3], [4, 5, 6, 7]],
                                                ins=[sd.t.opt()], outs=[gt.t.opt()]),
                 R=[sd] + list(sd.subs.values()), W=[gt])

    emit_ab(b, c, ad["ab"], Sq, send_at, after_group)
    emit_d(b, c, ad, Sq, (gaths, CH), n_exp)
    S.emit()
    return nc


def emit_d(b, c, ad, Sq, gath, n_exp):
    S = b.S
    nc = b.nc
    T = Sq // 4
    NT = T // 128
    MG = min(512, T)
    NTG = T // MG
    TPG = MG // 128
    ab, g1bc, g2bc = ad["ab"], ad["g1bc"], ad["g2bc"]
    ident = c["ident"]; ct = c["t"]
    xs = b.inp("xs", [T, 1024])
    if gath is None:
        oa_in = b.inp("oa_in", [T, 512])
        ob_in = b.inp("ob_in", [T, 512])
    else:
        sel_d = b.inp("sel", [128, 4])
    wg_d = b.inp("w_gates", [1024, 2048])
    wog_d = b.inp("w_o_gdn", [512, 1024])
    wod_d = b.inp("w_o_diff", [512, 1024])
    wout_d = b.inp("w_out", [1024, 1024])
    wr_d = b.inp("w_router", [1024, 256])
    rb_d = b.inp("rbias", [128, 256])
    wgu_d = b.inp("w_gu", [n_exp, 1024, 512])
    wdn_d = b.inp("w_dn", [n_exp, 256, 1024])
    outd = b.outp("out", [T, 1024])

    acc = S.sb("acc", [128, NT, 1024])
    h2b = S.sb("h2T_bf", [128, 8, T], BF16)
    wt = S.sb("wt_all", [128, NT, n_exp + 1])
    b.memset(wt.t[:, :, 256:n_exp + 1], 1.0, W=[wt])
    with S.scope():
        wr = S.sb("wr", [128, 8, 256]); rb = S.sb("rb", [128, 256])
        b.dma(wr.t[:], wr_d.t[:, :].rearrange("(k p) c -> p k c", p=128), R=[wr_d], W=[wr])
        b.dma(rb.t[:], rb_d.t[:, :], R=[rb_d], W=[rb])
        wgb = [S.sb(f"wgb{i}", [128, 8, 512]) for i in range(2)]
        xt = [S.sb(f"dxt{i}", [128, 1024]) for i in range(1)]
        xn = S.sb("dxn", [128, 1024]); ssx = S.sb("dssx", [128, 1])
        hT1 = S.sb("hT1", [128, 8, 128]); h2f = hT1
        sg = S.sb("sg", [128, 2048])
        if gath is None:
            oat = S.sb("oat", [128, 512]); obt = S.sb("obt", [128, 512])
        else:
            cand = [S.sb(f"cand{i}", [128, 4, 256]) for i in range(2)]
            cmb = S.sb("cmb", [128, 4, 256])
            sel = S.sb("sel_sb", [128, 4])
            b.dma(sel.t[:], sel_d.t[:, :], R=[sel_d], W=[sel])
            gaths, CH = gath
        oaT = S.sb("oaT", [128, 4, 128]); obT = S.sb("obT", [128, 4, 128])
        mg = S.sb("merged", [128, 1024]); tmp = S.sb("dtmp", [128, 512]); mT = hT1
        scr = mg
        class _V:
            def __init__(self, tn, lo):
                self.t = tn.t[:, lo:lo + 256]; self.b = tn.b
        sc_ = _V(sg, 0); ch = _V(sg, 256); mc = _V(sg, 512)
        m8 = S.sb("r_m8", [128, 8]); gs = S.sb("r_gs", [128, 8]); gm = S.sb("r_gm", [128, 8]); pen = S.sb("r_pen", [128, 8])
        rsum = S.sb("r_sum", [128, 2])
        gi = 0
        for tt in range(NT):
            x_ = xt[0]
            b.dma(x_.t[:], xs.t[tt * 128:(tt + 1) * 128, :], R=[xs], W=[x_])
            if gath is None:
                b.dma(oat.t[:], oa_in.t[tt * 128:(tt + 1) * 128, :], R=[oa_in], W=[oat])
                b.dma(obt.t[:], ob_in.t[tt * 128:(tt + 1) * 128, :], R=[ob_in], W=[obt])
                srcs = ((oat, lambda cc: oat.t[:, cc * 128:(cc + 1) * 128]), (obt, lambda cc: obt.t[:, cc * 128:(cc + 1) * 128]))
            else:
                cf = lambda t_: t_.t[:].rearrange("p a b -> p (a b)")
                for hp in range(4):
                    cd = cand[hp % 2]
                    gtok = hp * T + tt * 128
                    gsrc = gaths[gtok // CH]
                    lr = gtok % CH
                    b.dma(cd.t[:], gsrc.t.rearrange("(i s) c -> s i c", i=4)[lr:lr + 128, :, :], R=[gsrc], W=[cd])
                    if hp == 0:
                        b.ts(cf(cmb), cf(cd), sel.t[:, 0:1], None, ALU.mult, None, R=[cd, sel], W=[cmb])
                    else:
                        b.stt(cf(cmb), cf(cd), sel.t[:, hp:hp + 1], cf(cmb), ALU.mult, ALU.add, R=[cd, sel, cmb], W=[cmb])
                srcs = ((cmb, lambda cc: cmb.t[:, cc, 0:128]), (cmb, lambda cc: cmb.t[:, cc, 128:256]))
            norm_tile2(b, c, x_, xn, ssx, scr, ab, 0, [hT1.t], [hT1], 0)
            for cbk in range(4):
                w = wgb[gi % 2]; gi += 1
                b.dma(w.t[:], wg_d.t[:, cbk * 512:(cbk + 1) * 512].rearrange("(k p) c -> p k c", p=128), R=[wg_d], W=[w])
                bk = b.bank()
                for k in range(8):
                    b.mm(bk, bk.t[:, 0:512], hT1.t[:, k, :], w.t[:, k, :], R=[hT1, w], start=(k == 0), stop=(k == 7))
                b.act(sg.t[:, cbk * 512:(cbk + 1) * 512], bk.t[:, 0:512], AF.Sigmoid, R=[bk], W=[sg])
            for ((src, view), dst) in zip(srcs, (oaT, obT)):
                bk = b.bank()
                for cc in range(4):
                    b.tp(bk, bk.t[:, cc * 128:(cc + 1) * 128], view(cc), ident, R=[src, ct])
                b.cp(dst.t[:].rearrange("p a b -> p (a b)"), bk.t[:, 0:512], R=[bk], W=[dst])
            wogT = wgb[gi % 2]; gi += 1
            b.dma(wogT.t[:].rearrange("p k c -> p (k c)").rearrange("p (a f) -> p a f", a=4), wog_d.t[:, :].rearrange("(k p) c -> p k c", p=128), R=[wog_d], W=[wogT])
            wodT = wgb[gi % 2]; gi += 1
            b.dma(wodT.t[:].rearrange("p k c -> p (k c)").rearrange("p (a f) -> p a f", a=4), wod_d.t[:, :].rearrange("(k p) c -> p k c", p=128), R=[wod_d], W=[wodT])
            wog_v = wogT.t[:].rearrange("p k c -> p (k c)").rearrange("p (a f) -> p a f", a=4)
            wod_v = wodT.t[:].rearrange("p k c -> p (k c)").rearrange("p (a f) -> p a f", a=4)
            for hf in range(2):
                bk = b.bank()
                for cc in range(4):
                    b.mm(bk, bk.t[:, 0:512], oaT.t[:, cc, :], wog_v[:, cc, hf * 512:(hf + 1) * 512], R=[oaT, wogT], start=(cc == 0), stop=(cc == 3))
                b.tt(mg.t[:, hf * 512:(hf + 1) * 512], bk.t[:, 0:512], sg.t[:, hf * 512:(hf + 1) * 512], ALU.mult, R=[bk, sg], W=[mg])
                bk = b.bank()
                for cc in range(4):
                    b.mm(bk, bk.t[:, 0:512], obT.t[:, cc, :], wod_v[:, cc, hf * 512:(hf + 1) * 512], R=[obT, wodT], start=(cc == 0), stop=(cc == 3))
                b.tt(tmp.t[:], bk.t[:, 0:512], sg.t[:, 1024 + hf * 512:1024 + (hf + 1) * 512], ALU.mult, R=[bk, sg], W=[tmp])
                b.tt(mg.t[:, hf * 512:(hf + 1) * 512], mg.t[:, hf * 512:(hf + 1) * 512], tmp.t[:], ALU.add, R=[mg, tmp], W=[mg])
            for k2 in range(2):
                bk = b.bank()
                for kk in range(4):
                    k = k2 * 4 + kk
                    b.tp(bk, bk.t[:, kk * 128:(kk + 1) * 128], mg.t[:, k * 128:(k + 1) * 128], ident, R=[mg, ct])
                b.cp(mT.t[:, k2 * 4:(k2 + 1) * 4, :].rearrange("p a b -> p (a b)"), bk.t[:, 0:512], R=[bk], W=[mT])
            for hf in range(2):
                wo_ = wgb[gi % 2]; gi += 1
                b.dma(wo_.t[:], wout_d.t[:, hf * 512:(hf + 1) * 512].rearrange("(k p) c -> p k c", p=128), R=[wout_d], W=[wo_])
                bk = b.bank()
                for k in range(8):
                    b.mm(bk, bk.t[:, 0:512], mT.t[:, k, :], wo_.t[:, k, :], R=[mT, wo_], start=(k == 0), stop=(k == 7))
                b.tt(tmp.t[:], bk.t[:, 0:512], g1bc.t[:, hf * 512:(hf + 1) * 512], ALU.mult, R=[bk, g1bc], W=[tmp])
                b.tt(acc.t[:, tt, hf * 512:(hf + 1) * 512], tmp.t[:], x_.t[:, hf * 512:(hf + 1) * 512], ALU.add, R=[tmp, x_], W=[acc.sub(tt)])
            norm_tile2(b, c, acc.sub(tt), xn, ssx, scr, ab, 16, [h2f.t, h2b.t], [h2f, h2b.sub(tt // TPG)], 0, xap=acc.t[:, tt, :], cols=[0, tt * 128])
            bk = b.bank()
            for k in range(8):
                b.mm(bk, bk.t[:, 0:256], h2f.t[:, k, :], wr.t[:, k, :], R=[h2f, wr], start=(k == 0), stop=(k == 7))
            b.act(sc_.t[:], bk.t[:, 0:256], AF.Sigmoid, R=[bk], W=[sc_])
            b.tt(ch.t[:], sc_.t[:], rb.t[:], ALU.add, R=[sc_, rb], W=[ch])
            for g in range(8):
                b.S.op("dve", lambda e, g=g: e.max(out=m8.t[:], in_=ch.t[:, g * 32:(g + 1) * 32]), R=[ch], W=[m8])
                b.tt(gs.t[:, g:g + 1], m8.t[:, 0:1], m8.t[:, 1:2], ALU.add, R=[m8], W=[gs])
            b.S.op("dve", lambda e: e.max(out=m8.t[:], in_=gs.t[:]), R=[gs], W=[m8])
            b.ts(gm.t[:], gs.t[:], m8.t[:, 3:4], None, ALU.is_ge, None, R=[gs, m8], W=[gm])
            b.ts(pen.t[:], gm.t[:], -1.0, 1e9, ALU.add, ALU.mult, R=[gm], W=[pen])
            for g in range(8):
                b.ts(mc.t[:, g * 32:(g + 1) * 32], ch.t[:, g * 32:(g + 1) * 32], gm.t[:, g:g + 1], pen.t[:, g:g + 1], ALU.mult, ALU.add,
                     R=[ch, gm, pen], W=[mc])
            b.S.op("dve", lambda e: e.max(out=m8.t[:], in_=mc.t[:]), R=[mc], W=[m8])
            b.ts(mc.t[:], mc.t[:], m8.t[:, 7:8], None, ALU.is_ge, None, R=[mc, m8], W=[mc])
            b.tt(mc.t[:], mc.t[:], sc_.t[:], ALU.mult, R=[mc, sc_], W=[mc])
            b.S.op("dve", lambda e: e.tensor_reduce(out=rsum.t[:, 0:1], in_=mc.t[:], axis=AX.X, op=ALU.add), R=[mc], W=[rsum])
            b.S.op("dve", lambda e: e.reciprocal(out=rsum.t[:, 1:2], in_=rsum.t[:, 0:1]), R=[rsum], W=[rsum])
            b.ts(wt.t[:, tt, 0:256], mc.t[:], rsum.t[:, 1:2], 2.5, ALU.mult, ALU.mult, R=[mc, rsum], W=[wt])
    with S.scope():
        wgu_st = [S.sb(f"wgu_st{i}", [128, 8, 512]) for i in range(2)]
        wd_st = [S.sb(f"wd_st{i}", [128, 2, 1024]) for i in range(2)]
        wgu_bf = [S.sb(f"wgu_bf{i}", [128, 8, 512], BF16) for i in range(2)]
        wd_bf = [S.sb(f"wd_bf{i}", [128, 2, 1024], BF16) for i in range(2)]
        AT = [S.sb(f"AT{i}", [128, 2, MG], BF16) for i in range(2)]
        slu = [S.sb(f"slu{i}", [128, MG]) for i in range(2)]
        for e in range(n_exp):
            p = e % 2
            b.dma(wgu_st[p].t[:], wgu_d.t[e].rearrange("(k p) c -> p k c", p=128), R=[wgu_d], W=[wgu_st[p]])
            b.dma(wd_st[p].t[:], wdn_d.t[e].rearrange("(k p) c -> p k c", p=128), R=[wdn_d], W=[wd_st[p]])
            b.cp(wgu_bf[p].t[:, 0:4, :], wgu_st[p].t[:, 0:4, :], R=[wgu_st[p]], W=[wgu_bf[p]], eng="pool")
            b.cp(wgu_bf[p].t[:, 4:8, :], wgu_st[p].t[:, 4:8, :], R=[wgu_st[p]], W=[wgu_bf[p]], eng="pool")
            for c2 in range(2):
                b.tt(wd_bf[p].t[:, c2, :], wd_st[p].t[:, c2, :], g2bc.t[:], ALU.mult, R=[wd_st[p], g2bc], W=[wd_bf[p]], eng="pool")
            def gu_stage(tg, c2):
                bg, bu = b.banks[2 * c2], b.banks[2 * c2 + 1]
                for (bk, fc) in ((bg, c2), (bu, 2 + c2)):
                    for k in range(8):
                        b.mm(bk, bk.t[:, 0:MG], wgu_bf[p].t[:, k, fc * 128:(fc + 1) * 128], h2b.t[:, k, tg * MG:(tg + 1) * MG],
                             R=[wgu_bf[p], h2b.sub(tg)], start=(k == 0), stop=(k == 7))
                A = AT[tg % 2]
                sl = slu[c2]
                b.act(sl.t[:], bg.t[:, 0:MG], AF.Silu, R=[bg], W=[sl])
                b.tt(A.t[:, c2, :], sl.t[:], bu.t[:, 0:MG], ALU.mult, R=[sl, bu], W=[A])

            def down_stage(tg):
                A = AT[tg % 2]
                for ti in range(TPG):
                    tile_ = tg * TPG + ti
                    for hf in range(2):
                        bk = b.bank()
                        for c2 in range(2):
                            b.mm(bk, bk.t[:, 0:512], A.t[:, c2, ti * 128:(ti + 1) * 128], wd_bf[p].t[:, c2, hf * 512:(hf + 1) * 512],
                                 R=[A, wd_bf[p]], start=(c2 == 0), stop=(c2 == 1))
                        b.stt(acc.t[:, tile_, hf * 512:(hf + 1) * 512], bk.t[:, 0:512], wt.t[:, tile_, e:e + 1],
                              acc.t[:, tile_, hf * 512:(hf + 1) * 512], ALU.mult, ALU.add, R=[bk, wt, acc.sub(tile_)], W=[acc.sub(tile_)])

            b.bank_pool = [4, 5, 6, 7]
            for tg in range(NTG):
                gu_stage(tg, 0)
                gu_stage(tg, 1)
                if tg >= 1:
                    down_stage(tg - 1)
            down_stage(NTG - 1)
            b.bank_pool = list(range(8))
        for tt in range(NT):
            b.dma(outd.t[tt * 128:(tt + 1) * 128, :], acc.t[:, tt, :], R=[acc.sub(tt)], W=[outd.sub(tt)], final=True)


def prep_d(inp, bidx, j, T, oa_b, ob_b):
    m = prep_common(inp, bidx)
    m["consts"] = host_consts(alibi_slope(0))
    w_in = inp["w_in"][0]
    m["xs"] = np.ascontiguousarray(inp["x"][bidx, j * T:(j + 1) * T])
    m["oa_in"] = np.ascontiguousarray(oa_b[j * T:(j + 1) * T])
    m["ob_in"] = np.ascontiguousarray(ob_b[j * T:(j + 1) * T])
    m["w_gates"] = np.ascontiguousarray(w_in[:, W_OFF["ga"]:W_OFF["ga"] + 2048])
    m["w_o_gdn"] = np.ascontiguousarray(inp["w_o_gdn"][0])
    m["w_o_diff"] = np.ascontiguousarray(inp["w_o_diff"][0])
    m["w_out"] = np.ascontiguousarray(inp["w_out"][0])
    m["w_router"] = np.ascontiguousarray(inp["w_router"][0])
    m["rbias"] = np.ascontiguousarray(np.tile(inp["router_bias"][0][None, :], (128, 1)))
    return m


_EXP_CACHE = {}


def expert_stack(inp):
    key = id(inp["w_exp_gate_up"])
    if key not in _EXP_CACHE:
        _EXP_CACHE.clear()
        wgu = np.concatenate([inp["w_exp_gate_up"][0], inp["w_shared_gate_up"][0][None]], axis=0)
        wdn = np.concatenate([inp["w_exp_down"][0], inp["w_shared_down"][0][None]], axis=0)
        _EXP_CACHE[key] = (np.ascontiguousarray(wgu), np.ascontiguousarray(wdn))
    return _EXP_CACHE[key]


def prep_fused(inp, cid, T):
    bi, j = cid // 4, cid % 4
    m = prep_ab(inp, bi, j)
    w_in = inp["w_in"][0]
    m["xs"] = np.ascontiguousarray(inp["x"][bi, j * T:(j + 1) * T])
    m["w_gates"] = np.ascontiguousarray(w_in[:, W_OFF["ga"]:W_OFF["ga"] + 2048])
    m["w_o_gdn"] = np.ascontiguousarray(inp["w_o_gdn"][0])
    m["w_o_diff"] = np.ascontiguousarray(inp["w_o_diff"][0])
    m["w_out"] = np.ascontiguousarray(inp["w_out"][0])
    m["w_router"] = np.ascontiguousarray(inp["w_router"][0])
    m["rbias"] = np.ascontiguousarray(np.tile(inp["router_bias"][0][None, :], (128, 1)))
    sel = np.zeros((128, 4), np.float32)
    sel[:, j] = 1.0
    m["sel"] = sel
    return m


def kernel(**inp):
    inp = {k: np.asarray(v) for k, v in inp.items()}
    B, Sq, _ = inp["x"].shape
    T = Sq // 4
    nc = build_fused(Sq)
    wgu, wdn = expert_stack(inp)
    maps = []
    for cid in range(8):
        m = prep_fused(inp, cid, T)
        m["w_gu"] = wgu
        m["w_dn"] = wdn
        maps.append(m)
    res = run_bass_kernel_spmd(nc, maps, core_ids=list(range(8)))
    out = np.zeros((B, Sq, 1024), np.float32)
    for cid in range(8):
        bi, j = cid // 4, cid % 4
        out[bi, j * T:(j + 1) * T] = res.results[cid]["out"]
    return out
```

```python
import contextlib
import numpy as np
import concourse.bass as bass
import concourse.mybir as mybir
from concourse.bass_utils import run_bass_kernel_spmd

F32 = mybir.dt.float32
BF16 = mybir.dt.bfloat16
I32 = mybir.dt.int32
AF = mybir.ActivationFunctionType
ALU = mybir.AluOpType


class Buf:
    __slots__ = ("name", "w", "r")

    def __init__(self, name):
        self.name = name
        self.w = None
        self.r = []


class Tn:
    def __init__(self, t, name):
        self.t = t
        self.name = name
        self.b = Buf(name)
        self.subs = {}

    def sub(self, key):
        if key not in self.subs:
            self.subs[key] = Buf(f"{self.name}:{key}")
        return self.subs[key]


class Op:
    __slots__ = ("eng", "fn", "deps", "is_dma", "sem", "val", "signals", "final", "inc")

    def __init__(self, eng, fn, is_dma=False):
        self.eng = eng
        self.fn = fn
        self.deps = []
        self.is_dma = is_dma
        self.sem = None
        self.val = None
        self.signals = False
        self.final = False
        self.inc = 1


def _buf(x):
    return x.b if hasattr(x, 'b') else x


class Sched:
    ENGS = ("pe", "act", "dve", "pool", "sp")

    def __init__(self, nc, n_dma_sems=None):
        self.nc = nc
        self.stack = contextlib.ExitStack()
        self.stacks = [self.stack]
        self.ops = {e: [] for e in self.ENGS}
        self.esem = {e: nc.alloc_semaphore(name=f"sem_{e}") for e in self.ENGS}
        nd = n_dma_sems or {"sp": 24, "pool": 12, "act": 4}
        self.dsem = {q: [nc.alloc_semaphore(name=f"dsem_{q}{i}") for i in range(n)] for q, n in nd.items()}
        self.dlast = {q: [None] * n for q, n in nd.items()}
        self.dcnt = {q: [0] * n for q, n in nd.items()}
        self.dnext = {q: 0 for q in nd}
        self.n_ops = 0

    def sb(self, name, shape, dtype=F32):
        t = self.stacks[-1].enter_context(self.nc.sbuf_tensor(name, list(shape), dtype))
        return Tn(t, name)

    @contextlib.contextmanager
    def scope(self):
        st = contextlib.ExitStack()
        self.stacks.append(st)
        try:
            yield
        finally:
            self.barrier()
            self.stacks.pop()
            st.close()

    def barrier(self):
        lasts = []
        for e in self.ENGS:
            for o in reversed(self.ops[e]):
                if o.fn is not None and not o.is_dma:
                    lasts.append(o)
                    break
        for q in self.dlast:
            lasts += [o for o in self.dlast[q] if o is not None]
        for e in self.ENGS:
            o = Op(e, None)
            o.sem = self.esem[e]
            o.deps = list(lasts)
            self.ops[e].append(o)

    def ps(self, name, shape, dtype=F32):
        t = self.stack.enter_context(self.nc.psum_tensor(name, list(shape), dtype))
        return Tn(t, name)

    def dram(self, ap, name):
        return Tn(ap, name)

    def scratch(self, name, shape, dtype=F32):
        t = self.nc.dram_tensor(name, list(shape), dtype, kind="Internal")
        return Tn(t.ap(), name)

    def _track(self, op, R, W):
        eng = op.eng
        deps = []

        def add(d, kind):
            if d is None:
                return
            if not d.is_dma and not op.is_dma and d.eng == eng:
                if eng == "pe":
                    return
            deps.append(d)

        Rb = [_buf(x) for x in R]
        Wb = [_buf(x) for x in W]
        for b in Rb:
            add(b.w, "raw")
        for b in Wb:
            add(b.w, "waw")
            for r in b.r:
                add(r, "war")
        for b in Rb:
            b.r.append(op)
        for b in Wb:
            b.w = op
            b.r = []
        seen = set()
        for d in deps:
            if id(d) not in seen and d is not op:
                seen.add(id(d))
                op.deps.append(d)

    def op(self, eng, fn, R=(), W=()):
        o = Op(eng, fn)
        o.sem = self.esem[eng]
        self._track(o, R, W)
        self.ops[eng].append(o)
        self.n_ops += 1
        return o

    def dma(self, q, out, in_, R=(), W=(), final=False, fn=None, **kw):
        if fn is None:
            fn = lambda e: e.dma_start(out=out, in_=in_, **kw)
        o = Op(q, fn, is_dma=True)
        o.inc = 16
        k = self.dnext[q]
        self.dnext[q] = (k + 1) % len(self.dsem[q])
        o.sem = self.dsem[q][k]
        self.dcnt[q][k] += 16
        o.val = self.dcnt[q][k]
        o.signals = True
        o.final = final
        self._track(o, R, W)
        prev = self.dlast[q][k]
        if prev is not None and prev not in o.deps:
            o.deps.append(prev)
        self.dlast[q][k] = o
        self.ops[q].append(o)
        self.n_ops += 1
        return o

    def cc(self, fn, R=(), W=()):
        if "cc" not in self.dsem:
            self.dsem["cc"] = [self.nc.alloc_semaphore(name="dsem_cc")]
            self.dlast["cc"] = [None]
            self.dcnt["cc"] = [0]
        o = Op("pool", fn, is_dma=True)
        o.inc = 1
        o.sem = self.dsem["cc"][0]
        self.dcnt["cc"][0] += 1
        o.val = self.dcnt["cc"][0]
        o.signals = True
        self._track(o, R, W)
        prev = self.dlast["cc"][0]
        if prev is not None and prev not in o.deps:
            o.deps.append(prev)
        self.dlast["cc"][0] = o
        self.ops["pool"].append(o)
        return o

    def emit(self):
        for e in self.ENGS:
            for o in self.ops[e]:
                for d in o.deps:
                    d.signals = True
        for e in self.ENGS:
            c = 0
            for o in self.ops[e]:
                if o.fn is None:
                    o.signals = False
                    o.val = c
                    continue
                if not o.is_dma and o.signals:
                    c += 1
                    o.val = c
        nc = self.nc

        def replay(ename):
            def run(e):
                waited = {}
                finals = []
                for o in self.ops[ename]:
                    need = {}
                    for d in o.deps:
                        k = id(d.sem)
                        if k not in need or need[k][1] < d.val:
                            need[k] = (d.sem, d.val)
                    for k, (sem, val) in need.items():
                        if waited.get(k, 0) >= val:
                            continue
                        e.wait_ge(sem, val)
                        waited[k] = val
                    if o.fn is None:
                        continue
                    ins = o.fn(e)
                    if o.signals:
                        ins.then_inc(o.sem, o.inc)
                    if o.final:
                        finals.append(o)
                for o in finals:
                    e.wait_ge(o.sem, o.val)
            return run

        with nc.Block() as block:
            block.sync(replay("sp"))
            block.tensor(replay("pe"))
            block.scalar(replay("act"))
            block.vector(replay("dve"))
            block.gpsimd(replay("pool"))
        self.stack.close()


AX = mybir.AxisListType
D = 1024
KC = 8
EPS = 1e-6
LAM_INIT = 0.2
W_OFF = dict(qA=0, kA=512, vA=1024, zA=1536, beta=2048, decay=2052, qB=2056, kB=2568, vB=3080, ga=3592, gb=4616)


class Bld:
    def __init__(self, nc):
        self.nc = nc
        self.S = Sched(nc)
        self.banks = [self.S.ps(f"bank{i}", [128, 512]) for i in range(8)]
        self.bi = 0
        self.bank_pool = list(range(8))
        self.ins = {}

    def inp(self, name, shape, dtype=F32):
        ap = self.nc.dram_tensor(name, list(shape), dtype, kind="ExternalInput").ap()
        t = self.S.dram(ap, name)
        self.ins[name] = t
        return t

    def outp(self, name, shape, dtype=F32):
        ap = self.nc.dram_tensor(name, list(shape), dtype, kind="ExternalOutput").ap()
        return self.S.dram(ap, name)

    def bank(self):
        b = self.banks[self.bank_pool[self.bi % len(self.bank_pool)]]
        self.bi += 1
        return b

    def mm(self, bank, out, lhsT, rhs, R, start=True, stop=True):
        self.S.op("pe", lambda e: e.matmul(out, lhsT, rhs, start=start, stop=stop), R=R, W=[bank])

    def tp(self, bank, out, in_, ident, R):
        self.S.op("pe", lambda e: e.transpose(out, in_, ident), R=R, W=[bank])

    def act(self, out, in_, func, R, W, **kw):
        self.S.op("act", lambda e: e.activation(out=out, in_=in_, func=func, **kw), R=R, W=W)

    def ts(self, out, in0, s1, s2, op0, op1, R, W, eng="dve"):
        if op1 is None:
            self.S.op(eng, lambda e: e.tensor_scalar(out=out, in0=in0, scalar1=s1, scalar2=None, op0=op0), R=R, W=W)
        else:
            self.S.op(eng, lambda e: e.tensor_scalar(out=out, in0=in0, scalar1=s1, scalar2=s2, op0=op0, op1=op1), R=R, W=W)

    def tt(self, out, in0, in1, op, R, W, eng="dve"):
        self.S.op(eng, lambda e: e.tensor_tensor(out=out, in0=in0, in1=in1, op=op), R=R, W=W)

    def stt(self, out, in0, scalar, in1, op0, op1, R, W, eng="dve"):
        self.S.op(eng, lambda e: e.scalar_tensor_tensor(out=out, in0=in0, scalar=scalar, in1=in1, op0=op0, op1=op1), R=R, W=W)

    def cp(self, out, in_, R, W, eng="dve"):
        self.S.op(eng, lambda e: e.tensor_copy(out=out, in_=in_), R=R, W=W)

    def memset(self, ap, val, W, eng="dve"):
        self.S.op(eng, lambda e: e.memset(ap, val), R=[], W=W)

    def dma(self, out, in_, R, W, q="sp", final=False):
        self.S.dma(q, out, in_, R=R, W=W, final=final)

    def rstd(self, out, in_, R, W, scale, eps=EPS, lnbias=0.0):
        self.act(out, in_, AF.Ln, R=R, W=W, scale=scale, bias=eps)
        self.act(out, out, AF.Exp, R=W, W=W, scale=-0.5, bias=lnbias)


def build_consts(b, slope=None):
    S = b.S
    c = {}
    cin = b.inp("consts", [128, 128 * 6])
    ct = S.sb("consts_sb", [128, 128 * 6])
    b.dma(ct.t[:], cin.t[:, :], R=[cin], W=[ct])
    c["t"] = ct
    c["ident"] = ct.t[:, 0:128]
    c["ones"] = ct.t[:, 128:256]
    c["bd"] = ct.t[:, 256:384]
    c["UT"] = ct.t[0:64, 384:448]
    c["LTs"] = ct.t[0:64, 448:512]
    c["Cm"] = ct.t[:, 512:640]
    c["brel"] = ct.t[:, 640:704]
    return c


def host_consts(slope):
    c = np.zeros((128, 768), np.float32)
    c[:, 0:128] = np.eye(128)
    c[:, 128:256] = 1.0
    c[0:64, 256:320] = 1.0
    c[64:128, 320:384] = 1.0
    p = np.arange(64)[:, None]
    f = np.arange(64)[None, :]
    c[0:64, 384:448] = (p <= f)
    c[0:64, 448:512] = (p > f)
    k = np.arange(128)[:, None]
    q = np.arange(128)[None, :]
    cm = np.where(k <= q, 1.0, np.where((k // 64) == (q // 64), np.exp(-2.0 * slope * (k - q)), 0.0))
    c[:, 512:640] = cm
    r = np.arange(64)[None, :]
    c[:, 640:704] = slope * (k + 128.0 * (r - 63) - 127.0)
    return c


def emit_adaln(b, c, want):
    S = b.S
    ccol = b.inp("c_col", [128, 8])
    wada = b.inp("w_ada", [1024, 6144])
    bada = b.inp("b_ada", [1, 6144])
    g1c = b.inp("g1c", [128, 8])
    g2c = b.inp("g2c", [128, 8])
    modc = S.sb("modc", [128, 48])
    ab = S.sb("ab", [128, 32])
    out = {"modc": modc, "ab": ab}
    if want == "gates":
        out["g1bc"] = S.sb("g1bc", [128, 1024])
        out["g2bc"] = S.sb("g2bc", [128, 1024])
    with S.scope():
        _emit_adaln_body(b, c, want, out, ccol, wada, bada, g1c, g2c)
    return out


def _emit_adaln_body(b, c, want, out, ccol, wada, bada, g1c, g2c):
    S = b.S
    modc = out["modc"]
    ab = out["ab"]
    sc = S.sb("sc", [128, 8])
    gt = S.sb("gcols", [128, 16])
    brow = S.sb("brow", [1, 6144])
    modrow = S.sb("modrow", [1, 6144])
    wb = [S.sb(f"wadab{i}", [128, 8, 512]) for i in range(2)]
    b.dma(sc.t[:], ccol.t[:, :], R=[ccol], W=[sc])
    b.dma(gt.t[:, 0:8], g1c.t[:, :], R=[g1c], W=[gt])
    b.dma(gt.t[:, 8:16], g2c.t[:, :], R=[g2c], W=[gt])
    b.dma(brow.t[:], bada.t[:, :], R=[bada], W=[brow])
    b.act(sc.t[:], sc.t[:], AF.Silu, R=[sc], W=[sc])
    for jb in range(12):
        w = wb[jb % 2]
        b.dma(w.t[:], wada.t[:, jb * 512:(jb + 1) * 512].rearrange("(k p) c -> p k c", p=128), R=[wada], W=[w])
        bk = b.bank()
        for k in range(8):
            b.mm(bk, bk.t[0:1, 0:512], sc.t[:, k:k + 1], w.t[:, k, :], R=[sc, w], start=(k == 0), stop=(k == 7))
        b.tt(modrow.t[0:1, jb * 512:(jb + 1) * 512], bk.t[0:1, 0:512], brow.t[0:1, jb * 512:(jb + 1) * 512], ALU.add,
             R=[bk, brow], W=[modrow])
    bk = b.bank()
    for j in range(48):
        b.mm(bk, bk.t[:, j:j + 1], modrow.t[0:1, j * 128:(j + 1) * 128], c["ones"][0:1, 0:1], R=[modrow, c["t"]])
    b.cp(modc.t[:], bk.t[:, 0:48], R=[bk], W=[modc])
    b.ts(ab.t[:, 0:8], modc.t[:, 8:16], 1.0, None, ALU.add, None, R=[modc], W=[ab])
    b.tt(ab.t[:, 0:8], ab.t[:, 0:8], gt.t[:, 0:8], ALU.mult, R=[ab, gt], W=[ab])
    b.cp(ab.t[:, 8:16], modc.t[:, 0:8], R=[modc], W=[ab])
    b.ts(ab.t[:, 16:24], modc.t[:, 32:40], 1.0, None, ALU.add, None, R=[modc], W=[ab])
    b.tt(ab.t[:, 16:24], ab.t[:, 16:24], gt.t[:, 8:16], ALU.mult, R=[ab, gt], W=[ab])
    b.cp(ab.t[:, 24:32], modc.t[:, 24:32], R=[modc], W=[ab])
    if want == "gates":
        g1bc = out["g1bc"]
        g2bc = out["g2bc"]
        for (dst, off) in ((g1bc, 2048), (g2bc, 5120)):
            for hf in range(2):
                bk = b.bank()
                b.mm(bk, bk.t[:, 0:512], c["ones"][0:1, 0:128], modrow.t[0:1, off + hf * 512: off + (hf + 1) * 512],
                     R=[modrow, c["t"]])
                b.cp(dst.t[:, hf * 512:(hf + 1) * 512], bk.t[:, 0:512], R=[bk], W=[dst])
    return out


def emit_norm_hT(b, c, xsrc, row0, ntiles, acol, bcol, hT, xt_bufs, ssq, hT_dtype_ap=None, x_keep=None):
    S = b.S
    for tt in range(ntiles):
        xt = xt_bufs[tt % len(xt_bufs)] if x_keep is None else x_keep[tt]
        b.dma(xt.t[:], xsrc.t[row0 + tt * 128: row0 + (tt + 1) * 128, :], R=[xsrc], W=[xt])
    return


def norm_tile(b, c, xt, xn, ss, acol, bcol, hT, col0, scratch):
    b.act(scratch.t[:], xt.t[:], AF.Square, R=[xt], W=[scratch, ss], accum_out=ss.t[:, 0:1])
    b.rstd(ss.t[:, 0:1], ss.t[:, 0:1], R=[ss], W=[ss], scale=1.0 / D)
    b.ts(xn.t[:], xt.t[:], ss.t[:, 0:1], None, ALU.mult, None, R=[xt, ss], W=[xn])
    for k2 in range(2):
        bk = b.bank()
        for kk in range(4):
            k = k2 * 4 + kk
            b.tp(bk, bk.t[:, kk * 128:(kk + 1) * 128], xn.t[:, k * 128:(k + 1) * 128], c["ident"], R=[xn, c["t"]])
        for kk in range(4):
            k = k2 * 4 + kk
            b.act(hT[:, k, col0:col0 + 128], bk.t[:, kk * 128:(kk + 1) * 128], AF.Identity, R=[bk, acol[1]], W=[acol[2]],
                  scale=acol[0][:, k:k + 1], bias=bcol[0][:, k:k + 1])


def norm_tile2(b, c, xt, xn, ss, scratch, ab, aoff, hT, hTt, col0, xap=None, cols=None):
    if xap is None:
        xap = xt.t[:]
    b.act(scratch.t[:], xap, AF.Square, R=[xt], W=[scratch, ss], accum_out=ss.t[:, 0:1])
    b.rstd(ss.t[:, 0:1], ss.t[:, 0:1], R=[ss], W=[ss], scale=1.0 / D)
    b.ts(xn.t[:], xap, ss.t[:, 0:1], None, ALU.mult, None, R=[xt, ss], W=[xn])
    for k2 in range(2):
        bk = b.bank()
        for kk in range(4):
            k = k2 * 4 + kk
            b.tp(bk, bk.t[:, kk * 128:(kk + 1) * 128], xn.t[:, k * 128:(k + 1) * 128], c["ident"], R=[xn, c["t"]])
        for kk in range(4):
            k = k2 * 4 + kk
            for di, (dst, dstT) in enumerate(zip(hT, hTt)):
                cc0 = col0 if cols is None else cols[di]
                b.act(dst[:, k, cc0:cc0 + 128], bk.t[:, kk * 128:(kk + 1) * 128], AF.Identity, R=[bk, ab], W=[dstT],
                      scale=ab.t[:, aoff + k:aoff + k + 1], bias=ab.t[:, aoff + 8 + k:aoff + 9 + k])


def build_ab(Sq):
    nc = bass.Bass("TRN2", target_bir_lowering=False)
    b = Bld(nc)
    c = build_consts(b)
    ad = emit_adaln(b, c, want="none")
    send = b.outp("send", [Sq, 256])
    emit_ab(b, c, ad["ab"], Sq, lambda r0, n, c0, key: (send.t[r0:r0 + n, c0:c0 + 128], send.sub(key)))
    b.S.emit()
    return nc


def emit_ab(b, c, ab, Sq, send_at, after_group=None):
    with b.S.scope():
        _emit_ab_body(b, c, ab, Sq, send_at, after_group)


def _emit_ab_body(b, c, ab, Sq, send_at, after_group):
    S = b.S
    NG = Sq // 512
    xb = b.inp("xb", [Sq, 1024])
    wcm_d = b.inp("w_cm", [1024, 640])
    wzbd_d = b.inp("w_zbd", [1024, 130])
    wvb_d = b.inp("w_vb", [1024, 128])
    small_d = b.inp("small", [128, 384])
    lam_d = b.inp("lamrow", [1, 256])

    wcm = S.sb("wcm", [128, 8, 640])
    wzbd = S.sb("wzbd", [128, 8, 130])
    wvb = S.sb("wvb", [128, 8, 128])
    small = S.sb("small_sb", [128, 384])
    lamr = S.sb("lamr", [1, 256])
    b.dma(wcm.t[:], wcm_d.t[:, :].rearrange("(k p) c -> p k c", p=128), R=[wcm_d], W=[wcm])
    b.dma(wzbd.t[:], wzbd_d.t[:, :].rearrange("(k p) c -> p k c", p=128), R=[wzbd_d], W=[wzbd])
    b.dma(wvb.t[:], wvb_d.t[:, :].rearrange("(k p) c -> p k c", p=128), R=[wvb_d], W=[wvb])
    b.dma(small.t[:], small_d.t[:, :], R=[small_d], W=[small])
    b.dma(lamr.t[:], lam_d.t[:, :], R=[lam_d], W=[lamr])
    convw = lambda j, tap: small.t[:, j * 4 + tap: j * 4 + tap + 1]
    gng = small.t[0:64, 128:256]
    sub_g = S.sb("sub_g", [128, 128])
    b.ts(sub_g.t[:], small.t[:, 256:384], 1.0 - LAM_INIT, None, ALU.mult, None, R=[small], W=[sub_g])
    negA = S.sb("negA", [128, 1])
    b.act(negA.t[:], small.t[:, 12:13], AF.Exp, R=[small], W=[negA])
    b.ts(negA.t[:], negA.t[:], -1.0, None, ALU.mult, None, R=[negA], W=[negA])
    lt = S.sb("lam_t", [1, 136])
    b.tt(lt.t[0:1, 0:64], lamr.t[0:1, 0:64], lamr.t[0:1, 64:128], ALU.mult, R=[lamr], W=[lt])
    b.tt(lt.t[0:1, 64:128], lamr.t[0:1, 128:192], lamr.t[0:1, 192:256], ALU.mult, R=[lamr], W=[lt])
    b.S.op("dve", lambda e: e.tensor_reduce(out=lt.t[0:1, 128:129], in_=lt.t[0:1, 0:64], axis=AX.X, op=ALU.add), R=[lt], W=[lt])
    b.S.op("dve", lambda e: e.tensor_reduce(out=lt.t[0:1, 129:130], in_=lt.t[0:1, 64:128], axis=AX.X, op=ALU.add), R=[lt], W=[lt])
    b.act(lt.t[0:1, 128:130], lt.t[0:1, 128:130], AF.Exp, R=[lt], W=[lt])
    b.tt(lt.t[0:1, 130:131], lt.t[0:1, 129:130], lt.t[0:1, 128:129], ALU.subtract, R=[lt], W=[lt])
    b.ts(lt.t[0:1, 130:131], lt.t[0:1, 130:131], -LAM_INIT, None, ALU.add, None, R=[lt], W=[lt])
    neglam = S.sb("neglam", [128, 1])
    bk = b.bank()
    b.mm(bk, bk.t[:, 0:1], c["ones"][0:1, 0:128], lt.t[0:1, 130:131], R=[lt, c["t"]])
    b.cp(neglam.t[:], bk.t[:, 0:1], R=[bk], W=[neglam])

    qT = S.sb("qT_all", [128, Sq])
    kT = S.sb("kT_all", [128, Sq])
    Vx = S.sb("Vext", [128, Sq // 128, 130])
    b.memset(Vx.t[:, :, 128:130], 1.0, W=[Vx])
    hT = [S.sb(f"hT{i}", [128, 8, 512]) for i in range(1)]
    xt = [S.sb(f"xt{i}", [128, 1024]) for i in range(1)]
    xn = S.sb("xn", [128, 1024])
    scr = xn
    ssx = S.sb("ssx", [128, 1])
    pre = [S.sb(f"pre{j}", [128, 515]) for j in range(3)]
    for j in range(3):
        b.memset(pre[j].t[:, 0:3], 0.0, W=[pre[j]])
    cv = [S.sb(f"cv{j}", [128, 512]) for j in range(3)]
    sq = S.sb("sq", [128, 512])
    rs = S.sb("rs", [128, 512])
    qk_tmp = S.sb("qk_tmp", [128, 512])
    St = S.sb("state", [128, 128])
    b.memset(St.t[:], 0.0, W=[St])
    def cb(name, shape):
        return [S.sb(f"{name}{i}", shape) for i in range(2)]
    zb = cb("zb", [64, 132]); gcol = cb("gcol", [64, 8]); e1 = cb("e1", [64, 64]); dts = cb("dts", [64, 64])
    dcs = cb("dcs", [64, 64]); Mb = [cb("Ma", [64, 64]), cb("Mb", [64, 64])]; Nb = [cb("Na", [64, 64]), cb("Nb", [64, 64])]
    Tb = [cb("Ta", [64, 64]), cb("Tb", [64, 64])]; ktm = cb("ktm", [64, 128]); kbt = cb("kbt", [64, 128])
    vbt = cb("vbt", [64, 128]); kdec = cb("kdec", [64, 128]); Ub = cb("Ub", [64, 128]); WTb = cb("WTb", [128, 64])
    AqT = cb("AqT", [64, 64]); gl = cb("gl", [128, 2]); vnew = cb("vnew", [64, 128]); o1 = cb("o1", [64, 128])
    ob_ = cb("ob_", [64, 128]); szb = cb("szb", [64, 128]); tmp64 = cb("tmp64", [64, 64]); oast = cb("oast", [64, 128])
    ssg = cb("ssg", [64, 2])
    PT = [S.sb(f"PT{i}", [128, 512]) for i in range(2)]
    obacc = [S.sb(f"obacc{i}", [128, 128]) for i in range(4)]
    rz = S.sb("rz", [128, 8])
    obst = [S.sb(f"obst{i}", [128, 128]) for i in range(2)]
    sso = S.sb("sso", [128, 2])
    ident = c["ident"]; ct = c["t"]
    SCALE = 64 ** -0.5

    for g in range(NG):
        h = hT[0]
        for tt in range(4):
            x_ = xt[0]
            b.dma(x_.t[:], xb.t[g * 512 + tt * 128: g * 512 + (tt + 1) * 128, :], R=[xb], W=[x_])
            norm_tile2(b, c, x_, xn, ssx, scr, ab, 0, [h.t], [h], tt * 128)
        for j in range(5):
            bk = b.bank()
            for k in range(8):
                b.mm(bk, bk.t[:, 0:512], wcm.t[:, k, j * 128:(j + 1) * 128], h.t[:, k, :], R=[wcm, h], start=(k == 0), stop=(k == 7))
            if j < 3:
                b.act(pre[j].t[:, 3:515], bk.t[:, 0:512], AF.Copy, R=[bk], W=[pre[j]])
                y = cv[j]
                b.ts(y.t[:], pre[j].t[:, 0:512], convw(j, 0), None, ALU.mult, None, R=[pre[j], small], W=[y])
                for tap in range(1, 4):
                    b.stt(y.t[:], pre[j].t[:, tap:tap + 512], convw(j, tap), y.t[:], ALU.mult, ALU.add, R=[pre[j], small, y], W=[y])
                b.cp(pre[j].t[:, 0:3], pre[j].t[:, 512:515], R=[pre[j]], W=[pre[j]])
                b.act(y.t[:], y.t[:], AF.Silu, R=[y], W=[y])
                if j < 2:
                    b.tt(sq.t[:], y.t[:], y.t[:], ALU.mult, R=[y], W=[sq])
                    bk2 = b.bank()
                    b.mm(bk2, bk2.t[:, 0:512], c["ones"], sq.t[:], R=[sq, ct])
                    b.act(rs.t[:], bk2.t[:, 0:512], AF.Ln, R=[bk2], W=[rs], bias=EPS)
                    b.act(rs.t[:], rs.t[:], AF.Exp, R=[rs], W=[rs], scale=-0.5, bias=(float(np.log(128 ** -0.5)) if j == 0 else 0.0))
                    b.tt(y.t[:], y.t[:], rs.t[:], ALU.mult, R=[y, rs], W=[y])
            else:
                dst = qT if j == 3 else kT
                gcolm = small.t[:, 14:15] if j == 3 else small.t[:, 15:16]
                b.act(qk_tmp.t[:], bk.t[:, 0:512], AF.Copy, R=[bk], W=[qk_tmp])
                b.tt(sq.t[:], qk_tmp.t[:], qk_tmp.t[:], ALU.mult, R=[qk_tmp], W=[sq])
                bk2 = b.bank()
                b.mm(bk2, bk2.t[:, 0:512], c["bd"], sq.t[:], R=[sq, ct])
                b.act(rs.t[:], bk2.t[:, 0:512], AF.Ln, R=[bk2], W=[rs], scale=1.0 / 64, bias=EPS)
                b.act(rs.t[:], rs.t[:], AF.Exp, R=[rs], W=[rs], scale=-0.5)
                b.stt(dst.t[:, g * 512:(g + 1) * 512], qk_tmp.t[:], gcolm, rs.t[:], ALU.mult, ALU.mult, R=[qk_tmp, small, rs], W=[dst.sub(g)])
        for tt in range(4):
            bk = b.bank()
            for k in range(8):
                b.mm(bk, bk.t[:, 0:128], h.t[:, k, tt * 128:(tt + 1) * 128], wvb.t[:, k, :], R=[wvb, h], start=(k == 0), stop=(k == 7))
            b.cp(Vx.t[:, g * 4 + tt, 0:128], bk.t[:, 0:128], R=[bk], W=[Vx.sub(g)])
        qn, kn, vs = cv[0], cv[1], cv[2]
        def chunk_pre(ch):
            p = ch % 2
            t0 = ch * 64
            kTc = kn.t[:, t0:t0 + 64]; qTc = qn.t[:, t0:t0 + 64]
            bk = b.bank()
            for k in range(8):
                b.mm(bk, bk.t[0:64, 0:130], h.t[:, k, t0:t0 + 64], wzbd.t[:, k, :], R=[wzbd, h], start=(k == 0), stop=(k == 7))
            z = zb[p]
            b.cp(z.t[:, 0:130], bk.t[0:64, 0:130], R=[bk], W=[z])
            yield
            gc = gcol[p]
            b.act(gc.t[:, 0:1], z.t[:, 128:129], AF.Sigmoid, R=[z], W=[gc])
            yield
            b.act(gc.t[:, 1:2], z.t[:, 129:130], AF.Exp, R=[z, small], W=[gc], bias=small.t[0:64, 13:14])
            yield
            b.act(gc.t[:, 1:2], gc.t[:, 1:2], AF.Ln, R=[gc], W=[gc], bias=1.0)
            yield
            b.tt(gc.t[:, 1:2], gc.t[:, 1:2], negA.t[0:64, 0:1], ALU.mult, R=[gc, negA], W=[gc])
            yield
            bk = b.bank()
            b.mm(bk, bk.t[0:64, 0:1], c["UT"], gc.t[:, 1:2], R=[gc, ct])
            yield
            b.cp(gc.t[:, 2:3], bk.t[0:64, 0:1], R=[bk], W=[gc])
            yield
            t64 = tmp64[p]
            b.ts(t64.t[:], c["UT"], gc.t[:, 1:2], None, ALU.mult, None, R=[gc, ct], W=[t64])
            yield
            bk = b.bank()
            b.mm(bk, bk.t[0:64, 0:64], c["ones"][0:64, 0:64], t64.t[:], R=[t64, ct])
            yield
            E = e1[p]
            b.ts(E.t[:], bk.t[0:64, 0:64], gc.t[:, 2:3], None, ALU.subtract, None, R=[bk, gc], W=[E])
            yield
            Dt = dts[p]; Dc = dcs[p]
            b.ts(Dt.t[:], E.t[:], 0.0, None, ALU.min, None, R=[E], W=[Dt])
            yield
            b.act(Dt.t[:], Dt.t[:], AF.Exp, R=[Dt], W=[Dt])
            yield
            b.tt(Dt.t[:], Dt.t[:], c["UT"], ALU.mult, R=[Dt, ct], W=[Dt])
            yield
            b.ts(Dc.t[:], E.t[:], -1.0, 0.0, ALU.mult, ALU.min, R=[E], W=[Dc])
            yield
            b.act(Dc.t[:], Dc.t[:], AF.Exp, R=[Dc], W=[Dc])
            yield
            b.tt(Dc.t[:], Dc.t[:], c["LTs"], ALU.mult, R=[Dc, ct], W=[Dc])
            yield
            bk = b.bank()
            b.mm(bk, bk.t[0:64, 0:64], kTc, kTc, R=[kn])
            yield
            M = Mb[0][p]; N = Nb[0][p]; T = Tb[0][p]
            b.stt(M.t[:], bk.t[0:64, 0:64], gc.t[:, 0:1], Dc.t[:], ALU.mult, ALU.mult, R=[bk, gc, Dc], W=[M])
            yield
            bk = b.bank()
            b.tp(bk, bk.t[0:64, 0:64], M.t[:], ident[0:64, 0:64], R=[M, ct])
            yield
            b.cp(N.t[:], bk.t[0:64, 0:64], R=[bk], W=[N])
            yield
            b.tt(T.t[:], ident[0:64, 0:64], N.t[:], ALU.subtract, R=[N, ct], W=[T])
            yield
            for s in range(1, 6):
                M2 = Mb[s % 2][p]; N2 = Nb[s % 2][p]; T2 = Tb[s % 2][p]
                bk = b.bank()
                b.mm(bk, bk.t[0:64, 0:64], N.t[:], M.t[:], R=[N, M])
                yield
                b.act(M2.t[:], bk.t[0:64, 0:64], AF.Copy, R=[bk], W=[M2])
                yield
                if s < 5:
                    bk = b.bank()
                    b.mm(bk, bk.t[0:64, 0:64], M.t[:], N.t[:], R=[N, M])
                    yield
                    b.cp(N2.t[:], bk.t[0:64, 0:64], R=[bk], W=[N2])
                    yield
                bk = b.bank()
                b.mm(bk, bk.t[0:64, 0:64], M2.t[:], T.t[:], R=[M2, T])
                yield
                b.tt(T2.t[:], bk.t[0:64, 0:64], T.t[:], ALU.add, R=[bk, T], W=[T2])
                yield
                M, N, T = M2, N2, T2
            bk = b.bank()
            b.tp(bk, bk.t[0:64, 0:128], kTc, ident, R=[kn, ct])
            yield
            b.tp(bk, bk.t[0:64, 128:256], vs.t[:, t0:t0 + 64], ident, R=[vs, ct])
            yield
            b.act(gc.t[:, 3:4], gc.t[:, 2:3], AF.Exp, R=[gc], W=[gc])
            yield
            b.tt(gc.t[:, 4:5], gc.t[:, 3:4], gc.t[:, 0:1], ALU.mult, R=[gc], W=[gc])
            yield
            b.cp(ktm[p].t[:], bk.t[0:64, 0:128], R=[bk], W=[ktm[p]])
            yield
            b.ts(kbt[p].t[:], bk.t[0:64, 0:128], gc.t[:, 4:5], None, ALU.mult, None, R=[bk, gc], W=[kbt[p]])
            yield
            b.ts(vbt[p].t[:], bk.t[0:64, 128:256], gc.t[:, 0:1], None, ALU.mult, None, R=[bk, gc], W=[vbt[p]])
            yield
            bk = b.bank()
            b.mm(bk, bk.t[0:64, 0:128], T.t[:], vbt[p].t[:], R=[T, vbt[p]])
            yield
            b.cp(Ub[p].t[:], bk.t[0:64, 0:128], R=[bk], W=[Ub[p]])
            yield
            bk = b.bank()
            b.mm(bk, bk.t[:, 0:64], kbt[p].t[:], T.t[:], R=[T, kbt[p]])
            yield
            b.act(WTb[p].t[:], bk.t[:, 0:64], AF.Copy, R=[bk], W=[WTb[p]])
            yield
            bk = b.bank()
            b.mm(bk, bk.t[0:64, 0:64], kTc, qTc, R=[kn, qn])
            yield
            b.tt(AqT[p].t[:], bk.t[0:64, 0:64], Dt.t[:], ALU.mult, R=[bk, Dt], W=[AqT[p]])
            yield
            bk = b.bank()
            b.mm(bk, bk.t[:, 0:1], c["ones"][0:64, 0:128], gc.t[:, 1:2], R=[gc, ct])
            yield
            b.cp(gl[p].t[:, 0:1], bk.t[:, 0:1], R=[bk], W=[gl[p]])
            yield
            b.act(gl[p].t[:, 1:2], gl[p].t[:, 0:1], AF.Exp, R=[gl[p]], W=[gl[p]])
            yield
            b.act(gc.t[:, 5:6], gc.t[:, 2:3], AF.Exp, R=[gc, gl[p]], W=[gc], scale=-1.0, bias=gl[p].t[0:64, 0:1])
            yield
            b.ts(kdec[p].t[:], ktm[p].t[:], gc.t[:, 5:6], None, ALU.mult, None, R=[ktm[p], gc], W=[kdec[p]])
            yield
            yield

        def chunk_seq(ch):
            p = ch % 2
            t0 = ch * 64
            qTc = qn.t[:, t0:t0 + 64]
            z = zb[p]
            gc = gcol[p]
            bkv = b.bank()
            b.mm(bkv, bkv.t[0:64, 0:128], WTb[p].t[:], St.t[:], R=[WTb[p], St])
            b.tt(vnew[p].t[:], Ub[p].t[:], bkv.t[0:64, 0:128], ALU.subtract, R=[bkv, Ub[p]], W=[vnew[p]])
            bk1 = b.bank()
            b.mm(bk1, bk1.t[0:64, 0:128], qTc, St.t[:], R=[qn, St])
            b.act(o1[p].t[:], bk1.t[0:64, 0:128], AF.Copy, R=[bk1, gc], W=[o1[p]], scale=gc.t[:, 3:4])
            bk2 = b.bank()
            b.mm(bk2, bk2.t[0:64, 0:128], AqT[p].t[:], vnew[p].t[:], R=[AqT[p], vnew[p]])
            b.tt(ob_[p].t[:], bk2.t[0:64, 0:128], o1[p].t[:], ALU.add, R=[bk2, o1[p]], W=[ob_[p]])
            bks = b.bank()
            b.mm(bks, bks.t[:, 0:128], kdec[p].t[:], vnew[p].t[:], R=[kdec[p], vnew[p]])
            b.stt(St.t[:], St.t[:], gl[p].t[:, 1:2], bks.t[:, 0:128], ALU.mult, ALU.add, R=[St, gl[p], bks], W=[St])
            b.act(szb[p].t[:], ob_[p].t[:], AF.Square, R=[ob_[p]], W=[szb[p], ssg[p]], accum_out=ssg[p].t[:, 0:1])
            b.rstd(ssg[p].t[:, 0:1], ssg[p].t[:, 0:1], R=[ssg[p]], W=[ssg[p]], scale=1.0 / 128)
            b.stt(ob_[p].t[:], ob_[p].t[:], ssg[p].t[:, 0:1], gng, ALU.mult, ALU.mult, R=[ob_[p], ssg[p], small], W=[ob_[p]])
            b.act(szb[p].t[:], z.t[:, 0:128], AF.Silu, R=[z], W=[szb[p]])
            b.tt(oast[p].t[:], ob_[p].t[:], szb[p].t[:], ALU.mult, R=[ob_[p], szb[p]], W=[oast[p]])
            sap, sbuf_ = send_at(g * 512 + t0, 64, 0, ("a", g, ch))
            b.dma(sap, oast[p].t[:], R=[oast[p]], W=[sbuf_], final=True)
        def gdn_gen():
            for pr in range(4):
                gens = [chunk_pre(2 * pr), chunk_pre(2 * pr + 1)]
                while gens:
                    for gnr in list(gens):
                        try:
                            next(gnr)
                        except StopIteration:
                            gens.remove(gnr)
                    yield
                chunk_seq(2 * pr)
                yield
                chunk_seq(2 * pr + 1)
                yield

        def attn_gen():
            for comp in range(2):
                lo, hi = comp * 64, (comp + 1) * 64
                nkb = 4 * g + 4
                for kb in range(nkb):
                    rel = kb - 4 * g
                    qlo = max(rel, 0) * 128
                    ps = b.banks[0]
                    b.mm(ps, ps.t[:, qlo:512], kT.t[lo:hi, kb * 128:(kb + 1) * 128], qT.t[lo:hi, g * 512 + qlo:(g + 1) * 512],
                         R=[kT.sub(kb // 4), qT.sub(g)])
                    P = PT[kb % 2]
                    for qs in range(max(rel, 0), 4):
                        r_ = kb - (4 * g + qs) + 63
                        b.act(P.t[:, qs * 128:(qs + 1) * 128], ps.t[:, qs * 128:(qs + 1) * 128], AF.Exp, R=[ps, ct], W=[P], scale=SCALE,
                              bias=c["brel"][:, r_:r_ + 1])
                    if rel >= 0:
                        b.tt(P.t[:, rel * 128:(rel + 1) * 128], P.t[:, rel * 128:(rel + 1) * 128], c["Cm"], ALU.mult, R=[P, ct], W=[P])
                    yield
                    for qs in range(max(rel, 0), 4):
                        po = b.banks[4 + qs]
                        b.mm(po, po.t[:, 0:130], P.t[:, qs * 128:(qs + 1) * 128], Vx.t[:, kb, 0:130], R=[P, Vx.sub(kb // 4)],
                             start=(kb == 0), stop=(kb == 4 * g + qs))
                    yield
                for qs in range(4):
                    po = b.banks[4 + qs]
                    rcol = rz.t[:, comp * 4 + qs: comp * 4 + qs + 1]
                    b.S.op("dve", lambda e, rcol=rcol, po=po: e.reciprocal(out=rcol, in_=po.t[:, 128:129]), R=[po], W=[rz])
                    if comp == 0:
                        b.ts(obacc[qs].t[:], po.t[:, 0:128], rcol, None, ALU.mult, None, R=[po, rz], W=[obacc[qs]])
                    else:
                        b.tt(rcol, rcol, neglam.t[:, 0:1], ALU.mult, R=[rz, neglam], W=[rz])
                        b.stt(obacc[qs].t[:], po.t[:, 0:128], rcol, obacc[qs].t[:], ALU.mult, ALU.add, R=[po, rz, obacc[qs]], W=[obacc[qs]])
                        o_ = obst[qs % 2]
                        b.act(o_.t[:], obacc[qs].t[:], AF.Square, R=[obacc[qs]], W=[o_, sso], accum_out=sso.t[:, 0:1])
                        b.rstd(sso.t[:, 0:1], sso.t[:, 0:1], R=[sso], W=[sso], scale=1.0 / 128)
                        b.stt(o_.t[:], obacc[qs].t[:], sso.t[:, 0:1], sub_g.t[:], ALU.mult, ALU.mult, R=[obacc[qs], sso, sub_g], W=[o_])
                        sap, sbuf_ = send_at(g * 512 + qs * 128, 128, 128, ("b", g, qs))
                        b.dma(sap, o_.t[:], R=[o_], W=[sbuf_], final=True)
        b.bank_pool = [1, 2, 3]
        gG, gA = gdn_gen(), attn_gen()
        nA_est = 2 * (4 * g + 4) * 2 + 2
        nG_est = 4 * 75
        ratio = nG_est / nA_est
        accr = 0.0
        aliveG = aliveA = True
        while aliveG or aliveA:
            if aliveA:
                try:
                    next(gA)
                except StopIteration:
                    aliveA = False
            accr += ratio
            n_g = int(accr) if aliveA else 1
            accr -= int(accr)
            for _ in range(max(n_g, 0)):
                if not aliveG:
                    break
                try:
                    next(gG)
                except StopIteration:
                    aliveG = False
        b.bank_pool = list(range(8))
        if after_group is not None:
            after_group(g)


def _cols(v):
    return np.ascontiguousarray(np.asarray(v, np.float32).reshape(-1, 128).T)


def alibi_slope(h):
    return float(2.0 ** (-8.0 * (h + 1) / 4))


def prep_common(inp, bidx):
    return {
        "c_col": _cols(inp["c"][bidx]),
        "w_ada": np.ascontiguousarray(inp["w_ada"][0]),
        "b_ada": np.ascontiguousarray(inp["b_ada"][0].reshape(1, -1)),
        "g1c": _cols(inp["norm1_g"][0]),
        "g2c": _cols(inp["norm2_g"][0]),
    }


def prep_ab(inp, bidx, h):
    w_in = inp["w_in"][0]
    sl = lambda name, width=128: w_in[:, W_OFF[name] + h * width: W_OFF[name] + (h + 1) * width]
    m = prep_common(inp, bidx)
    m["consts"] = host_consts(alibi_slope(h))
    m["xb"] = np.ascontiguousarray(inp["x"][bidx])
    m["w_cm"] = np.ascontiguousarray(np.concatenate([sl("qA"), sl("kA"), sl("vA"), sl("qB"), sl("kB")], axis=1))
    m["w_zbd"] = np.ascontiguousarray(np.concatenate([sl("zA"), sl("beta", 1), sl("decay", 1)], axis=1))
    m["w_vb"] = np.ascontiguousarray(sl("vB"))
    small = np.zeros((128, 384), np.float32)
    cw = inp["conv_w"][0]
    for j in range(3):
        small[:, j * 4:(j + 1) * 4] = cw[:, j * 512 + h * 128: j * 512 + (h + 1) * 128].T
    small[:, 12] = inp["a_log"][0, h]
    small[:, 13] = inp["dt_bias"][0, h]
    small[:, 14] = np.tile(inp["q_norm_g"][0], 2)
    small[:, 15] = np.tile(inp["k_norm_g"][0], 2)
    small[:, 128:256] = np.tile(inp["gdn_norm_g"][0][None, :], (128, 1))
    small[:, 256:384] = np.tile(inp["subln_g"][0][None, :], (128, 1))
    m["small"] = small
    m["lamrow"] = np.ascontiguousarray(np.concatenate(
        [inp["lambda_q1"][0], inp["lambda_k1"][0], inp["lambda_q2"][0], inp["lambda_k2"][0]]).reshape(1, 256))
    return m


def build_d(Sq, n_exp=257):
    nc = bass.Bass("TRN2", target_bir_lowering=False)
    b = Bld(nc)
    c = build_consts(b)
    ad = emit_adaln(b, c, want="gates")
    emit_d(b, c, ad, Sq, None, n_exp)
    b.S.emit()
    return nc


def build_fused(Sq, n_exp=257):
    nc = bass.Bass("TRN2", target_bir_lowering=False)
    b = Bld(nc)
    S = b.S
    c = build_consts(b)
    ad = emit_adaln(b, c, want="gates")
    CH = min(1024, Sq)
    NCH = Sq // CH
    sends = [S.dram(nc.dram_tensor(f"send_bounce{i}", [CH, 256], F32).ap(), f"send{i}") for i in range(NCH)]
    gaths = [S.dram(nc.dram_tensor(f"gath_bounce{i}", [4 * CH, 256], F32).ap(), f"gath{i}") for i in range(NCH)]

    def send_at(r0, n, c0, key):
        ch, lr = r0 // CH, r0 % CH
        return sends[ch].t[lr:lr + n, c0:c0 + 128], sends[ch].sub(key)

    def after_group(g):
        if ((g + 1) * 512) % CH == 0:
            ch = ((g + 1) * 512) // CH - 1
            sd, gt = sends[ch], gaths[ch]
            S.cc(lambda e: e.collective_compute("AllGather", ALU.bypass, replica_groups=[[0, 1, 2, 3], [4, 5, 6, 7]],
                                                ins=[sd.t.opt()], outs=[gt.t.opt()]),
                 R=[sd] + list(sd.subs.values()), W=[gt])

    emit_ab(b, c, ad["ab"], Sq, send_at, after_group)
    emit_d(b, c, ad, Sq, (gaths, CH), n_exp)
    S.emit()
    return nc


def emit_d(b, c, ad, Sq, gath, n_exp):
    S = b.S
    nc = b.nc
    T = Sq // 4
    NT = T // 128
    MG = min(512, T)
    NTG = T // MG
    TPG = MG // 128
    ab, g1bc, g2bc = ad["ab"], ad["g1bc"], ad["g2bc"]
    ident = c["ident"]; ct = c["t"]
    xs = b.inp("xs", [T, 1024])
    if gath is None:
        oa_in = b.inp("oa_in", [T, 512])
        ob_in = b.inp("ob_in", [T, 512])
    else:
        sel_d = b.inp("sel", [128, 4])
    wg_d = b.inp("w_gates", [1024, 2048])
    wog_d = b.inp("w_o_gdn", [512, 1024])
    wod_d = b.inp("w_o_diff", [512, 1024])
    wout_d = b.inp("w_out", [1024, 1024])
    wr_d = b.inp("w_router", [1024, 256])
    rb_d = b.inp("rbias", [128, 256])
    wgu_d = b.inp("w_gu", [n_exp, 1024, 512])
    wdn_d = b.inp("w_dn", [n_exp, 256, 1024])
    outd = b.outp("out", [T, 1024])

    acc = S.sb("acc", [128, NT, 1024])
    h2b = S.sb("h2T_bf", [128, 8, T], BF16)
    wt = S.sb("wt_all", [128, NT, n_exp + 1])
    b.memset(wt.t[:, :, 256:n_exp + 1], 1.0, W=[wt])
    with S.scope():
        wr = S.sb("wr", [128, 8, 256]); rb = S.sb("rb", [128, 256])
        b.dma(wr.t[:], wr_d.t[:, :].rearrange("(k p) c -> p k c", p=128), R=[wr_d], W=[wr])
        b.dma(rb.t[:], rb_d.t[:, :], R=[rb_d], W=[rb])
        wgb = [S.sb(f"wgb{i}", [128, 8, 512]) for i in range(2)]
        xt = [S.sb(f"dxt{i}", [128, 1024]) for i in range(1)]
        xn = S.sb("dxn", [128, 1024]); ssx = S.sb("dssx", [128, 1])
        hT1 = S.sb("hT1", [128, 8, 128]); h2f = hT1
        sg = S.sb("sg", [128, 2048])
        if gath is None:
            oat = S.sb("oat", [128, 512]); obt = S.sb("obt", [128, 512])
        else:
            cand = [S.sb(f"cand{i}", [128, 4, 256]) for i in range(2)]
            cmb = S.sb("cmb", [128, 4, 256])
            sel = S.sb("sel_sb", [128, 4])
            b.dma(sel.t[:], sel_d.t[:, :], R=[sel_d], W=[sel])
            gaths, CH = gath
        oaT = S.sb("oaT", [128, 4, 128]); obT = S.sb("obT", [128, 4, 128])
        mg = S.sb("merged", [128, 1024]); tmp = S.sb("dtmp", [128, 512]); mT = hT1
        scr = mg
        class _V:
            def __init__(self, tn, lo):
                self.t = tn.t[:, lo:lo + 256]; self.b = tn.b
        sc_ = _V(sg, 0); ch = _V(sg, 256); mc = _V(sg, 512)
        m8 = S.sb("r_m8", [128, 8]); gs = S.sb("r_gs", [128, 8]); gm = S.sb("r_gm", [128, 8]); pen = S.sb("r_pen", [128, 8])
        rsum = S.sb("r_sum", [128, 2])
        gi = 0
        for tt in range(NT):
            x_ = xt[0]
            b.dma(x_.t[:], xs.t[tt * 128:(tt + 1) * 128, :], R=[xs], W=[x_])
            if gath is None:
                b.dma(oat.t[:], oa_in.t[tt * 128:(tt + 1) * 128, :], R=[oa_in], W=[oat])
                b.dma(obt.t[:], ob_in.t[tt * 128:(tt + 1) * 128, :], R=[ob_in], W=[obt])
                srcs = ((oat, lambda cc: oat.t[:, cc * 128:(cc + 1) * 128]), (obt, lambda cc: obt.t[:, cc * 128:(cc + 1) * 128]))
            else:
                cf = lambda t_: t_.t[:].rearrange("p a b -> p (a b)")
                for hp in range(4):
                    cd = cand[hp % 2]
                    gtok = hp * T + tt * 128
                    gsrc = gaths[gtok // CH]
                    lr = gtok % CH
                    b.dma(cd.t[:], gsrc.t.rearrange("(i s) c -> s i c", i=4)[lr:lr + 128, :, :], R=[gsrc], W=[cd])
                    if hp == 0:
                        b.ts(cf(cmb), cf(cd), sel.t[:, 0:1], None, ALU.mult, None, R=[cd, sel], W=[cmb])
                    else:
                        b.stt(cf(cmb), cf(cd), sel.t[:, hp:hp + 1], cf(cmb), ALU.mult, ALU.add, R=[cd, sel, cmb], W=[cmb])
                srcs = ((cmb, lambda cc: cmb.t[:, cc, 0:128]), (cmb, lambda cc: cmb.t[:, cc, 128:256]))
            norm_tile2(b, c, x_, xn, ssx, scr, ab, 0, [hT1.t], [hT1], 0)
            for cbk in range(4):
                w = wgb[gi % 2]; gi += 1
                b.dma(w.t[:], wg_d.t[:, cbk * 512:(cbk + 1) * 512].rearrange("(k p) c -> p k c", p=128), R=[wg_d], W=[w])
                bk = b.bank()
                for k in range(8):
                    b.mm(bk, bk.t[:, 0:512], hT1.t[:, k, :], w.t[:, k, :], R=[hT1, w], start=(k == 0), stop=(k == 7))
                b.act(sg.t[:, cbk * 512:(cbk + 1) * 512], bk.t[:, 0:512], AF.Sigmoid, R=[bk], W=[sg])
            for ((src, view), dst) in zip(srcs, (oaT, obT)):
                bk = b.bank()
                for cc in range(4):
                    b.tp(bk, bk.t[:, cc * 128:(cc + 1) * 128], view(cc), ident, R=[src, ct])
                b.cp(dst.t[:].rearrange("p a b -> p (a b)"), bk.t[:, 0:512], R=[bk], W=[dst])
            wogT = wgb[gi % 2]; gi += 1
            b.dma(wogT.t[:].rearrange("p k c -> p (k c)").rearrange("p (a f) -> p a f", a=4), wog_d.t[:, :].rearrange("(k p) c -> p k c", p=128), R=[wog_d], W=[wogT])
            wodT = wgb[gi % 2]; gi += 1
            b.dma(wodT.t[:].rearrange("p k c -> p (k c)").rearrange("p (a f) -> p a f", a=4), wod_d.t[:, :].rearrange("(k p) c -> p k c", p=128), R=[wod_d], W=[wodT])
            wog_v = wogT.t[:].rearrange("p k c -> p (k c)").rearrange("p (a f) -> p a f", a=4)
            wod_v = wodT.t[:].rearrange("p k c -> p (k c)").rearrange("p (a f) -> p a f", a=4)
            for hf in range(2):
                bk = b.bank()
                for cc in range(4):
                    b.mm(bk, bk.t[:, 0:512], oaT.t[:, cc, :], wog_v[:, cc, hf * 512:(hf + 1) * 512], R=[oaT, wogT], start=(cc == 0), stop=(cc == 3))
                b.tt(mg.t[:, hf * 512:(hf + 1) * 512], bk.t[:, 0:512], sg.t[:, hf * 512:(hf + 1) * 512], ALU.mult, R=[bk, sg], W=[mg])
                bk = b.bank()
                for cc in range(4):
                    b.mm(bk, bk.t[:, 0:512], obT.t[:, cc, :], wod_v[:, cc, hf * 512:(hf + 1) * 512], R=[obT, wodT], start=(cc == 0), stop=(cc == 3))
                b.tt(tmp.t[:], bk.t[:, 0:512], sg.t[:, 1024 + hf * 512:1024 + (hf + 1) * 512], ALU.mult, R=[bk, sg], W=[tmp])
                b.tt(mg.t[:, hf * 512:(hf + 1) * 512], mg.t[:, hf * 512:(hf + 1) * 512], tmp.t[:], ALU.add, R=[mg, tmp], W=[mg])
            for k2 in range(2):
                bk = b.bank()
                for kk in range(4):
                    k = k2 * 4 + kk
                    b.tp(bk, bk.t[:, kk * 128:(kk + 1) * 128], mg.t[:, k * 128:(k + 1) * 128], ident, R=[mg, ct])
                b.cp(mT.t[:, k2 * 4:(k2 + 1) * 4, :].rearrange("p a b -> p (a b)"), bk.t[:, 0:512], R=[bk], W=[mT])
            for hf in range(2):
                wo_ = wgb[gi % 2]; gi += 1
                b.dma(wo_.t[:], wout_d.t[:, hf * 512:(hf + 1) * 512].rearrange("(k p) c -> p k c", p=128), R=[wout_d], W=[wo_])
                bk = b.bank()
                for k in range(8):
                    b.mm(bk, bk.t[:, 0:512], mT.t[:, k, :], wo_.t[:, k, :], R=[mT, wo_], start=(k == 0), stop=(k == 7))
                b.tt(tmp.t[:], bk.t[:, 0:512], g1bc.t[:, hf * 512:(hf + 1) * 512], ALU.mult, R=[bk, g1bc], W=[tmp])
                b.tt(acc.t[:, tt, hf * 512:(hf + 1) * 512], tmp.t[:], x_.t[:, hf * 512:(hf + 1) * 512], ALU.add, R=[tmp, x_], W=[acc.sub(tt)])
            norm_tile2(b, c, acc.sub(tt), xn, ssx, scr, ab, 16, [h2f.t, h2b.t], [h2f, h2b.sub(tt // TPG)], 0, xap=acc.t[:, tt, :], cols=[0, tt * 128])
            bk = b.bank()
            for k in range(8):
                b.mm(bk, bk.t[:, 0:256], h2f.t[:, k, :], wr.t[:, k, :], R=[h2f, wr], start=(k == 0), stop=(k == 7))
            b.act(sc_.t[:], bk.t[:, 0:256], AF.Sigmoid, R=[bk], W=[sc_])
            b.tt(ch.t[:], sc_.t[:], rb.t[:], ALU.add, R=[sc_, rb], W=[ch])
            for g in range(8):
                b.S.op("dve", lambda e, g=g: e.max(out=m8.t[:], in_=ch.t[:, g * 32:(g + 1) * 32]), R=[ch], W=[m8])
                b.tt(gs.t[:, g:g + 1], m8.t[:, 0:1], m8.t[:, 1:2], ALU.add, R=[m8], W=[gs])
            b.S.op("dve", lambda e: e.max(out=m8.t[:], in_=gs.t[:]), R=[gs], W=[m8])
            b.ts(gm.t[:], gs.t[:], m8.t[:, 3:4], None, ALU.is_ge, None, R=[gs, m8], W=[gm])
            b.ts(pen.t[:], gm.t[:], -1.0, 1e9, ALU.add, ALU.mult, R=[gm], W=[pen])
            for g in range(8):
                b.ts(mc.t[:, g * 32:(g + 1) * 32], ch.t[:, g * 32:(g + 1) * 32], gm.t[:, g:g + 1], pen.t[:, g:g + 1], ALU.mult, ALU.add,
                     R=[ch, gm, pen], W=[mc])
            b.S.op("dve", lambda e: e.max(out=m8.t[:], in_=mc.t[:]), R=[mc], W=[m8])
            b.ts(mc.t[:], mc.t[:], m8.t[:, 7:8], None, ALU.is_ge, None, R=[mc, m8], W=[mc])
            b.tt(mc.t[:], mc.t[:], sc_.t[:], ALU.mult, R=[mc, sc_], W=[mc])
            b.S.op("dve", lambda e: e.tensor_reduce(out=rsum.t[:, 0:1], in_=mc.t[:], axis=AX.X, op=ALU.add), R=[mc], W=[rsum])
            b.S.op("dve", lambda e: e.reciprocal(out=rsum.t[:, 1:2], in_=rsum.t[:, 0:1]), R=[rsum], W=[rsum])
            b.ts(wt.t[:, tt, 0:256], mc.t[:], rsum.t[:, 1:2], 2.5, ALU.mult, ALU.mult, R=[mc, rsum], W=[wt])
    with S.scope():
        wgu_st = [S.sb(f"wgu_st{i}", [128, 8, 512]) for i in range(2)]
        wd_st = [S.sb(f"wd_st{i}", [128, 2, 1024]) for i in range(2)]
        wgu_bf = [S.sb(f"wgu_bf{i}", [128, 8, 512], BF16) for i in range(2)]
        wd_bf = [S.sb(f"wd_bf{i}", [128, 2, 1024], BF16) for i in range(2)]
        AT = [S.sb(f"AT{i}", [128, 2, MG], BF16) for i in range(2)]
        slu = [S.sb(f"slu{i}", [128, MG]) for i in range(2)]
        for e in range(n_exp):
            p = e % 2
            b.dma(wgu_st[p].t[:], wgu_d.t[e].rearrange("(k p) c -> p k c", p=128), R=[wgu_d], W=[wgu_st[p]])
            b.dma(wd_st[p].t[:], wdn_d.t[e].rearrange("(k p) c -> p k c", p=128), R=[wdn_d], W=[wd_st[p]])
            b.act(wgu_bf[p].t[:, 0:4, :], wgu_st[p].t[:, 0:4, :], AF.Copy, R=[wgu_st[p]], W=[wgu_bf[p]])
            b.act(wgu_bf[p].t[:, 4:8, :], wgu_st[p].t[:, 4:8, :], AF.Copy, R=[wgu_st[p]], W=[wgu_bf[p]])
            for c2 in range(2):
                b.tt(wd_bf[p].t[:, c2, :], wd_st[p].t[:, c2, :], g2bc.t[:], ALU.mult, R=[wd_st[p], g2bc], W=[wd_bf[p]], eng="dve")
            def gu_stage(tg, c2):
                bg, bu = b.banks[2 * c2], b.banks[2 * c2 + 1]
                for (bk, fc) in ((bg, c2), (bu, 2 + c2)):
                    for k in range(8):
                        b.mm(bk, bk.t[:, 0:MG], wgu_bf[p].t[:, k, fc * 128:(fc + 1) * 128], h2b.t[:, k, tg * MG:(tg + 1) * MG],
                             R=[wgu_bf[p], h2b.sub(tg)], start=(k == 0), stop=(k == 7))
                A = AT[tg % 2]
                sl = slu[c2]
                b.act(sl.t[:], bg.t[:, 0:MG], AF.Silu, R=[bg], W=[sl])
                b.tt(A.t[:, c2, :], sl.t[:], bu.t[:, 0:MG], ALU.mult, R=[sl, bu], W=[A])

            def down_stage(tg):
                A = AT[tg % 2]
                for ti in range(TPG):
                    tile_ = tg * TPG + ti
                    for hf in range(2):
                        bk = b.bank()
                        for c2 in range(2):
                            b.mm(bk, bk.t[:, 0:512], A.t[:, c2, ti * 128:(ti + 1) * 128], wd_bf[p].t[:, c2, hf * 512:(hf + 1) * 512],
                                 R=[A, wd_bf[p]], start=(c2 == 0), stop=(c2 == 1))
                        b.stt(acc.t[:, tile_, hf * 512:(hf + 1) * 512], bk.t[:, 0:512], wt.t[:, tile_, e:e + 1],
                              acc.t[:, tile_, hf * 512:(hf + 1) * 512], ALU.mult, ALU.add, R=[bk, wt, acc.sub(tile_)], W=[acc.sub(tile_)])

            b.bank_pool = [4, 5, 6, 7]
            for tg in range(NTG):
                gu_stage(tg, 0)
                gu_stage(tg, 1)
                if tg >= 1:
                    down_stage(tg - 1)
            down_stage(NTG - 1)
            b.bank_pool = list(range(8))
        for tt in range(NT):
            b.dma(outd.t[tt * 128:(tt + 1) * 128, :], acc.t[:, tt, :], R=[acc.sub(tt)], W=[outd.sub(tt)], final=True)


def prep_d(inp, bidx, j, T, oa_b, ob_b):
    m = prep_common(inp, bidx)
    m["consts"] = host_consts(alibi_slope(0))
    w_in = inp["w_in"][0]
    m["xs"] = np.ascontiguousarray(inp["x"][bidx, j * T:(j + 1) * T])
    m["oa_in"] = np.ascontiguousarray(oa_b[j * T:(j + 1) * T])
    m["ob_in"] = np.ascontiguousarray(ob_b[j * T:(j + 1) * T])
    m["w_gates"] = np.ascontiguousarray(w_in[:, W_OFF["ga"]:W_OFF["ga"] + 2048])
    m["w_o_gdn"] = np.ascontiguousarray(inp["w_o_gdn"][0])
    m["w_o_diff"] = np.ascontiguousarray(inp["w_o_diff"][0])
    m["w_out"] = np.ascontiguousarray(inp["w_out"][0])
    m["w_router"] = np.ascontiguousarray(inp["w_router"][0])
    m["rbias"] = np.ascontiguousarray(np.tile(inp["router_bias"][0][None, :], (128, 1)))
    return m


_EXP_CACHE = {}


def expert_stack(inp):
    key = id(inp["w_exp_gate_up"])
    if key not in _EXP_CACHE:
        _EXP_CACHE.clear()
        wgu = np.concatenate([inp["w_exp_gate_up"][0], inp["w_shared_gate_up"][0][None]], axis=0)
        wdn = np.concatenate([inp["w_exp_down"][0], inp["w_shared_down"][0][None]], axis=0)
        _EXP_CACHE[key] = (np.ascontiguousarray(wgu), np.ascontiguousarray(wdn))
    return _EXP_CACHE[key]


def prep_fused(inp, cid, T):
    bi, j = cid // 4, cid % 4
    m = prep_ab(inp, bi, j)
    w_in = inp["w_in"][0]
    m["xs"] = np.ascontiguousarray(inp["x"][bi, j * T:(j + 1) * T])
    m["w_gates"] = np.ascontiguousarray(w_in[:, W_OFF["ga"]:W_OFF["ga"] + 2048])
    m["w_o_gdn"] = np.ascontiguousarray(inp["w_o_gdn"][0])
    m["w_o_diff"] = np.ascontiguousarray(inp["w_o_diff"][0])
    m["w_out"] = np.ascontiguousarray(inp["w_out"][0])
    m["w_router"] = np.ascontiguousarray(inp["w_router"][0])
    m["rbias"] = np.ascontiguousarray(np.tile(inp["router_bias"][0][None, :], (128, 1)))
    sel = np.zeros((128, 4), np.float32)
    sel[:, j] = 1.0
    m["sel"] = sel
    return m


def kernel(**inp):
    inp = {k: np.asarray(v) for k, v in inp.items()}
    B, Sq, _ = inp["x"].shape
    T = Sq // 4
    nc = build_fused(Sq)
    wgu, wdn = expert_stack(inp)
    maps = []
    for cid in range(8):
        m = prep_fused(inp, cid, T)
        m["w_gu"] = wgu
        m["w_dn"] = wdn
        maps.append(m)
    res = run_bass_kernel_spmd(nc, maps, core_ids=list(range(8)))
    out = np.zeros((B, Sq, 1024), np.float32)
    for cid in range(8):
        bi, j = cid // 4, cid % 4
        out[bi, j * T:(j + 1) * T] = res.results[cid]["out"]
    return out
```
